# Optimizing a Trainium2 kernel written in Bass

```python
import math
import jax, jax.numpy as jnp
from jax import lax
import numpy as np

D_MODEL = 1024
BATCH = 2
SEQ = 8192
DEPTH = 1

D_MIX = D_MODEL
D_A = D_MIX // 2
D_B = D_MIX // 2
BLK = 128
A_HEADS = 4
A_HD = D_A // A_HEADS
HEAD_DIM = 64
N_HEADS = D_B // HEAD_DIM
N_KV = 2
GQA = N_HEADS // N_KV
WINDOW = 128
NUM_BUCKETS = 32
MAX_DIST = 128
D_IN = 2 * D_A + D_B + 2 * N_KV * HEAD_DIM
N_GROUPS = 4
E_PER_GROUP = 8
N_EXPERTS = N_GROUPS * E_PER_GROUP
TOP_K = 2
D_FF_E = 256
PLE_DIM = 256
EPS = 1e-6

kernel_name = "hymba_gmlp_swa_hmoe_layer"


def rmsnorm(x, g):
    x32 = x.astype(jnp.float32)
    y = x32 * lax.rsqrt(jnp.mean(x32 * x32, axis=-1, keepdims=True) + EPS)
    return (y * g.astype(jnp.float32)).astype(x.dtype)


def layernorm(x, g, b):
    x32 = x.astype(jnp.float32)
    mu = jnp.mean(x32, axis=-1, keepdims=True)
    var = jnp.mean(jnp.square(x32 - mu), axis=-1, keepdims=True)
    y = (x32 - mu) * lax.rsqrt(var + EPS)
    return (y * g.astype(jnp.float32) + b.astype(jnp.float32)).astype(x.dtype)


def t5_bucket(rel):
    n = NUM_BUCKETS // 2
    max_exact = n // 2
    ret = jnp.where(rel > 0, n, 0)
    a = jnp.abs(rel)
    large = max_exact + (jnp.log(jnp.maximum(a, 1).astype(jnp.float32) / max_exact)
                         / math.log(MAX_DIST / max_exact) * (n - max_exact)).astype(jnp.int32)
    large = jnp.minimum(large, n - 1)
    return ret + jnp.where(a < max_exact, a, large)


def gmlp_group(u, v, ln_g, ln_b, w_s, b_s):
    B, S, _ = v.shape
    nc = S // BLK
    v = layernorm(v, ln_g, ln_b).reshape(B, nc, BLK, A_HEADS, A_HD)
    sv = jnp.einsum("hij,bcjhd->bcihd", w_s, v) + b_s.T[:, :, None]
    return u * sv.reshape(B, S, D_A)


def band(t):
    B, S = t.shape[:2]
    nb = S // BLK
    tp = jnp.pad(t, ((0, 0), (BLK, BLK), (0, 0), (0, 0)))
    tb = tp.reshape(B, nb + 2, BLK, N_KV, HEAD_DIM)
    return jnp.concatenate([tb[:, :-2], tb[:, 1:-1], tb[:, 2:]], axis=2)


def windowed_gqa(q, k, v, sink, bias_table):
    B, S, _ = q.shape
    nb = S // BLK
    q = q.reshape(B, nb, BLK, N_KV, GQA, HEAD_DIM)
    kb = band(k.reshape(B, S, N_KV, HEAD_DIM))
    vb = band(v.reshape(B, S, N_KV, HEAD_DIM))
    s = jnp.einsum("bnqkgd,bnjkd->bnkgqj", q, kb).astype(jnp.float32) * (HEAD_DIM ** -0.5)
    i = jnp.arange(BLK, dtype=jnp.int32)[:, None]
    j = jnp.arange(3 * BLK, dtype=jnp.int32)[None, :]
    rel = j - BLK - i
    bias = bias_table.astype(jnp.float32)[t5_bucket(rel)]
    bias = jnp.transpose(bias, (2, 0, 1)).reshape(N_KV, GQA, BLK, 3 * BLK)
    kpos = jnp.arange(nb, dtype=jnp.int32)[:, None, None] * BLK - BLK + j[None]
    valid = (jnp.abs(rel)[None] <= WINDOW) & (kpos >= 0) & (kpos < S)
    s = jnp.where(valid[None, :, None, None], s + bias, -1e30)
    sk = sink.astype(jnp.float32).reshape(N_KV, GQA, 1, 1)
    m = jnp.maximum(jnp.max(s, axis=-1, keepdims=True), sk)
    e = jnp.exp(s - m)
    denom = jnp.sum(e, axis=-1, keepdims=True) + jnp.exp(sk - m)
    pr = (e / denom).astype(vb.dtype)
    o = jnp.einsum("bnkgqj,bnjkd->bnqkgd", pr, vb)
    return o.reshape(B, S, N_HEADS * HEAD_DIM)


def hier_moe(xt, w_rg, b_rg, w_re, b_re, w_gate, w_up, w_down):
    T = xt.shape[0]
    rows = jnp.arange(T)
    pg = jax.nn.softmax((xt @ w_rg + b_rg).astype(jnp.float32), axis=-1)
    g_idx = jnp.argmax(pg, axis=-1)
    pg_top = pg[rows, g_idx]
    le = (xt @ w_re + b_re).astype(jnp.float32).reshape(T, N_GROUPS, E_PER_GROUP)
    pe = jax.nn.softmax(le[rows, g_idx], axis=-1)
    top_v, top_i = lax.top_k(pe, TOP_K)
    w = pg_top[:, None] * top_v / jnp.sum(top_v, axis=-1, keepdims=True)
    eidx = g_idx[:, None] * E_PER_GROUP + top_i
    comb = jnp.zeros((T, N_EXPERTS), jnp.float32).at[rows[:, None], eidx].add(w).astype(xt.dtype)
    y = jnp.zeros_like(xt)
    for e in range(N_EXPERTS):
        hdn = jax.nn.silu(xt @ w_gate[e]) * (xt @ w_up[e])
        y = y + comb[:, e:e + 1] * (hdn @ w_down[e])
    return y


def setup_inputs(seed: int = 0) -> dict:
    key = jax.random.key(seed)
    ks = jax.random.split(key, 32)

    def nrm(k, shape, scale):
        return jax.random.normal(k, shape, jnp.float32) * scale

    def gain(k, shape):
        return 1.0 + nrm(k, shape, 0.02)

    L = DEPTH
    return {
        "x": nrm(ks[0], (BATCH, SEQ, D_MODEL), 1.0),
        "p": nrm(ks[1], (DEPTH, BATCH, SEQ, PLE_DIM), 1.0),
        "rel_bias": nrm(ks[2], (NUM_BUCKETS, N_HEADS), 0.5),
        "g_mix": gain(ks[3], (L, D_MODEL)),
        "w_in": nrm(ks[4], (L, D_MODEL, D_IN), D_MODEL ** -0.5),
        "ln_v_g": gain(ks[5], (L, D_A)),
        "ln_v_b": nrm(ks[6], (L, D_A), 0.02),
        "w_spatial": nrm(ks[7], (L, A_HEADS, BLK, BLK), BLK ** -0.5),
        "b_spatial": 1.0 + nrm(ks[8], (L, A_HEADS, BLK), 0.1),
        "sink": nrm(ks[9], (L, N_HEADS), 0.5),
        "g_out_grp": gain(ks[10], (L, D_MIX)),
        "w_out": nrm(ks[11], (L, D_MIX, D_MODEL), D_MIX ** -0.5),
        "g_ffn": gain(ks[12], (L, D_MODEL)),
        "w_router_group": nrm(ks[13], (L, D_MODEL, N_GROUPS), D_MODEL ** -0.5),
        "b_router_group": nrm(ks[14], (L, N_GROUPS), 0.01),
        "w_router_expert": nrm(ks[15], (L, D_MODEL, N_EXPERTS), D_MODEL ** -0.5),
        "b_router_expert": nrm(ks[16], (L, N_EXPERTS), 0.01),
        "w_gate_e": nrm(ks[17], (L, N_EXPERTS, D_MODEL, D_FF_E), D_MODEL ** -0.5),
        "w_up_e": nrm(ks[18], (L, N_EXPERTS, D_MODEL, D_FF_E), D_MODEL ** -0.5),
        "w_down_e": nrm(ks[19], (L, N_EXPERTS, D_FF_E, D_MODEL), D_FF_E ** -0.5),
        "w_ple_proj": nrm(ks[20], (L, PLE_DIM, D_MODEL), PLE_DIM ** -0.5),
        "g_ple": gain(ks[21], (L, D_MODEL)),
        "w_ple_gate": nrm(ks[22], (L, D_MODEL, D_MODEL), D_MODEL ** -0.5),
        "b_ple_gate": nrm(ks[23], (L, D_MODEL), 0.02),
        "g_final": gain(ks[24], (D_MODEL,)),
    }


def reference(x, p, rel_bias, g_mix, w_in, ln_v_g, ln_v_b, w_spatial, b_spatial, sink,
              g_out_grp, w_out, g_ffn, w_router_group, b_router_group, w_router_expert,
              b_router_expert, w_gate_e, w_up_e, w_down_e, w_ple_proj, g_ple, w_ple_gate,
              b_ple_gate, g_final):
    B, S, D = x.shape
    h = x
    c0, c1, c2 = D_A, 2 * D_A, 2 * D_A + D_B
    c3 = c2 + N_KV * HEAD_DIM
    for i in range(DEPTH):
        a = rmsnorm(h, g_mix[i])
        z = a @ w_in[i]
        uv = jax.nn.gelu(z[..., :c1])
        y_a = gmlp_group(uv[..., :c0], uv[..., c0:c1], ln_v_g[i], ln_v_b[i],
                         w_spatial[i], b_spatial[i])
        y_b = windowed_gqa(z[..., c1:c2], z[..., c2:c3], z[..., c3:], sink[i], rel_bias)
        y = jnp.concatenate([rmsnorm(y_a, g_out_grp[i, :D_A]),
                             rmsnorm(y_b, g_out_grp[i, D_A:])], axis=-1)
        h = h + y @ w_out[i]
        m = rmsnorm(h, g_ffn[i]).reshape(B * S, D)
        h = h + hier_moe(m, w_router_group[i], b_router_group[i], w_router_expert[i],
                         b_router_expert[i], w_gate_e[i], w_up_e[i], w_down_e[i]).reshape(B, S, D)
        gate = jax.nn.sigmoid((h @ w_ple_gate[i] + b_ple_gate[i]).astype(jnp.float32)).astype(h.dtype)
        h = h + gate * rmsnorm(p[i] @ w_ple_proj[i], g_ple[i])
    return rmsnorm(h, g_final)
```

```python
import contextlib
import os
import numpy as np
import concourse.bass as bass
import concourse.mybir as mybir
from concourse.bass_utils import run_bass_kernel_spmd

F32 = mybir.dt.float32
BF16 = mybir.dt.bfloat16
AF = mybir.ActivationFunctionType
ALU = mybir.AluOpType
AX = mybir.AxisListType

NCORES = 8
D = 1024
SEQ = 8192
BATCH = 2
TPC = 2048
NB = 16
NBH = 18
D_IN = 1792
NE = 32
EPS = 1e-6
BIG = 1.0e30
CAP = 256
NOV = 31
OVB = NE * CAP
NSLOT = OVB + NOV * 128
I32 = mybir.dt.int32

ENGS = ("pe", "act", "dve", "pool", "sp")
EPOCH = 8192
NDMASEM = 24
NDMASEM_Q = {"pool": 72}


class Prog:
    def __init__(self, nc):
        self.nc = nc
        self.streams = {e: [] for e in ENGS}
        self.cnt = {e: 0 for e in ENGS}
        self.last_w = {}
        self.readers = {}
        self.waited = {}
        self.dma_known = {e: set() for e in ENGS}
        self.sems = {}
        self.dsems = {}
        self.dma_rr = {"sp": 0, "pool": 0, "act": 0}
        self.ndma = 0
        self.out_dmas = []
        self.bank_acc = {}

    def _sem(self, eng, epoch):
        k = (eng, epoch)
        if k not in self.sems:
            self.sems[k] = self.nc.alloc_semaphore(f"s_{eng}_{epoch}")
        return self.sems[k]

    def _semval(self, eng, seq):
        return (self._sem(eng, (seq - 1) // EPOCH), (seq - 1) % EPOCH + 1)

    def _dep_waits(self, eng, reads, writes, strict=False):
        deps = []
        for k in reads:
            r = self.last_w.get(k)
            if r is not None:
                deps.append((r, True))
        for k in writes:
            r = self.last_w.get(k)
            if r is not None:
                deps.append((r, False))
            for r in self.readers.get(k, {}).values():
                deps.append((r, False))
        for k in list(reads) + list(writes):
            if k.startswith("bank"):
                r = self.bank_acc.get(k)
                if r is not None and r[1] != eng:
                    deps.append((r, False))
        waits = []
        for r, raw in deps:
            if r[0] == "dma":
                _, did, sem, val = r
                if did in self.dma_known[eng]:
                    continue
                self.dma_known[eng].add(did)
                waits.append((sem, val))
            else:
                _, peng, seq = r
                if peng == eng and not strict and eng == "pe":
                    continue
                if self.waited.get((eng, peng), 0) >= seq:
                    continue
                self.waited[(eng, peng)] = seq
                waits.append(self._semval(peng, seq))
        return waits

    def _commit(self, ref, rkey, reads, writes):
        for k in writes:
            self.last_w[k] = ref
            self.readers[k] = {}
        for k in reads:
            self.readers.setdefault(k, {})[rkey] = ref

    op_limit = None

    def op(self, eng, fn, reads=(), writes=()):
        if self.op_limit is not None:
            if self.op_limit <= 0:
                return None
            self.op_limit -= 1
        waits = self._dep_waits(eng, reads, writes)
        self.cnt[eng] += 1
        seq = self.cnt[eng]
        ref = ("eng", eng, seq)
        self._commit(ref, eng, reads, writes)
        for k in list(reads) + list(writes):
            if k.startswith("bank"):
                self.bank_acc[k] = ref
        self.streams[eng].append((waits, fn, self._semval(eng, seq)[0], 1))
        return ref

    def dma(self, queue, fn, reads=(), writes=(), is_out=False):
        waits = self._dep_waits(queue, reads, writes, strict=True)
        nsem = NDMASEM_Q.get(queue, NDMASEM)
        idx = self.dma_rr[queue]
        self.dma_rr[queue] = (idx + 1) % nsem
        k = (queue, idx)
        if k not in self.dsems:
            self.dsems[k] = [self.nc.alloc_semaphore(f"d_{queue}_{idx}"), 0, None]
        ent = self.dsems[k]
        if ent[2] is not None and ent[2] not in self.dma_known[queue]:
            waits.append((ent[0], ent[1] * 16))
            self.dma_known[queue].add(ent[2])
        ent[1] += 1
        self.ndma += 1
        did = self.ndma
        ent[2] = did
        ref = ("dma", did, ent[0], ent[1] * 16)
        self._commit(ref, ("d", did), reads, writes)
        self.streams[queue].append((waits, fn, ent[0], 16))
        if is_out:
            self.out_dmas.append(ref)
        return ref

    def barrier(self):
        for eng in ENGS:
            waits = []
            for peng in ENGS:
                if (peng != eng or eng in ("act", "dve", "pool")) and self.cnt[peng] > 0 and self.waited.get((eng, peng), 0) < self.cnt[peng]:
                    self.waited[(eng, peng)] = self.cnt[peng]
                    waits.append(self._semval(peng, self.cnt[peng]))
            for (q, idx), ent in self.dsems.items():
                if ent[2] is not None and ent[2] not in self.dma_known[eng]:
                    self.dma_known[eng].add(ent[2])
                    waits.append((ent[0], ent[1] * 16))
            self.streams[eng].append((waits, None, None, 0))
        self.last_w = {}
        self.readers = {}
        self.bank_acc = {}

    def finish(self):
        waits = []
        for r in self.out_dmas:
            if r[1] not in self.dma_known["sp"]:
                self.dma_known["sp"].add(r[1])
                waits.append((r[2], r[3]))
        self.streams["sp"].append((waits, None, None, 0))

    def emit(self):
        nc = self.nc
        streams = self.streams

        def run(name, eng):
            for waits, fn, sem, inc in streams[name]:
                for s, v in waits:
                    eng.wait_ge(s, v)
                if fn is None:
                    continue
                fn(eng).then_inc(sem, inc)

        with nc.Block() as block:
            @block.sync
            def _(e):
                run("sp", e)

            @block.tensor
            def _(e):
                run("pe", e)

            @block.scalar
            def _(e):
                run("act", e)

            @block.vector
            def _(e):
                run("dve", e)

            @block.gpsimd
            def _(e):
                run("pool", e)


def bc(ap, shape):
    return ap.unsqueeze(len(ap.shape)).broadcast_to(list(shape))


def build_program(stage=99, ndbg=0, nblk=None):
    nc = bass.Bass("TRN2", target_bir_lowering=False)

    def din(name, shape):
        return nc.dram_tensor(name, list(shape), F32, kind="ExternalInput").ap()

    xh = din("xh", [NBH * 128, D])
    pin = din("p", [TPC, 256])
    flags_d = din("flags", [128, 2])
    biasg_d = din("biasg", [128, 3072])
    maskc_d = din("maskc", [128, 3072])
    ident_d = din("ident", [128, 128])
    w_in_d = din("w_in", [D, D_IN])
    gvec_d = din("gvec", [128, 24])
    lnv_d = din("lnv", [2, 512])
    wsT_d = din("wsT", [128, 512])
    bsT_d = din("bsT", [128, 4])
    sink_d = din("sink", [1, 8])
    w_out_d = din("w_out", [D, D])
    wr_d = din("wr", [D, 36])
    br_d = din("br", [1, 36])
    wall_d = din("w_exp", [NE * 128, 3, 2048])
    wpp_d = din("w_ple_proj", [256, D])
    wpg_d = din("w_ple_gate", [D, D])
    rows_d = din("rows", [3, D])
    tri_d = din("tri", [128, 256])
    cvec_d = din("cvec", [128, 128])
    mg_d = nc.dram_tensor("mg", [NSLOT, D], BF16, kind="Internal").ap()
    og_d = nc.dram_tensor("og", [NSLOT, D], BF16, kind="Internal").ap()
    wbf_d = nc.dram_tensor("wbf", [NE * 128, 6144], BF16, kind="Internal").ap()
    out_d = nc.dram_tensor("out", [TPC, D], F32, kind="ExternalOutput").ap()
    dbg_d = None
    if ndbg:
        dbg_d = nc.dram_tensor("dbg", [128, ndbg], F32, kind="ExternalOutput").ap()

    P = Prog(nc)
    def sb(name, shape, dt):
        return nc.alloc_sbuf_tensor("s_" + name, shape, dt)
    bank = [nc.alloc_psum_tensor(f"bank{i}", [128, 512], F32) for i in range(8)]
    bkey = [f"bank{i}" for i in range(8)]

    h = sb("h", [128, NB, D], F32)
    hnb = sb("hnb", [128, NB, D], BF16)
    w12 = sb("w12", [128, 2, NB], F32)
    idx = sb("idx", [128, 2, NB], I32)
    trib = sb("trib", [128, 256], BF16)
    cvec = sb("cvec", [128, 128], F32)
    gidx = sb("gidx", [128, NOV], I32)
    idb = sb("idb", [128, 128], BF16)
    idf = sb("idf", [128, 128], F32)
    gvec = sb("gvec", [128, 24], F32)
    cst_ = sb("cst", [128, 4], F32)
    stat = sb("stat", [128, 160], F32)
    junk = sb("junk", [128, D], BF16)
    L = sb("L", [128, NB, 36], F32)
    flags = sb("flags", [128, 2], F32)

    dbg_col = [0]

    def dump(ap_sb, ncols, keys, cast=False):
        c0 = dbg_col[0]
        dbg_col[0] += ncols
        assert dbg_col[0] <= ndbg, dbg_col[0]
        q = "pool" if cast else "sp"
        P.dma(q, lambda e: e.dma_start(out=dbg_d[:, c0:c0 + ncols], in_=ap_sb), reads=keys, is_out=True)
        return c0

    conv_todo = [(ex, k) for ex in range(NE) for k in range(3)]

    def conv_step(n):
        for _ in range(n):
            if not conv_todo:
                return
            ex, k = conv_todo.pop(0)
            P.dma("pool", lambda e, ex=ex, k=k: e.dma_start(out=wbf_d[ex * 128:(ex + 1) * 128, k * 2048:(k + 1) * 2048],
                                                            in_=wall_d[ex * 128:(ex + 1) * 128, k, :]), writes=[f"wbf{ex}_{k}"])

    def rstd_from_ss(col, n, key):
        P.op("dve", lambda e: e.tensor_scalar(out=stat[:, col:col + 1], in0=stat[:, col:col + 1], scalar1=1.0 / n,
                                              scalar2=EPS, op0=ALU.mult, op1=ALU.add), reads=[key], writes=[key])
        P.op("pool", lambda e: e.tensor_tensor(out=stat[:, col:col + 1], in0=stat[:, col:col + 1], in1=cst_[:, 0:1],
                                               op=ALU.pow), reads=[key, "cst"], writes=[key])

    P.dma("sp", lambda e: e.dma_start(out=idf[:], in_=ident_d[:, :]), writes=["idf"])
    P.dma("pool", lambda e: e.dma_start(out=idb[:], in_=ident_d[:, :]), writes=["idb"])
    P.dma("sp", lambda e: e.dma_start(out=gvec[:], in_=gvec_d[:, :]), writes=["gvec"])
    P.dma("sp", lambda e: e.dma_start(out=flags[:], in_=flags_d[:, :]), writes=["flags"])
    P.op("dve", lambda e: e.memset(cst_[:, 0:1], -0.5), writes=["cst"])
    P.dma("pool", lambda e: e.dma_start(out=trib[:], in_=tri_d[:, :]), writes=["trib"])
    P.dma("sp", lambda e: e.dma_start(out=cvec[:], in_=cvec_d[:, :]), writes=["cvec"])
    gmix = gvec[:, 0:8]
    gout = gvec[:, 8:16]
    gffn = gvec[:, 16:24]

    SS_X, SS_A, SS_B, SS_M, SS_V, SS_P, SS_F = 0, 18, 34, 50, 66, 98, 114

    with contextlib.ExitStack() as _es:
        yna = _es.enter_context(nc.sbuf_tensor("s_yna", [128, NB, 512], BF16))
        qT = _es.enter_context(nc.sbuf_tensor("s_qT", [128, 4, TPC], BF16))
        kT = _es.enter_context(nc.sbuf_tensor("s_kT", [128, NBH * 128], BF16))
        vaug = _es.enter_context(nc.sbuf_tensor("s_vaug", [128, NBH, 2, 66], BF16))
        expB = _es.enter_context(nc.sbuf_tensor("s_expB", [128, 3072], BF16))
        esink = _es.enter_context(nc.sbuf_tensor("s_esink", [128, 8], F32))

        with contextlib.ExitStack() as _es:
            w_in_bf = _es.enter_context(nc.sbuf_tensor("s_w_in_bf", [128, 8, D_IN], BF16))
            xn = _es.enter_context(nc.sbuf_tensor("s_xn", [128, 2, D], BF16))
            aT = _es.enter_context(nc.sbuf_tensor("s_aT", [128, 2, 8, 128], BF16))
            uv = _es.enter_context(nc.sbuf_tensor("s_uv", [128, 2, 2, 512], F32))
            vc = _es.enter_context(nc.sbuf_tensor("s_vc", [128, 512], F32))
            vn = _es.enter_context(nc.sbuf_tensor("s_vn", [128, 512], BF16))
            ya = _es.enter_context(nc.sbuf_tensor("s_ya", [128, 512], F32))
            lnv = _es.enter_context(nc.sbuf_tensor("s_lnv", [128, 2, 512], F32))
            wsT = _es.enter_context(nc.sbuf_tensor("s_wsT", [128, 512], BF16))
            bsT = _es.enter_context(nc.sbuf_tensor("s_bsT", [128, 4], F32))
            mTf32 = hnb[:].rearrange("p c t -> p (c t)").bitcast(F32)
            btmp = mTf32[:, 0:3072]
            mtmp = mTf32[:, 3072:6144]
            xhalo = mTf32[:, 6144:8192].rearrange("p (s d) -> p s d", s=2)
            for c in range(8):
                P.dma("pool", lambda e, c=c: e.dma_start(out=w_in_bf[:, c, :], in_=w_in_d[c * 128:(c + 1) * 128, :]),
                      writes=[f"w_in{c}"])
            P.dma("pool", lambda e: e.dma_start(out=wsT[:], in_=wsT_d[:, :]), writes=["wsT"])
            P.dma("sp", lambda e: e.dma_start(out=bsT[:], in_=bsT_d[:, :]), writes=["bsT"])
            for i in range(2):
                P.dma("sp", lambda e, i=i: e.dma_start(out=lnv[:, i, :], in_=lnv_d[i:i + 1, :].broadcast_to([128, 512])),
                      writes=["lnv"])
            P.dma("sp", lambda e: e.dma_start(out=esink[:], in_=sink_d[0:1, :].broadcast_to([128, 8])), writes=["esink"])
            P.op("act", lambda e: e.activation(out=esink[:], in_=esink[:], func=AF.Exp), reads=["esink"], writes=["esink"])
            P.dma("sp", lambda e: e.dma_start(out=btmp, in_=biasg_d[:, :]), writes=["btmp"])
            P.dma("sp", lambda e: e.dma_start(out=mtmp, in_=maskc_d[:, :]), writes=["mtmp"])
            P.op("act", lambda e: e.activation(out=btmp, in_=btmp, func=AF.Exp), reads=["btmp"], writes=["btmp"])
            P.op("dve", lambda e: e.tensor_tensor(out=expB[:], in0=btmp, in1=mtmp, op=ALU.mult),
                 reads=["btmp", "mtmp"], writes=["expB"])
            P.op("dve", lambda e: e.memset(vaug[:, :, :, 64:65], 1.0), writes=["vones"])

            order = [0, NBH - 1] + list(range(1, NBH - 1))
            if nblk is not None:
                order = order[:nblk]
            wk = [f"w_in{c}" for c in range(8)]

            def blk(it):
                bi = order[it]
                halo = bi in (0, NBH - 1)
                hs = 0 if bi == 0 else 1
                xt = xhalo[:, hs, :] if halo else h[:, bi - 1, :]
                xk = f"xhalo{hs}" if halo else f"h{bi - 1}"
                return bi, halo, hs, xt, xk, it % 2

            def a0(it):
                bi, halo, hs, xt, xk, sl = blk(it)
                P.dma("sp", lambda e: e.dma_start(out=xt, in_=xh[bi * 128:(bi + 1) * 128, :]), writes=[xk])

            def a1(it):
                bi, halo, hs, xt, xk, sl = blk(it)
                sk = f"ssx{bi}"
                P.op("act", lambda e: e.activation(out=junk[:], in_=xt, func=AF.Square, accum_out=stat[:, SS_X + bi:SS_X + bi + 1]),
                     reads=[xk], writes=["junk", sk])
                rstd_from_ss(SS_X + bi, D, sk)

            def a1x(it):
                bi, halo, hs, xt, xk, sl = blk(it)
                P.op("act", lambda e: e.activation(out=xn[:, sl, :], in_=xt, func=AF.Copy, scale=stat[:, SS_X + bi:SS_X + bi + 1]),
                     reads=[xk, f"ssx{bi}"], writes=[f"xn{sl}"])

            def a2(it):
                bi, halo, hs, xt, xk, sl = blk(it)
                TBb = bank[sl][:].bitcast(BF16)
                for c in range(8):
                    P.op("pe", lambda e, c=c: e.transpose(TBb[:, c * 128:(c + 1) * 128], xn[:, sl, c * 128:(c + 1) * 128], idb[:]),
                         reads=[f"xn{sl}", "idb"], writes=[bkey[sl]])
                P.op("dve", lambda e: e.tensor_tensor(out=aT[:, sl], in0=TBb.rearrange("p (c t) -> p c t", c=8),
                                                      in1=bc(gmix, [128, 8, 128]), op=ALU.mult),
                     reads=[bkey[sl], "gvec"], writes=[f"aT{sl}"])

            def bst(it):
                bi, halo, hs, xt, xk, sl = blk(it)
                b = bi - 1
                for c in range(8):
                    P.op("pe", lambda e, c=c: e.matmul(bank[5][:, 0:128], lhsT=w_in_bf[:, c, 1536:1664], rhs=aT[:, sl, c, :],
                                                       start=(c == 0), stop=(c == 7)),
                         reads=[f"aT{sl}", wk[c]], writes=[bkey[5]])
                for c in range(8):
                    P.op("pe", lambda e, c=c: e.matmul(bank[5][:, 128:256], lhsT=aT[:, sl, c, :], rhs=w_in_bf[:, c, 1664:1792],
                                                       start=(c == 0), stop=(c == 7)),
                         reads=[f"aT{sl}", wk[c]], writes=[bkey[5]])
                if not halo:
                    for n, bk in ((0, 2), (1, 3)):
                        for c in range(8):
                            P.op("pe", lambda e, c=c, n=n, bk=bk: e.matmul(bank[bk][:, :], lhsT=aT[:, sl, c, :],
                                                                           rhs=w_in_bf[:, c, n * 512:(n + 1) * 512],
                                                                           start=(c == 0), stop=(c == 7)),
                                 reads=[f"aT{sl}", wk[c]], writes=[bkey[bk]])
                    for qc in range(4):
                        for c in range(8):
                            P.op("pe", lambda e, c=c, qc=qc: e.matmul(bank[4][:, qc * 128:(qc + 1) * 128],
                                                                      lhsT=w_in_bf[:, c, 1024 + qc * 128:1024 + (qc + 1) * 128],
                                                                      rhs=aT[:, sl, c, :], start=(c == 0), stop=(c == 7)),
                                 reads=[f"aT{sl}", wk[c]], writes=[bkey[4]])
                P.op("act", lambda e: e.activation(out=kT[:, bi * 128:(bi + 1) * 128], in_=bank[5][:, 0:128], func=AF.Copy),
                     reads=[bkey[5]], writes=[f"kT{bi}"])
                vsrc = bank[5][:, 128:256].rearrange("p (k d) -> p k d", k=2)
                if halo:
                    P.op("dve", lambda e: e.tensor_scalar(out=vaug[:, bi, :, 0:64], in0=vsrc, scalar1=flags[:, hs:hs + 1], scalar2=None,
                                                          op0=ALU.mult), reads=[bkey[5], "flags"], writes=[f"va{bi}"])
                    P.op("dve", lambda e: e.tensor_copy(out=vaug[:, bi, :, 64:65],
                                                        in_=flags[:, hs:hs + 1].unsqueeze(1).broadcast_to([128, 2, 1])),
                         reads=["flags", "vones"], writes=[f"vo{bi}"])
                    return
                P.op("dve", lambda e: e.tensor_copy(out=vaug[:, bi, :, 0:64], in_=vsrc), reads=[bkey[5]], writes=[f"va{bi}"])
                for n, bk in ((0, 2), (1, 3)):
                    P.op("act", lambda e, n=n, bk=bk: e.activation(out=uv[:, sl, n, :], in_=bank[bk][:, :], func=AF.Gelu_apprx_tanh),
                         reads=[bkey[bk]], writes=[f"uv{sl}{n}"])
                P.op("act", lambda e: e.activation(out=qT[:, :, b * 128:(b + 1) * 128],
                                                   in_=bank[4][:, :].rearrange("p (c t) -> p c t", c=4), func=AF.Copy),
                     reads=[bkey[4]], writes=[f"qT{b}"])

            def cst_ln(it):
                bi, halo, hs, xt, xk, sl = blk(it)
                if halo:
                    return
                b = bi - 1
                vk = f"ssv{b}"
                c6 = SS_V + 2 * b
                P.op("dve", lambda e: e.bn_stats(out=stat[:, 150:156], in_=uv[:, sl, 1, :]), reads=[f"uv{sl}1"], writes=["bn6"])
                P.op("dve", lambda e: e.bn_aggr(out=stat[:, c6:c6 + 2], in_=stat[:, 150:156]), reads=["bn6"], writes=[vk])
                P.op("dve", lambda e: e.tensor_scalar(out=stat[:, c6 + 1:c6 + 2], in0=stat[:, c6 + 1:c6 + 2], scalar1=EPS,
                                                      scalar2=None, op0=ALU.add), reads=[vk], writes=[vk])
                P.op("pool", lambda e: e.tensor_tensor(out=stat[:, c6 + 1:c6 + 2], in0=stat[:, c6 + 1:c6 + 2],
                                                       in1=cst_[:, 0:1], op=ALU.pow), reads=[vk, "cst"], writes=[vk])
                P.op("dve", lambda e: e.tensor_scalar(out=vc[:], in0=uv[:, sl, 1, :], scalar1=stat[:, c6:c6 + 1],
                                                      scalar2=stat[:, c6 + 1:c6 + 2], op0=ALU.subtract, op1=ALU.mult),
                     reads=[f"uv{sl}1", vk], writes=["vc"])
                P.op("dve", lambda e: e.tensor_tensor(out=vc[:], in0=vc[:], in1=lnv[:, 0, :], op=ALU.mult),
                     reads=["vc", "lnv"], writes=["vc"])
                P.op("dve", lambda e: e.tensor_tensor(out=vn[:], in0=vc[:], in1=lnv[:, 1, :], op=ALU.add),
                     reads=["vc", "lnv"], writes=["vn"])

            def cst_sp(it):
                bi, halo, hs, xt, xk, sl = blk(it)
                if halo:
                    return
                b = bi - 1
                for hh in range(4):
                    P.op("pe", lambda e, hh=hh: e.matmul(bank[6][:, hh * 128:(hh + 1) * 128], lhsT=wsT[:, hh * 128:(hh + 1) * 128],
                                                         rhs=vn[:, hh * 128:(hh + 1) * 128], start=True, stop=True),
                         reads=["vn", "wsT"], writes=[bkey[6]])
                for hh in range(4):
                    P.op("dve", lambda e, hh=hh: e.scalar_tensor_tensor(out=ya[:, hh * 128:(hh + 1) * 128],
                                                                        in0=bank[6][:, hh * 128:(hh + 1) * 128],
                                                                        scalar=bsT[:, hh:hh + 1],
                                                                        in1=uv[:, sl, 0, hh * 128:(hh + 1) * 128],
                                                                        op0=ALU.add, op1=ALU.mult),
                         reads=[bkey[6], "bsT", f"uv{sl}0"], writes=["ya"])
                ak = f"ssa{b}"
                P.op("act", lambda e: e.activation(out=junk[:, 0:512], in_=ya[:], func=AF.Square, accum_out=stat[:, SS_A + b:SS_A + b + 1]),
                     reads=["ya"], writes=["junk", ak])
                rstd_from_ss(SS_A + b, 512, ak)
                P.op("act", lambda e: e.activation(out=yna[:, b, :], in_=ya[:], func=AF.Copy, scale=stat[:, SS_A + b:SS_A + b + 1]),
                     reads=["ya", ak], writes=[f"yna{b}"])

            nit = len(order)
            for it in range(nit):
                a0(it)
            if nit > 0:
                a1(0)
                a1x(0)
                a2(0)
            if nit > 1:
                a1(1)
                a1x(1)
            for it in range(nit + 1):
                if it - 1 >= 0:
                    cst_ln(it - 1)
                if it + 2 < nit:
                    a1(it + 2)
                if it + 1 < nit:
                    a2(it + 1)
                if it < nit:
                    bst(it)
                if it + 2 < nit:
                    a1x(it + 2)
                if it - 1 >= 0:
                    cst_sp(it - 1)
                if it >= 1:
                    conv_step(2)

            if stage == 1 and nblk is not None:
                dump(expB[:, 0:512], 512, ["expB"], cast=True)
                P.barrier()
            if stage == 1 and nblk is None:
                dump(yna[:, 0, :], 512, ["yna0"], cast=True)
                dump(yna[:, 15, :], 512, ["yna15"], cast=True)
                dump(qT[:, :, 0:128], 512, ["qT0"], cast=True)
                dump(kT[:, 0:256], 256, ["kT0", "kT1"], cast=True)
                dump(vaug[:, 0:2], 264, ["va0", "va1", "vo0", "vones"], cast=True)
                dump(vaug[:, 17], 132, ["va17", "vo17"], cast=True)
                dump(stat[:, 0:18], 18, [f"ssx{i}" for i in range(18)], cast=False)
                P.barrier()
        if stage == 1:
            P.finish()
            P.emit()
            return nc
        P.barrier()

        with contextlib.ExitStack() as _es:
            w_out_bf = _es.enter_context(nc.sbuf_tensor("s_w_out_bf", [128, 8, D], BF16))
            wr = _es.enter_context(nc.sbuf_tensor("s_wr", [128, 8, 36], BF16))
            brt = _es.enter_context(nc.sbuf_tensor("s_brt", [128, 36], F32))
            E = _es.enter_context(nc.sbuf_tensor("s_E", [128, 2, 3, 512], BF16))
            den = _es.enter_context(nc.sbuf_tensor("s_den", [128, 8], F32))
            yb = _es.enter_context(nc.sbuf_tensor("s_yb", [128, 512], F32))
            ynb = _es.enter_context(nc.sbuf_tensor("s_ynb", [128, NB, 512], BF16))
            yT = _es.enter_context(nc.sbuf_tensor("s_yT", [128, 2, 8, 128], BF16))
            mTf = _es.enter_context(nc.sbuf_tensor("s_mTf", [128, 8, 128], BF16))
            for c in range(8):
                P.dma("pool", lambda e, c=c: e.dma_start(out=w_out_bf[:, c, :], in_=w_out_d[c * 128:(c + 1) * 128, :]),
                      writes=[f"w_out{c}"])
            P.dma("pool", lambda e: e.dma_start(out=wr[:], in_=wr_d.rearrange("(c p) f -> p c f", p=128)), writes=["wr"])
            P.dma("sp", lambda e: e.dma_start(out=brt[:], in_=br_d[0:1, :].broadcast_to([128, 36])), writes=["brt"])
            zrow = _es.enter_context(nc.sbuf_tensor("s_zrow", [128, D], BF16))
            P.op("dve", lambda e: e.memset(zrow[:], 0.0), writes=["zrow"])
            for r0 in range(0, NSLOT, 128 * 5):
                nr = min(128 * 5, NSLOT - r0)
                P.dma("sp", lambda e, r0=r0, nr=nr: e.dma_start(
                    out=mg_d[r0:r0 + nr, :].rearrange("(j p) d -> p j d", p=128),
                    in_=zrow[:].unsqueeze(1).broadcast_to([128, nr // 128, D])), reads=["zrow"], writes=["mg"])
            expB4 = expB[:].rearrange("p (kb hh q) -> p kb hh q", kb=3, hh=8)
            SB = (0, 1, 2)
            OB = (3, 4)
            TYB, OPB, RB = 5, (6, 7), 5

            def k1_s(b, kv):
                bi = b + 1
                pr = slice(kv * 64, (kv + 1) * 64)
                for kb in range(3):
                    kblk = bi - 1 + kb
                    P.op("pe", lambda e, kb=kb, kblk=kblk: e.matmul(bank[SB[kb]][:, :], lhsT=kT[pr, kblk * 128:(kblk + 1) * 128],
                                                                     rhs=qT[pr, :, b * 128:(b + 1) * 128], start=True, stop=True),
                         reads=[f"kT{kblk}", f"qT{b}"], writes=[bkey[SB[kb]]])

            def k1_e(b, kv):
                for kb in range(3):
                    P.op("act", lambda e, kb=kb: e.activation(out=E[:, kv, kb, :], in_=bank[SB[kb]][:, :], func=AF.Exp, scale=0.125),
                         reads=[bkey[SB[kb]]], writes=[f"E{kv}{kb}"])
                for kb in range(3):
                    P.op("dve", lambda e, kb=kb: e.tensor_tensor(
                        out=E[:, kv, kb, :].rearrange("p (g q) -> p g q", g=4), in0=E[:, kv, kb, :].rearrange("p (g q) -> p g q", g=4),
                        in1=expB4[:, kb, kv * 4:(kv + 1) * 4, :], op=ALU.mult),
                         reads=[f"E{kv}{kb}", "expB"], writes=[f"E{kv}{kb}"])

            def k1_pv(b, kv):
                bi = b + 1
                ob = OB[kv]
                for g in range(4):
                    for kb in range(3):
                        kblk = bi - 1 + kb
                        P.op("pe", lambda e, kb=kb, g=g, kblk=kblk: e.matmul(
                            bank[ob][:, g * 65:(g + 1) * 65], lhsT=E[:, kv, kb, g * 128:(g + 1) * 128],
                            rhs=vaug[:, kblk, kv, 0:65], start=(kb == 0), stop=(kb == 2)),
                             reads=[f"E{kv}{kb}", f"va{kblk}", f"vo{kblk}", "vones"], writes=[bkey[ob]])

            def k2a(b):
                for kv in range(2):
                    ob = OB[kv]
                    o3 = bank[ob][:, 0:260].rearrange("p (g d) -> p g d", g=4)
                    P.op("dve", lambda e, kv=kv, o3=o3: e.tensor_tensor(out=den[:, kv * 4:(kv + 1) * 4].unsqueeze(2), in0=o3[:, :, 64:65],
                                                                        in1=esink[:, kv * 4:(kv + 1) * 4].unsqueeze(2), op=ALU.add),
                         reads=[bkey[ob], "esink"], writes=[f"den{kv}"])
                    P.op("dve", lambda e, kv=kv: e.reciprocal(out=den[:, kv * 4:(kv + 1) * 4], in_=den[:, kv * 4:(kv + 1) * 4]),
                         reads=[f"den{kv}"], writes=[f"den{kv}"])
                    P.op("dve", lambda e, kv=kv, o3=o3: e.tensor_tensor(
                        out=yb[:, kv * 256:(kv + 1) * 256].rearrange("p (g d) -> p g d", g=4), in0=o3[:, :, 0:64],
                        in1=bc(den[:, kv * 4:(kv + 1) * 4], [128, 4, 64]), op=ALU.mult),
                         reads=[bkey[ob], f"den{kv}"], writes=[f"yb{kv}"])
                bk_ = f"ssb{b}"
                P.op("act", lambda e: e.activation(out=junk[:, 0:512], in_=yb[:], func=AF.Square, accum_out=stat[:, SS_B + b:SS_B + b + 1]),
                     reads=["yb0", "yb1"], writes=["junk", bk_])
                rstd_from_ss(SS_B + b, 512, bk_)

            def k2y(b):
                P.op("act", lambda e: e.activation(out=ynb[:, b, :], in_=yb[:], func=AF.Copy, scale=stat[:, SS_B + b:SS_B + b + 1]),
                     reads=["yb0", "yb1", f"ssb{b}"], writes=[f"ynb{b}"])

            def k2b_t(b):
                par = b % 2
                tyb = (5, 2)[par]
                opb = ((6, 7), (3, 4))[par]
                T0 = bank[tyb][:].bitcast(BF16)
                for c in range(8):
                    src = yna[:, b, c * 128:(c + 1) * 128] if c < 4 else ynb[:, b, (c - 4) * 128:(c - 3) * 128]
                    P.op("pe", lambda e, c=c, src=src: e.transpose(T0[:, c * 128:(c + 1) * 128], src, idb[:]),
                         reads=[f"yna{b}", f"ynb{b}", "idb"], writes=[bkey[tyb]])
                P.op("dve", lambda e: e.tensor_tensor(out=yT[:, par], in0=T0.rearrange("p (c t) -> p c t", c=8),
                                                      in1=bc(gout, [128, 8, 128]), op=ALU.mult),
                     reads=[bkey[tyb], "gvec"], writes=[f"yT{par}"])
                for n in range(2):
                    for c in range(8):
                        P.op("pe", lambda e, c=c, n=n: e.matmul(bank[opb[n]][:, :], lhsT=yT[:, par, c, :],
                                                                rhs=w_out_bf[:, c, n * 512:(n + 1) * 512], start=(c == 0), stop=(c == 7)),
                             reads=[f"yT{par}", f"w_out{c}"], writes=[bkey[opb[n]]])

            def k2b_h(b):
                opb = ((6, 7), (3, 4))[b % 2]
                for n in range(2):
                    P.op("dve", lambda e, n=n: e.tensor_tensor(out=h[:, b, n * 512:(n + 1) * 512], in0=bank[opb[n]][:, :],
                                                               in1=h[:, b, n * 512:(n + 1) * 512], op=ALU.add),
                         reads=[bkey[opb[n]], f"h{b}"], writes=[f"h{b}"])

            def k3a(b):
                mk = f"ssm{b}"
                P.op("act", lambda e: e.activation(out=junk[:], in_=h[:, b, :], func=AF.Square, accum_out=stat[:, SS_M + b:SS_M + b + 1]),
                     reads=[f"h{b}"], writes=["junk", mk])
                rstd_from_ss(SS_M + b, D, mk)

            def k3b(b):
                P.op("act", lambda e: e.activation(out=hnb[:, b, :], in_=h[:, b, :], func=AF.Copy, scale=stat[:, SS_M + b:SS_M + b + 1]),
                     reads=[f"h{b}", f"ssm{b}"], writes=[f"hnb{b}"])
                tb = 0
                TBm = bank[tb][:].bitcast(BF16)
                for c in range(8):
                    P.op("pe", lambda e, c=c: e.transpose(TBm[:, c * 128:(c + 1) * 128], hnb[:, b, c * 128:(c + 1) * 128], idb[:]),
                         reads=[f"hnb{b}", "idb"], writes=[bkey[tb]])
                P.op("dve", lambda e: e.tensor_tensor(out=mTf[:], in0=TBm.rearrange("p (c t) -> p c t", c=8),
                                                      in1=bc(gffn, [128, 8, 128]), op=ALU.mult),
                     reads=[bkey[tb], "gvec"], writes=["mTf"])
                for c in range(8):
                    P.op("pe", lambda e, c=c: e.matmul(bank[1][:, 0:36], lhsT=mTf[:, c, :], rhs=wr[:, c, :], start=(c == 0), stop=(c == 7)),
                         reads=["mTf", "wr"], writes=[bkey[1]])
                P.op("dve", lambda e: e.tensor_tensor(out=L[:, b, :], in0=bank[1][:, 0:36], in1=brt[:], op=ALU.add),
                     reads=[bkey[1], "brt"], writes=["L"])

            for i in range(-1, NB):
                a, m_ = i + 1, i
                va, vm = 0 <= a < NB, 0 <= m_ < NB
                if va:
                    k1_s(a, 0)
                    k1_e(a, 0)
                if vm:
                    k2a(m_)
                if va:
                    k1_s(a, 1)
                    k1_e(a, 1)
                    k1_pv(a, 0)
                    k1_pv(a, 1)
                if vm:
                    k2y(m_)
                conv_step(3)
            for j in range(NB + 1):
                o_, z = j, j - 1
                vo, vz = 0 <= o_ < NB, 0 <= z < NB
                if vo:
                    k2b_t(o_)
                if vz:
                    k3a(z)
                if vo:
                    k2b_h(o_)
                if vz:
                    k3b(z)
                conv_step(3)
            conv_step(len(conv_todo))
            if stage == 2:
                dump(h[:, 0, :], 1024, ["h0"])
                dump(h[:, 15, :], 1024, ["h15"])
                dump(L[:].rearrange("p b f -> p (b f)"), 576, ["L"])
                P.barrier()
    if stage == 2:
        P.finish()
        P.emit()
        return nc
    P.barrier()

    e256 = cvec[:, 0:32]
    thr = cvec[:, 32:46]
    uvec = cvec[:, 46:77]
    pcol = cvec[:, 77:78]
    onesr = cvec[:, 78:110]
    with contextlib.ExitStack() as _es:
        r_a = _es.enter_context(nc.sbuf_tensor("s_r_a", [128, NB, 4], F32))
        r_b = _es.enter_context(nc.sbuf_tensor("s_r_b", [128, NB, 4], F32))
        r_s = _es.enter_context(nc.sbuf_tensor("s_r_s", [128, 8, NB], F32))
        lem = _es.enter_context(nc.sbuf_tensor("s_lem", [128, NB, NE], F32))
        Mb = _es.enter_context(nc.sbuf_tensor("s_Mb", [128, NB, NE], BF16))
        oh1 = _es.enter_context(nc.sbuf_tensor("s_oh1", [128, NB, NE], F32))
        oh2 = _es.enter_context(nc.sbuf_tensor("s_oh2", [128, NB, NE], F32))
        tot = _es.enter_context(nc.sbuf_tensor("s_tot", [128, NB, NE], F32))
        rk = _es.enter_context(nc.sbuf_tensor("s_rk", [128, NB, NE], F32))
        acc = _es.enter_context(nc.sbuf_tensor("s_acc", [128, 8, NE], F32))
        cmp = _es.enter_context(nc.sbuf_tensor("s_cmp", [128, NE * 31], F32))
        posf = _es.enter_context(nc.sbuf_tensor("s_posf", [128, 2, NB], F32))
        lg = L[:, :, 0:4]
        le = L[:, :, 4:36]
        gmax, sg, m1, m2, rr = (r_s[:, i, :] for i in range(5))
        w1 = w12[:, 0, :]
        w2 = w12[:, 1, :]
        P.op("dve", lambda e: e.tensor_reduce(out=gmax, in_=lg, axis=AX.X, op=ALU.max), reads=["L"], writes=["gmax"])
        P.op("dve", lambda e: e.tensor_tensor(out=r_a[:], in0=lg, in1=bc(gmax, [128, NB, 4]), op=ALU.is_equal),
             reads=["L", "gmax"], writes=["gm"])
        P.op("dve", lambda e: e.tensor_tensor(out=r_b[:], in0=lg, in1=bc(gmax, [128, NB, 4]), op=ALU.subtract),
             reads=["L", "gmax"], writes=["r_b"])
        P.op("act", lambda e: e.activation(out=r_b[:], in_=r_b[:], func=AF.Exp), reads=["r_b"], writes=["r_b"])
        P.op("dve", lambda e: e.tensor_reduce(out=sg, in_=r_b[:], axis=AX.X, op=ALU.add), reads=["r_b"], writes=["sg"])
        P.op("dve", lambda e: e.tensor_scalar(out=r_a[:], in0=r_a[:], scalar1=1.0, scalar2=BIG, op0=ALU.subtract, op1=ALU.mult),
             reads=["gm"], writes=["gm"])
        P.op("dve", lambda e: e.tensor_tensor(out=lem[:].rearrange("p b (g x) -> p b g x", g=4),
                                              in0=le.rearrange("p b (g x) -> p b g x", g=4),
                                              in1=bc(r_a[:], [128, NB, 4, 8]), op=ALU.add), reads=["L", "gm"], writes=["lem"])
        P.op("dve", lambda e: e.tensor_reduce(out=m1, in_=lem[:], axis=AX.X, op=ALU.max), reads=["lem"], writes=["m1"])
        P.op("dve", lambda e: e.tensor_tensor(out=oh1[:], in0=lem[:], in1=bc(m1, [128, NB, NE]), op=ALU.is_equal),
             reads=["lem", "m1"], writes=["oh1"])
        P.op("dve", lambda e: e.scalar_tensor_tensor(out=lem[:], in0=oh1[:], scalar=-BIG, in1=lem[:], op0=ALU.mult, op1=ALU.add),
             reads=["oh1", "lem"], writes=["lem"])
        P.op("dve", lambda e: e.tensor_reduce(out=m2, in_=lem[:], axis=AX.X, op=ALU.max), reads=["lem"], writes=["m2"])
        P.op("dve", lambda e: e.tensor_tensor(out=oh2[:], in0=lem[:], in1=bc(m2, [128, NB, NE]), op=ALU.is_equal),
             reads=["lem", "m2"], writes=["oh2"])
        P.op("dve", lambda e: e.tensor_tensor(out=rr, in0=m2, in1=m1, op=ALU.subtract), reads=["m1", "m2"], writes=["rr"])
        P.op("act", lambda e: e.activation(out=rr, in_=rr, func=AF.Exp), reads=["rr"], writes=["rr"])
        P.op("dve", lambda e: e.scalar_tensor_tensor(out=w1, in0=rr, scalar=1.0, in1=sg, op0=ALU.add, op1=ALU.mult),
             reads=["rr", "sg"], writes=["w1"])
        P.op("dve", lambda e: e.reciprocal(out=w1, in_=w1), reads=["w1"], writes=["w1"])
        P.op("dve", lambda e: e.tensor_tensor(out=w2, in0=rr, in1=w1, op=ALU.mult), reads=["rr", "w1"], writes=["w2"])
        P.op("dve", lambda e: e.tensor_tensor(out=Mb[:], in0=oh1[:], in1=oh2[:], op=ALU.add), reads=["oh1", "oh2"], writes=["Mb"])
        Mflat = Mb[:].rearrange("p b f -> p (b f)")
        P.op("pe", lambda e: e.matmul(bank[0][:, :], lhsT=trib[:, 0:128], rhs=Mflat, start=True, stop=True),
             reads=["Mb", "trib"], writes=[bkey[0]])
        P.op("pe", lambda e: e.matmul(bank[1][:, :], lhsT=trib[:, 128:256], rhs=Mflat, start=True, stop=True),
             reads=["Mb", "trib"], writes=[bkey[1]])
        totf = tot[:].rearrange("p b f -> p (b f)")
        tot_eb = totf.rearrange("p (f b) -> p f b", b=NB)
        P.op("dve", lambda e: e.tensor_copy(out=tot_eb, in_=bank[1][:, :].rearrange("p (b f) -> p f b", b=NB)),
             reads=[bkey[1]], writes=["tot"])
        Sf = lem[:].rearrange("p b f -> p (b f)")
        S_eb = Sf.rearrange("p (f b) -> p f b", b=NB)
        onesf = cmp[:, 0:NB * NE]
        P.op("dve", lambda e: e.memset(onesf, 1.0), writes=["cmp"])
        P.op("dve", lambda e: e.tensor_tensor_scan(out=Sf, data0=onesf, data1=totf, initial=0.0, op0=ALU.mult, op1=ALU.add),
             reads=["cmp", "tot", "lem"], writes=["lem"])
        cnt = acc[:, 0, :]
        ckey = "cnt"
        P.op("dve", lambda e: e.tensor_reduce(out=cnt, in_=tot_eb, axis=AX.X, op=ALU.add), reads=["tot"], writes=["cnt"])
        base = acc[:, 1, :]
        P.op("dve", lambda e: e.tensor_tensor(out=base, in0=S_eb[:, :, NB - 1], in1=cnt, op=ALU.subtract), reads=["lem", "cnt"], writes=["base"])
        P.op("dve", lambda e: e.tensor_tensor(out=Sf, in0=Sf, in1=totf, op=ALU.subtract), reads=["lem", "tot"], writes=["lem"])
        P.op("dve", lambda e: e.tensor_tensor(out=S_eb, in0=S_eb, in1=bc(base, [128, NE, NB]), op=ALU.subtract), reads=["lem", "base"], writes=["lem"])
        P.op("dve", lambda e: e.tensor_tensor(out=rk[:].rearrange("p b f -> p f b"), in0=bank[0][:, :].rearrange("p (b f) -> p f b", b=NB),
                                              in1=S_eb, op=ALU.add), reads=[bkey[0], "lem"], writes=[f"rk{b}" for b in range(NB)])
        ont, ote, ots, dlt = (acc[:, i, :] for i in range(2, 6))
        cmp3 = cmp[:, 0:NE * 14].rearrange("p (f k) -> p f k", k=14)
        P.op("dve", lambda e: e.tensor_tensor(out=cmp3, in0=bc(cnt, [128, NE, 14]), in1=thr.unsqueeze(1).broadcast_to([128, NE, 14]),
                                              op=ALU.is_gt), reads=[ckey, "cvec"], writes=["cmp"])
        P.op("dve", lambda e: e.tensor_reduce(out=ont, in_=cmp3, axis=AX.X, op=ALU.add), reads=["cmp"], writes=["ont"])
        P.op("dve", lambda e: e.tensor_tensor_scan(out=ote, data0=onesr, data1=ont, initial=0.0, op0=ALU.mult, op1=ALU.add),
             reads=["ont", "cvec"], writes=["ote"])
        P.op("dve", lambda e: e.tensor_tensor(out=ots, in0=ote, in1=ont, op=ALU.subtract), reads=["ote", "ont"], writes=["ots"])
        P.op("dve", lambda e: e.tensor_scalar(out=dlt, in0=ots, scalar1=128.0, scalar2=float(OVB - CAP), op0=ALU.mult, op1=ALU.add),
             reads=["ots"], writes=["dlt"])
        P.op("dve", lambda e: e.tensor_tensor(out=dlt, in0=dlt, in1=e256, op=ALU.subtract), reads=["dlt", "cvec"], writes=["dlt"])
        rkeys = [f"rk{b}" for b in range(NB)]
        P.op("dve", lambda e: e.tensor_scalar(out=lem[:], in0=rk[:], scalar1=float(CAP), scalar2=None, op0=ALU.is_ge),
             reads=rkeys, writes=["lem"])
        P.op("dve", lambda e: e.tensor_tensor(out=lem[:], in0=lem[:], in1=dlt.unsqueeze(1).broadcast_to([128, NB, NE]), op=ALU.mult),
             reads=["lem", "dlt"], writes=["lem"])
        P.op("dve", lambda e: e.tensor_tensor(out=rk[:], in0=rk[:], in1=e256.unsqueeze(1).broadcast_to([128, NB, NE]), op=ALU.add),
             reads=rkeys + ["cvec"], writes=["pos"])
        P.op("dve", lambda e: e.tensor_tensor(out=rk[:], in0=rk[:], in1=lem[:], op=ALU.add), reads=["pos", "lem"], writes=["pos"])
        for k, oh in ((0, oh1), (1, oh2)):
            P.op("dve", lambda e, oh=oh: e.tensor_tensor(out=lem[:], in0=rk[:], in1=oh[:], op=ALU.mult),
                 reads=["pos", f"oh{k + 1}", "lem"], writes=["lem"])
            P.op("dve", lambda e, k=k: e.tensor_reduce(out=posf[:, k, :], in_=lem[:], axis=AX.X, op=ALU.add),
                 reads=["lem"], writes=[f"posf{k}"])
        P.op("dve", lambda e: e.tensor_copy(out=idx[:], in_=posf[:]), reads=["posf0", "posf1"], writes=["idx"])
        eov = acc[:, 6, 0:NOV]
        cmpu = cmp[:, 0:NOV * NE].rearrange("p (u f) -> p u f", f=NE)
        P.op("dve", lambda e: e.tensor_tensor(out=cmpu, in0=ote.unsqueeze(1).broadcast_to([128, NOV, NE]), in1=bc(uvec, [128, NOV, NE]),
                                              op=ALU.is_le), reads=["ote", "cvec", "cmp"], writes=["cmp"])
        P.op("dve", lambda e: e.tensor_reduce(out=eov, in_=cmpu, axis=AX.X, op=ALU.add), reads=["cmp"], writes=["eov"])
        gf = acc[:, 7, 0:NOV]
        P.op("dve", lambda e: e.tensor_scalar(out=gf, in0=eov, scalar1=128.0, scalar2=None, op0=ALU.mult),
             reads=["eov"], writes=["gf"])
        P.op("dve", lambda e: e.tensor_scalar(out=gf, in0=gf, scalar1=pcol, scalar2=None, op0=ALU.add),
             reads=["gf", "cvec"], writes=["gf"])
        P.op("dve", lambda e: e.tensor_copy(out=gidx[:], in_=gf), reads=["gf"], writes=["gidx"])
        if stage == 3:
            dump(idx[:].rearrange("p k b -> p (k b)").bitcast(F32), 32, ["idx"])
            dump(w12[:].rearrange("p k b -> p (k b)"), 32, ["w1", "w2"])
            dump(acc[:].rearrange("p k f -> p (k f)"), 256, ["eov", "ote", "ots", "dlt", ckey, "ont"])
        P.barrier()
    if stage == 3:
        P.finish()
        P.emit()
        return nc

    NW = 4
    with contextlib.ExitStack() as _es:
        wall = _es.enter_context(nc.sbuf_tensor("s_wall", [128, NW, 3, 2048], BF16))
        xg = _es.enter_context(nc.sbuf_tensor("s_xg", [128, 3, 2, D], BF16))
        xgT = _es.enter_context(nc.sbuf_tensor("s_xgT", [128, 2, 8, CAP], BF16))
        sgt = _es.enter_context(nc.sbuf_tensor("s_sgt", [128, 2, CAP], BF16))
        hdT = _es.enter_context(nc.sbuf_tensor("s_hdT", [128, 2, 2, CAP], BF16))
        ogt = _es.enter_context(nc.sbuf_tensor("s_ogt", [128, 2, 2, D], BF16))
        n_ov = NOV if os.environ.get("K_NOV") is None else int(os.environ["K_NOV"])
        _regs = {}

        def breg(e, val):
            if val not in _regs:
                _regs[val] = e.to_reg(val)
            return _regs[val]

        tiles = [(ex * CAP, 2, ex, None) for ex in range(NE)] + [(OVB + u * 128, 1, None, u) for u in range(n_ov)]
        NT = len(tiles)
        mgkeys = [f"mgs{b}{k}" for b in range(NB) for k in range(2)]
        okeys = [f"og{i}" for i in range(NT)]

        def st_weights(i):
            row0, nj, ex, u = tiles[i]
            ws = i % NW
            dst = wall[:, ws].rearrange("p k f -> p (k f)")
            if ex is not None:
                P.dma("pool", lambda e: e.dma_start(out=dst, in_=wbf_d[ex * 128:(ex + 1) * 128, :]), writes=[f"w{ws}"])
            else:
                P.dma("pool", lambda e: e.indirect_dma_start(
                    out=dst, out_offset=None, in_=wbf_d[:, :], in_offset=bass.IndirectOffsetOnAxis(ap=gidx[:, u:u + 1], axis=0),
                    bounds_check=breg(e, NE * 128 - 1), oob_is_err=False), reads=["gidx"], writes=[f"w{ws}"])

        def st_load(i):
            row0, nj, ex, u = tiles[i]
            s3 = i % 3
            P.dma("sp", lambda e: e.dma_start(out=xg[:, s3, 0:nj, :],
                                              in_=mg_d[row0:row0 + nj * 128, :].rearrange("(j p) d -> p j d", p=128)),
                  reads=mgkeys, writes=[f"xg{s3}"])

        def st_T(i):
            row0, nj, ex, u = tiles[i]
            s3, sl = i % 3, i % 2
            for j in range(nj):
                TB = bank[j][:].bitcast(BF16)
                for c in range(8):
                    P.op("pe", lambda e, j=j, c=c, TB=TB: e.transpose(TB[:, c * 128:(c + 1) * 128],
                                                                      xg[:, s3, j, c * 128:(c + 1) * 128], idb[:]),
                         reads=[f"xg{s3}", "idb"], writes=[bkey[j]])
                P.op("dve", lambda e, j=j, TB=TB: e.tensor_tensor(out=xgT[:, sl, :, j * 128:(j + 1) * 128],
                                                                  in0=TB.rearrange("p (c t) -> p c t", c=8),
                                                                  in1=bc(gffn, [128, 8, 128]), op=ALU.mult),
                     reads=[bkey[j], "gvec"], writes=[f"xgT{sl}"])

        def st_GU(i):
            row0, nj, ex, u = tiles[i]
            sl, ws, ns = i % 2, i % NW, nj * 128
            for fc in range(2):
                gb = bank[2 + fc]
                for half in range(2):
                    for c in range(8):
                        P.op("pe", lambda e, c=c, gb=gb, fc=fc, half=half: e.matmul(
                            gb[:, half * CAP:half * CAP + ns], lhsT=wall[:, ws, half, c * 256 + fc * 128:c * 256 + (fc + 1) * 128],
                            rhs=xgT[:, sl, c, 0:ns], start=(c == 0), stop=(c == 7)),
                             reads=[f"w{ws}", f"xgT{sl}"], writes=[bkey[2 + fc]])
                P.op("act", lambda e, fc=fc, gb=gb: e.activation(out=sgt[:, fc, 0:ns], in_=gb[:, 0:ns], func=AF.Silu),
                     reads=[bkey[2 + fc]], writes=[f"sgt{fc}"])
                P.op("dve", lambda e, fc=fc, gb=gb: e.tensor_tensor(out=hdT[:, sl, fc, 0:ns], in0=gb[:, CAP:CAP + ns],
                                                                    in1=sgt[:, fc, 0:ns], op=ALU.mult),
                     reads=[bkey[2 + fc], f"sgt{fc}"], writes=[f"hdT{sl}{fc}"])

        def st_D(i):
            row0, nj, ex, u = tiles[i]
            sl, ws, ns = i % 2, i % NW, nj * 128
            for j in range(nj):
                for n in range(2):
                    ob = 4 + j * 2 + n
                    for fc in range(2):
                        P.op("pe", lambda e, fc=fc, ob=ob, j=j, n=n: e.matmul(
                            bank[ob][:, :], lhsT=hdT[:, sl, fc, j * 128:(j + 1) * 128],
                            rhs=wall[:, ws, 2, fc * D + n * 512:fc * D + (n + 1) * 512], start=(fc == 0), stop=(fc == 1)),
                             reads=[f"hdT{sl}{fc}", f"w{ws}"], writes=[bkey[ob]])
                    if n == 0:
                        P.op("act", lambda e, ob=ob, j=j, n=n: e.activation(out=ogt[:, sl, j, n * 512:(n + 1) * 512], in_=bank[ob][:, :],
                                                                           func=AF.Copy), reads=[bkey[ob]], writes=[f"ogt{sl}{j}{n}"])
                    else:
                        P.op("dve", lambda e, ob=ob, j=j, n=n: e.tensor_copy(out=ogt[:, sl, j, n * 512:(n + 1) * 512], in_=bank[ob][:, :]),
                             reads=[bkey[ob]], writes=[f"ogt{sl}{j}{n}"])
            P.dma("sp", lambda e: e.dma_start(out=og_d[row0:row0 + ns, :].rearrange("(j p) d -> p j d", p=128), in_=ogt[:, sl, 0:nj, :]),
                  reads=[f"ogt{sl}{j}{n}" for j in range(nj) for n in range(2)], writes=[okeys[i]])

        for i in range(min(3, NT)):
            st_weights(i)
        for b in range(NB):
            for k in range(2):
                P.dma("pool", lambda e, b=b, k=k: e.indirect_dma_start(
                    out=mg_d[:, :], out_offset=bass.IndirectOffsetOnAxis(ap=idx[:, k, b:b + 1], axis=0),
                    in_=hnb[:, b, :], in_offset=None), reads=["idx"], writes=[f"mgs{b}{k}"])
        st_load(0)
        st_load(1)
        st_T(0)
        for i in range(NT + 1):
            if i + 2 < NT:
                st_load(i + 2)
            if i + 1 < NT:
                st_T(i + 1)
            if i - 1 >= 0:
                st_D(i - 1)
            if i + 3 < NT:
                st_weights(i + 3)
            if i < NT:
                st_GU(i)
        P.barrier()

    with contextlib.ExitStack() as _es:
        wpg = _es.enter_context(nc.sbuf_tensor("s_wpg", [128, 8, D], BF16))
        wpp = _es.enter_context(nc.sbuf_tensor("s_wpp", [128, 2, D], BF16))
        rows = _es.enter_context(nc.sbuf_tensor("s_rows", [128, 3, D], F32))
        pb = _es.enter_context(nc.sbuf_tensor("s_pb", [128, 2, 256], BF16))
        pT = _es.enter_context(nc.sbuf_tensor("s_pT", [128, 2, 128], BF16))
        pp = _es.enter_context(nc.sbuf_tensor("s_pp", [128, 2, D], F32))
        hb = _es.enter_context(nc.sbuf_tensor("s_hb", [128, D], BF16))
        hT = _es.enter_context(nc.sbuf_tensor("s_hT", [128, 2, 8, 128], BF16))
        gt = _es.enter_context(nc.sbuf_tensor("s_gt", [128, D], F32))
        ot = _es.enter_context(nc.sbuf_tensor("s_ot", [128, 2, D], F32))
        ogc = _es.enter_context(nc.sbuf_tensor("s_ogc", [128, 2, 2, D], BF16))
        P.dma("pool", lambda e: e.dma_start(out=wpp[:], in_=wpp_d.rearrange("(c p) f -> p c f", p=128)), writes=["wpp"])
        P.dma("pool", lambda e: e.dma_start(out=wpg[:], in_=wpg_d.rearrange("(c p) f -> p c f", p=128)), writes=["wpg"])
        for i in range(3):
            P.dma("sp", lambda e, i=i: e.dma_start(out=rows[:, i, :], in_=rows_d[i:i + 1, :].broadcast_to([128, D])),
                  writes=["rows"])

        def s0a(b):
            sl = b % 2
            P.dma("pool", lambda e: e.dma_start(out=pb[:, sl, :], in_=pin[b * 128:(b + 1) * 128, :]), writes=[f"pb{sl}"])
            for k in range(2):
                P.dma("pool", lambda e, k=k: e.indirect_dma_start(
                    out=ogc[:, sl, k, :], out_offset=None, in_=og_d[:, :],
                    in_offset=bass.IndirectOffsetOnAxis(ap=idx[:, k, b:b + 1], axis=0)), writes=[f"ogc{sl}{k}"])

        def s0b(b):
            sl = b % 2
            for k in range(2):
                P.op("dve", lambda e, k=k: e.scalar_tensor_tensor(
                    out=h[:, b, :], in0=ogc[:, sl, k, :], scalar=w12[:, k, b:b + 1], in1=h[:, b, :], op0=ALU.mult, op1=ALU.add),
                     reads=[f"ogc{sl}{k}", f"h{b}"], writes=[f"h{b}"])

        def s1(b):
            sl = b % 2
            T0 = bank[0][:].bitcast(BF16)
            for k in range(2):
                P.op("pe", lambda e, k=k: e.transpose(T0[:, k * 128:(k + 1) * 128], pb[:, sl, k * 128:(k + 1) * 128], idb[:]),
                     reads=[f"pb{sl}", "idb"], writes=[bkey[0]])
            P.op("act", lambda e: e.activation(out=pT[:], in_=T0[:, 0:256].rearrange("p (k t) -> p k t", k=2), func=AF.Copy),
                 reads=[bkey[0]], writes=["pT"])
            P.op("act", lambda e: e.activation(out=hb[:], in_=h[:, b, :], func=AF.Copy), reads=[f"h{b}"], writes=["hb"])
            for n in range(2):
                for k in range(2):
                    P.op("pe", lambda e, k=k, n=n: e.matmul(bank[1 + n][:, :], lhsT=pT[:, k, :], rhs=wpp[:, k, n * 512:(n + 1) * 512],
                                                            start=(k == 0), stop=(k == 1)),
                         reads=["pT", "wpp"], writes=[bkey[1 + n]])
            T3 = bank[3][:].bitcast(BF16)
            for c in range(8):
                P.op("pe", lambda e, c=c: e.transpose(T3[:, c * 128:(c + 1) * 128], hb[:, c * 128:(c + 1) * 128], idb[:]),
                     reads=["hb", "idb"], writes=[bkey[3]])
            for n in range(2):
                P.op("act", lambda e, n=n: e.activation(out=pp[:, sl, n * 512:(n + 1) * 512], in_=bank[1 + n][:, :], func=AF.Copy),
                     reads=[bkey[1 + n]], writes=[f"pp{sl}"])
            P.op("act", lambda e: e.activation(out=hT[:, sl], in_=T3.rearrange("p (c t) -> p c t", c=8), func=AF.Copy),
                 reads=[bkey[3]], writes=[f"hT{sl}"])

        def s1b(b):
            sl = b % 2
            pk = f"ssp{b}"
            P.op("act", lambda e: e.activation(out=junk[:], in_=pp[:, sl, :], func=AF.Square, accum_out=stat[:, SS_P + b:SS_P + b + 1]),
                 reads=[f"pp{sl}"], writes=["junk", pk])
            rstd_from_ss(SS_P + b, D, pk)

        def s2a(b):
            sl = b % 2
            pk = f"ssp{b}"
            for n in range(2):
                for c in range(8):
                    P.op("pe", lambda e, c=c, n=n: e.matmul(bank[4 + n][:, :], lhsT=hT[:, sl, c, :], rhs=wpg[:, c, n * 512:(n + 1) * 512],
                                                            start=(c == 0), stop=(c == 7)),
                         reads=[f"hT{sl}", "wpg"], writes=[bkey[4 + n]])
            for n in range(2):
                P.op("dve", lambda e, n=n: e.tensor_tensor(out=gt[:, n * 512:(n + 1) * 512], in0=bank[4 + n][:, :],
                                                           in1=rows[:, 0, n * 512:(n + 1) * 512], op=ALU.add),
                     reads=[bkey[4 + n], "rows"], writes=["gt"])
            P.op("act", lambda e: e.activation(out=gt[:], in_=gt[:], func=AF.Sigmoid), reads=["gt"], writes=["gt"])
            P.op("dve", lambda e: e.scalar_tensor_tensor(out=pp[:, sl, :], in0=pp[:, sl, :], scalar=stat[:, SS_P + b:SS_P + b + 1],
                                                         in1=rows[:, 1, :], op0=ALU.mult, op1=ALU.mult),
                 reads=[f"pp{sl}", pk, "rows"], writes=[f"pp{sl}"])
            P.op("dve", lambda e: e.tensor_tensor(out=pp[:, sl, :], in0=pp[:, sl, :], in1=gt[:], op=ALU.mult),
                 reads=[f"pp{sl}", "gt"], writes=[f"pp{sl}"])
            P.op("pool", lambda e: e.tensor_tensor(out=h[:, b, :], in0=h[:, b, :], in1=pp[:, sl, :], op=ALU.add),
                 reads=[f"pp{sl}", f"h{b}"], writes=[f"h{b}"])

        def s2b(b):
            fk = f"ssf{b}"
            P.op("act", lambda e: e.activation(out=junk[:], in_=h[:, b, :], func=AF.Square, accum_out=stat[:, SS_F + b:SS_F + b + 1]),
                 reads=[f"h{b}"], writes=["junk", fk])
            rstd_from_ss(SS_F + b, D, fk)

        def s2c(b):
            sl = b % 2
            fk = f"ssf{b}"
            P.op("dve", lambda e: e.scalar_tensor_tensor(out=ot[:, sl, :], in0=h[:, b, :], scalar=stat[:, SS_F + b:SS_F + b + 1],
                                                         in1=rows[:, 2, :], op0=ALU.mult, op1=ALU.mult),
                 reads=[f"h{b}", fk, "rows"], writes=[f"ot{sl}"])
            P.dma("sp", lambda e: e.dma_start(out=out_d[b * 128:(b + 1) * 128, :], in_=ot[:, sl, :]),
                  reads=[f"ot{sl}"], is_out=True)

        s0a(0)
        s0a(1)
        s0b(0)
        s0b(1)
        s1(0)
        s0a(2)
        s1b(0)
        for i in range(NB + 2):
            if i + 2 < NB:
                s0b(i + 2)
            if i + 1 < NB:
                s1(i + 1)
            if i + 3 < NB:
                s0a(i + 3)
            if i < NB:
                s2a(i)
            if 0 <= i - 2 < NB:
                s2c(i - 2)
            if 0 <= i - 1 < NB:
                s2b(i - 1)
            if i + 1 < NB:
                s1b(i + 1)
    P.finish()
    P.emit()
    return nc


def _t5_bucket(rel):
    n = 16
    max_exact = 8
    ret = np.where(rel > 0, n, 0)
    a = np.abs(rel)
    af = np.maximum(a, 1).astype(np.float32)
    large = max_exact + (np.log(af / np.float32(max_exact)) / np.float32(np.log(128 / 8)) * np.float32(n - max_exact)).astype(np.int32)
    large = np.minimum(large, n - 1)
    return ret + np.where(a < max_exact, a, large)


def _static_tables():
    j = np.arange(128)[:, None, None]
    kb = np.arange(3)[None, :, None]
    q = np.arange(128)[None, None, :]
    rel = (kb - 1) * 128 + j - q
    bucket = _t5_bucket(rel)
    valid = (np.abs(rel) <= 128).astype(np.float32)
    return bucket, valid


def _w_in_layout(w):
    w = w.copy()
    w[:, 1024:1536] = w[:, 1024:1536].reshape(D, 2, 4, 64).transpose(0, 2, 1, 3).reshape(D, 512)
    return np.ascontiguousarray(w)


def _expert_layout(wg, wu, wd):
    out = np.empty((NE, 128, 3, 2048), np.float32)
    out[:, :, 0, :] = wg.reshape(NE, 8, 128, 256).transpose(0, 2, 1, 3).reshape(NE, 128, 2048)
    out[:, :, 1, :] = wu.reshape(NE, 8, 128, 256).transpose(0, 2, 1, 3).reshape(NE, 128, 2048)
    out[:, :, 2, :] = wd.reshape(NE, 2, 128, D).transpose(0, 2, 1, 3).reshape(NE, 128, 2048)
    return out.reshape(NE * 128, 3, 2048)


def _tri_const():
    t = np.zeros((128, 256), np.float32)
    t[:, 0:128] = (np.arange(128)[:, None] < np.arange(128)[None, :]).astype(np.float32)
    t[:, 128:256] = 1.0
    return t


def _cvec_const():
    c = np.zeros((128, 128), np.float32)
    c[:, 0:32] = np.arange(32)[None, :] * CAP
    c[:, 32:46] = CAP + 128 * np.arange(14)[None, :]
    c[:, 46:77] = np.arange(31)[None, :]
    c[:, 77] = np.arange(128)
    c[:, 78:110] = 1.0
    return c


def make_in_maps(inp):
    f = lambda a: np.ascontiguousarray(np.asarray(a, dtype=np.float32))
    x = f(inp["x"])
    p = f(inp["p"])[0]
    bucket, valid = _static_tables()
    rel_bias = f(inp["rel_bias"])
    bg = rel_bias[bucket]
    biasg = np.ascontiguousarray(bg.transpose(0, 1, 3, 2)).reshape(128, 3072)
    maskc = np.ascontiguousarray(np.broadcast_to(valid[:, :, None, :], (128, 3, 8, 128))).reshape(128, 3072)
    colmajor = lambda v: np.ascontiguousarray(f(v).reshape(8, 128).T)
    gvec = np.concatenate([colmajor(inp["g_mix"][0]), colmajor(inp["g_out_grp"][0]), colmajor(inp["g_ffn"][0])], axis=1)
    shared = {
        "biasg": biasg, "maskc": maskc, "ident": np.eye(128, dtype=np.float32),
        "w_in": _w_in_layout(f(inp["w_in"][0])), "gvec": np.ascontiguousarray(gvec),
        "lnv": np.ascontiguousarray(np.stack([f(inp["ln_v_g"][0]), f(inp["ln_v_b"][0])])),
        "wsT": np.ascontiguousarray(f(inp["w_spatial"][0]).transpose(2, 0, 1)).reshape(128, 512),
        "bsT": np.ascontiguousarray(f(inp["b_spatial"][0]).T),
        "sink": f(inp["sink"]), "w_out": f(inp["w_out"][0]),
        "wr": np.ascontiguousarray(np.concatenate([f(inp["w_router_group"][0]), f(inp["w_router_expert"][0])], axis=1)),
        "br": np.ascontiguousarray(np.concatenate([f(inp["b_router_group"][0]), f(inp["b_router_expert"][0])])[None, :]),
        "w_exp": _expert_layout(f(inp["w_gate_e"][0]), f(inp["w_up_e"][0]), f(inp["w_down_e"][0])),
        "w_ple_proj": f(inp["w_ple_proj"][0]), "w_ple_gate": f(inp["w_ple_gate"][0]),
        "rows": np.ascontiguousarray(np.stack([f(inp["b_ple_gate"][0]), f(inp["g_ple"][0]), f(inp["g_final"])])),
        "tri": _tri_const(), "cvec": _cvec_const(),
    }
    maps = []
    cps = NCORES // BATCH
    for core in range(NCORES):
        bidx, ci = divmod(core, cps)
        t0 = ci * TPC
        xh = np.zeros((NBH * 128, D), np.float32)
        lo, hi = t0 - 128, t0 + TPC + 128
        slo, shi = max(lo, 0), min(hi, SEQ)
        xh[slo - lo:shi - lo] = x[bidx, slo:shi]
        flags = np.zeros((128, 2), np.float32)
        flags[:, 0] = 1.0 if lo >= 0 else 0.0
        flags[:, 1] = 1.0 if hi <= SEQ else 0.0
        m = dict(shared)
        m["xh"] = xh
        m["p"] = np.ascontiguousarray(p[bidx, t0:t0 + TPC])
        m["flags"] = flags
        maps.append(m)
    return maps


_NC_CACHE = {}


def kernel(**inputs):
    if "nc" not in _NC_CACHE:
        _NC_CACHE["nc"] = build_program()
    nc = _NC_CACHE["nc"]
    maps = make_in_maps(inputs)
    res = run_bass_kernel_spmd(nc, maps, core_ids=list(range(NCORES)))
    cps = NCORES // BATCH
    out = np.empty((BATCH, SEQ, D), np.float32)
    for core in range(NCORES):
        bidx, ci = divmod(core, cps)
        out[bidx, ci * TPC:(ci + 1) * TPC] = res.results[core]["out"]
    return out
```

```python
import contextlib
import os
import numpy as np
import concourse.bass as bass
import concourse.mybir as mybir
from concourse.bass_utils import run_bass_kernel_spmd

F32 = mybir.dt.float32
BF16 = mybir.dt.bfloat16
AF = mybir.ActivationFunctionType
ALU = mybir.AluOpType
AX = mybir.AxisListType

NCORES = 8
D = 1024
SEQ = 8192
BATCH = 2
TPC = 2048
NB = 16
NBH = 18
D_IN = 1792
NE = 32
EPS = 1e-6
BIG = 1.0e30
CAP = 256
NOV = 31
OVB = NE * CAP
NSLOT = OVB + NOV * 128
I32 = mybir.dt.int32

ENGS = ("pe", "act", "dve", "pool", "sp")
EPOCH = 8192
NDMASEM = 24
NDMASEM_Q = {"pool": 72}


class Prog:
    def __init__(self, nc):
        self.nc = nc
        self.streams = {e: [] for e in ENGS}
        self.cnt = {e: 0 for e in ENGS}
        self.last_w = {}
        self.readers = {}
        self.waited = {}
        self.dma_known = {e: set() for e in ENGS}
        self.sems = {}
        self.dsems = {}
        self.dma_rr = {"sp": 0, "pool": 0, "act": 0}
        self.ndma = 0
        self.out_dmas = []
        self.bank_acc = {}

    def _sem(self, eng, epoch):
        k = (eng, epoch)
        if k not in self.sems:
            self.sems[k] = self.nc.alloc_semaphore(f"s_{eng}_{epoch}")
        return self.sems[k]

    def _semval(self, eng, seq):
        return (self._sem(eng, (seq - 1) // EPOCH), (seq - 1) % EPOCH + 1)

    def _dep_waits(self, eng, reads, writes, strict=False):
        deps = []
        for k in reads:
            r = self.last_w.get(k)
            if r is not None:
                deps.append((r, True))
        for k in writes:
            r = self.last_w.get(k)
            if r is not None:
                deps.append((r, False))
            for r in self.readers.get(k, {}).values():
                deps.append((r, False))
        for k in list(reads) + list(writes):
            if k.startswith("bank"):
                r = self.bank_acc.get(k)
                if r is not None and r[1] != eng:
                    deps.append((r, False))
        waits = []
        for r, raw in deps:
            if r[0] == "dma":
                _, did, sem, val = r
                if did in self.dma_known[eng]:
                    continue
                self.dma_known[eng].add(did)
                waits.append((sem, val))
            else:
                _, peng, seq = r
                if peng == eng and not strict and eng == "pe":
                    continue
                if self.waited.get((eng, peng), 0) >= seq:
                    continue
                self.waited[(eng, peng)] = seq
                waits.append(self._semval(peng, seq))
        return waits

    def _commit(self, ref, rkey, reads, writes):
        for k in writes:
            self.last_w[k] = ref
            self.readers[k] = {}
        for k in reads:
            self.readers.setdefault(k, {})[rkey] = ref

    op_limit = None

    def op(self, eng, fn, reads=(), writes=()):
        if self.op_limit is not None:
            if self.op_limit <= 0:
                return None
            self.op_limit -= 1
        waits = self._dep_waits(eng, reads, writes)
        self.cnt[eng] += 1
        seq = self.cnt[eng]
        ref = ("eng", eng, seq)
        self._commit(ref, eng, reads, writes)
        for k in list(reads) + list(writes):
            if k.startswith("bank"):
                self.bank_acc[k] = ref
        self.streams[eng].append((waits, fn, self._semval(eng, seq)[0], 1))
        return ref

    def dma(self, queue, fn, reads=(), writes=(), is_out=False):
        waits = self._dep_waits(queue, reads, writes, strict=True)
        nsem = NDMASEM_Q.get(queue, NDMASEM)
        idx = self.dma_rr[queue]
        self.dma_rr[queue] = (idx + 1) % nsem
        k = (queue, idx)
        if k not in self.dsems:
            self.dsems[k] = [self.nc.alloc_semaphore(f"d_{queue}_{idx}"), 0, None]
        ent = self.dsems[k]
        if ent[2] is not None and ent[2] not in self.dma_known[queue]:
            waits.append((ent[0], ent[1] * 16))
            self.dma_known[queue].add(ent[2])
        ent[1] += 1
        self.ndma += 1
        did = self.ndma
        ent[2] = did
        ref = ("dma", did, ent[0], ent[1] * 16)
        self._commit(ref, ("d", did), reads, writes)
        self.streams[queue].append((waits, fn, ent[0], 16))
        if is_out:
            self.out_dmas.append(ref)
        return ref

    def barrier(self, dma=True):
        for eng in ENGS:
            waits = []
            for peng in ENGS:
                if (peng != eng or eng in ("act", "dve", "pool")) and self.cnt[peng] > 0 and self.waited.get((eng, peng), 0) < self.cnt[peng]:
                    self.waited[(eng, peng)] = self.cnt[peng]
                    waits.append(self._semval(peng, self.cnt[peng]))
            for (q, idx), ent in (self.dsems.items() if dma else ()):
                if ent[2] is not None and ent[2] not in self.dma_known[eng]:
                    self.dma_known[eng].add(ent[2])
                    waits.append((ent[0], ent[1] * 16))
            self.streams[eng].append((waits, None, None, 0))
        self.last_w = {} if dma else {k: r for k, r in self.last_w.items() if r[0] == "dma"}
        self.readers = {}
        self.bank_acc = {}

    def finish(self):
        waits = []
        for r in self.out_dmas:
            if r[1] not in self.dma_known["sp"]:
                self.dma_known["sp"].add(r[1])
                waits.append((r[2], r[3]))
        self.streams["sp"].append((waits, None, None, 0))

    def emit(self):
        nc = self.nc
        streams = self.streams

        def run(name, eng):
            for waits, fn, sem, inc in streams[name]:
                for s, v in waits:
                    eng.wait_ge(s, v)
                if fn is None:
                    continue
                fn(eng).then_inc(sem, inc)

        with nc.Block() as block:
            @block.sync
            def _(e):
                run("sp", e)

            @block.tensor
            def _(e):
                run("pe", e)

            @block.scalar
            def _(e):
                run("act", e)

            @block.vector
            def _(e):
                run("dve", e)

            @block.gpsimd
            def _(e):
                run("pool", e)


def bc(ap, shape):
    return ap.unsqueeze(len(ap.shape)).broadcast_to(list(shape))


def build_program(stage=99, ndbg=0, nblk=None):
    nc = bass.Bass("TRN2", target_bir_lowering=False)

    def din(name, shape):
        return nc.dram_tensor(name, list(shape), F32, kind="ExternalInput").ap()

    xh = din("xh", [NBH * 128, D])
    pin = din("p", [TPC, 256])
    flags_d = din("flags", [128, 2])
    biasg_d = din("biasg", [128, 3072])
    maskc_d = din("maskc", [128, 3072])
    ident_d = din("ident", [128, 128])
    w_in_d = din("w_in", [D, D_IN])
    gvec_d = din("gvec", [128, 24])
    lnv_d = din("lnv", [2, 512])
    wsT_d = din("wsT", [128, 512])
    bsT_d = din("bsT", [128, 4])
    sink_d = din("sink", [1, 8])
    w_out_d = din("w_out", [D, D])
    wr_d = din("wr", [D, 36])
    br_d = din("br", [1, 36])
    wall_d = din("w_exp", [NE * 128, 3, 2048])
    wpp_d = din("w_ple_proj", [256, D])
    wpg_d = din("w_ple_gate", [D, D])
    rows_d = din("rows", [3, D])
    tri_d = din("tri", [128, 256])
    cvec_d = din("cvec", [128, 128])
    mg_d = nc.dram_tensor("mg", [NSLOT, D], BF16, kind="Internal").ap()
    og_d = nc.dram_tensor("og", [NSLOT, D], BF16, kind="Internal").ap()
    wbf_d = nc.dram_tensor("wbf", [NE * 128, 6144], BF16, kind="Internal").ap()
    out_d = nc.dram_tensor("out", [TPC, D], F32, kind="ExternalOutput").ap()
    dbg_d = None
    if ndbg:
        dbg_d = nc.dram_tensor("dbg", [128, ndbg], F32, kind="ExternalOutput").ap()

    P = Prog(nc)
    def sb(name, shape, dt):
        return nc.alloc_sbuf_tensor("s_" + name, shape, dt)
    bank = [nc.alloc_psum_tensor(f"bank{i}", [128, 512], F32) for i in range(8)]
    bkey = [f"bank{i}" for i in range(8)]

    h = sb("h", [128, NB, D], F32)
    hnb = sb("hnb", [128, NB, D], BF16)
    w12 = sb("w12", [128, 2, NB], F32)
    idx = sb("idx", [128, 2, NB], I32)
    trib = sb("trib", [128, 256], BF16)
    cvec = sb("cvec", [128, 128], F32)
    gidx = sb("gidx", [128, NOV], I32)
    idb = sb("idb", [128, 128], BF16)
    idf = sb("idf", [128, 128], F32)
    gvec = sb("gvec", [128, 24], F32)
    cst_ = sb("cst", [128, 4], F32)
    stat = sb("stat", [128, 160], F32)
    junk = sb("junk", [128, D], BF16)
    L = sb("L", [128, NB, 36], F32)
    flags = sb("flags", [128, 2], F32)

    dbg_col = [0]

    def dump(ap_sb, ncols, keys, cast=False):
        c0 = dbg_col[0]
        dbg_col[0] += ncols
        assert dbg_col[0] <= ndbg, dbg_col[0]
        q = "pool" if cast else "sp"
        P.dma(q, lambda e: e.dma_start(out=dbg_d[:, c0:c0 + ncols], in_=ap_sb), reads=keys, is_out=True)
        return c0

    N_LATE = 8
    conv_todo = [(ex, k) for ex in range(NE - N_LATE) for k in range(3)]
    conv_late = [(ex, k) for ex in range(NE - N_LATE, NE) for k in range(3)]

    def conv_step(n):
        for _ in range(n):
            if not conv_todo:
                return
            ex, k = conv_todo.pop(0)
            P.dma("pool", lambda e, ex=ex, k=k: e.dma_start(out=wbf_d[ex * 128:(ex + 1) * 128, k * 2048:(k + 1) * 2048],
                                                            in_=wall_d[ex * 128:(ex + 1) * 128, k, :]), writes=[f"wbf{ex}_{k}"])

    def rstd_from_ss(col, n, key):
        P.op("dve", lambda e: e.tensor_scalar(out=stat[:, col:col + 1], in0=stat[:, col:col + 1], scalar1=1.0 / n,
                                              scalar2=EPS, op0=ALU.mult, op1=ALU.add), reads=[key], writes=[key])
        P.op("pool", lambda e: e.tensor_tensor(out=stat[:, col:col + 1], in0=stat[:, col:col + 1], in1=cst_[:, 0:1],
                                               op=ALU.pow), reads=[key, "cst"], writes=[key])

    P.dma("sp", lambda e: e.dma_start(out=idf[:], in_=ident_d[:, :]), writes=["idf"])
    P.dma("pool", lambda e: e.dma_start(out=idb[:], in_=ident_d[:, :]), writes=["idb"])
    P.dma("sp", lambda e: e.dma_start(out=gvec[:], in_=gvec_d[:, :]), writes=["gvec"])
    P.dma("sp", lambda e: e.dma_start(out=flags[:], in_=flags_d[:, :]), writes=["flags"])
    P.op("dve", lambda e: e.memset(cst_[:, 0:1], -0.5), writes=["cst"])
    P.dma("pool", lambda e: e.dma_start(out=trib[:], in_=tri_d[:, :]), writes=["trib"])
    P.dma("sp", lambda e: e.dma_start(out=cvec[:], in_=cvec_d[:, :]), writes=["cvec"])
    gmix = gvec[:, 0:8]
    gout = gvec[:, 8:16]
    gffn = gvec[:, 16:24]

    SS_X, SS_A, SS_B, SS_M, SS_V, SS_P, SS_F = 0, 18, 34, 50, 66, 98, 114

    with contextlib.ExitStack() as _es:
        yna = _es.enter_context(nc.sbuf_tensor("s_yna", [128, NB, 512], BF16))
        qT = _es.enter_context(nc.sbuf_tensor("s_qT", [128, 4, TPC], BF16))
        kT = _es.enter_context(nc.sbuf_tensor("s_kT", [128, NBH * 128], BF16))
        vaug = _es.enter_context(nc.sbuf_tensor("s_vaug", [128, NBH, 2, 66], BF16))
        expB = _es.enter_context(nc.sbuf_tensor("s_expB", [128, 3072], BF16))
        esink = _es.enter_context(nc.sbuf_tensor("s_esink", [128, 8], F32))

        with contextlib.ExitStack() as _es:
            w_in_bf = _es.enter_context(nc.sbuf_tensor("s_w_in_bf", [128, 8, D_IN], BF16))
            xn = _es.enter_context(nc.sbuf_tensor("s_xn", [128, 2, D], BF16))
            aT = _es.enter_context(nc.sbuf_tensor("s_aT", [128, 2, 8, 128], BF16))
            uv = _es.enter_context(nc.sbuf_tensor("s_uv", [128, 2, 2, 512], F32))
            vc = _es.enter_context(nc.sbuf_tensor("s_vc", [128, 512], F32))
            vn = _es.enter_context(nc.sbuf_tensor("s_vn", [128, 512], BF16))
            ya = _es.enter_context(nc.sbuf_tensor("s_ya", [128, 512], F32))
            lnv = _es.enter_context(nc.sbuf_tensor("s_lnv", [128, 2, 512], F32))
            wsT = _es.enter_context(nc.sbuf_tensor("s_wsT", [128, 512], BF16))
            bsT = _es.enter_context(nc.sbuf_tensor("s_bsT", [128, 4], F32))
            mTf32 = hnb[:].rearrange("p c t -> p (c t)").bitcast(F32)
            btmp = mTf32[:, 0:3072]
            mtmp = mTf32[:, 3072:6144]
            xhalo = mTf32[:, 6144:8192].rearrange("p (s d) -> p s d", s=2)
            for c in range(8):
                P.dma("pool", lambda e, c=c: e.dma_start(out=w_in_bf[:, c, :], in_=w_in_d[c * 128:(c + 1) * 128, :]),
                      writes=[f"w_in{c}"])
            P.dma("pool", lambda e: e.dma_start(out=wsT[:], in_=wsT_d[:, :]), writes=["wsT"])
            P.dma("sp", lambda e: e.dma_start(out=bsT[:], in_=bsT_d[:, :]), writes=["bsT"])
            for i in range(2):
                P.dma("sp", lambda e, i=i: e.dma_start(out=lnv[:, i, :], in_=lnv_d[i:i + 1, :].broadcast_to([128, 512])),
                      writes=["lnv"])
            P.dma("sp", lambda e: e.dma_start(out=esink[:], in_=sink_d[0:1, :].broadcast_to([128, 8])), writes=["esink"])
            P.op("act", lambda e: e.activation(out=esink[:], in_=esink[:], func=AF.Exp), reads=["esink"], writes=["esink"])
            P.dma("sp", lambda e: e.dma_start(out=btmp, in_=biasg_d[:, :]), writes=["btmp"])
            P.dma("sp", lambda e: e.dma_start(out=mtmp, in_=maskc_d[:, :]), writes=["mtmp"])
            P.op("act", lambda e: e.activation(out=btmp, in_=btmp, func=AF.Exp), reads=["btmp"], writes=["btmp"])
            P.op("dve", lambda e: e.tensor_tensor(out=expB[:], in0=btmp, in1=mtmp, op=ALU.mult),
                 reads=["btmp", "mtmp"], writes=["expB"])
            P.op("dve", lambda e: e.memset(vaug[:, :, :, 64:65], 1.0), writes=["vones"])

            order = [0, NBH - 1] + list(range(1, NBH - 1))
            if nblk is not None:
                order = order[:nblk]
            wk = [f"w_in{c}" for c in range(8)]

            def blk(it):
                bi = order[it]
                halo = bi in (0, NBH - 1)
                hs = 0 if bi == 0 else 1
                xt = xhalo[:, hs, :] if halo else h[:, bi - 1, :]
                xk = f"xhalo{hs}" if halo else f"h{bi - 1}"
                return bi, halo, hs, xt, xk, it % 2

            def a0(it):
                bi, halo, hs, xt, xk, sl = blk(it)
                P.dma("sp", lambda e: e.dma_start(out=xt, in_=xh[bi * 128:(bi + 1) * 128, :]), writes=[xk])

            def a1(it):
                bi, halo, hs, xt, xk, sl = blk(it)
                sk = f"ssx{bi}"
                P.op("act", lambda e: e.activation(out=junk[:], in_=xt, func=AF.Square, accum_out=stat[:, SS_X + bi:SS_X + bi + 1]),
                     reads=[xk], writes=["junk", sk])
                rstd_from_ss(SS_X + bi, D, sk)

            def a1x(it):
                bi, halo, hs, xt, xk, sl = blk(it)
                P.op("act", lambda e: e.activation(out=xn[:, sl, :], in_=xt, func=AF.Copy, scale=stat[:, SS_X + bi:SS_X + bi + 1]),
                     reads=[xk, f"ssx{bi}"], writes=[f"xn{sl}"])

            def a2(it):
                bi, halo, hs, xt, xk, sl = blk(it)
                TBb = bank[sl][:].bitcast(BF16)
                for c in range(8):
                    P.op("pe", lambda e, c=c: e.transpose(TBb[:, c * 128:(c + 1) * 128], xn[:, sl, c * 128:(c + 1) * 128], idb[:]),
                         reads=[f"xn{sl}", "idb"], writes=[bkey[sl]])
                P.op("dve", lambda e: e.tensor_tensor(out=aT[:, sl], in0=TBb.rearrange("p (c t) -> p c t", c=8),
                                                      in1=bc(gmix, [128, 8, 128]), op=ALU.mult),
                     reads=[bkey[sl], "gvec"], writes=[f"aT{sl}"])

            def bst(it):
                bi, halo, hs, xt, xk, sl = blk(it)
                b = bi - 1
                for c in range(8):
                    P.op("pe", lambda e, c=c: e.matmul(bank[5][:, 0:128], lhsT=w_in_bf[:, c, 1536:1664], rhs=aT[:, sl, c, :],
                                                       start=(c == 0), stop=(c == 7)),
                         reads=[f"aT{sl}", wk[c]], writes=[bkey[5]])
                for c in range(8):
                    P.op("pe", lambda e, c=c: e.matmul(bank[5][:, 128:256], lhsT=aT[:, sl, c, :], rhs=w_in_bf[:, c, 1664:1792],
                                                       start=(c == 0), stop=(c == 7)),
                         reads=[f"aT{sl}", wk[c]], writes=[bkey[5]])
                if not halo:
                    for n, bk in ((0, 2), (1, 3)):
                        for c in range(8):
                            P.op("pe", lambda e, c=c, n=n, bk=bk: e.matmul(bank[bk][:, :], lhsT=aT[:, sl, c, :],
                                                                           rhs=w_in_bf[:, c, n * 512:(n + 1) * 512],
                                                                           start=(c == 0), stop=(c == 7)),
                                 reads=[f"aT{sl}", wk[c]], writes=[bkey[bk]])
                    for qc in range(4):
                        for c in range(8):
                            P.op("pe", lambda e, c=c, qc=qc: e.matmul(bank[4][:, qc * 128:(qc + 1) * 128],
                                                                      lhsT=w_in_bf[:, c, 1024 + qc * 128:1024 + (qc + 1) * 128],
                                                                      rhs=aT[:, sl, c, :], start=(c == 0), stop=(c == 7)),
                                 reads=[f"aT{sl}", wk[c]], writes=[bkey[4]])
                P.op("act", lambda e: e.activation(out=kT[:, bi * 128:(bi + 1) * 128], in_=bank[5][:, 0:128], func=AF.Copy),
                     reads=[bkey[5]], writes=[f"kT{bi}"])
                vsrc = bank[5][:, 128:256].rearrange("p (k d) -> p k d", k=2)
                if halo:
                    P.op("dve", lambda e: e.tensor_scalar(out=vaug[:, bi, :, 0:64], in0=vsrc, scalar1=flags[:, hs:hs + 1], scalar2=None,
                                                          op0=ALU.mult), reads=[bkey[5], "flags"], writes=[f"va{bi}"])
                    P.op("dve", lambda e: e.tensor_copy(out=vaug[:, bi, :, 64:65],
                                                        in_=flags[:, hs:hs + 1].unsqueeze(1).broadcast_to([128, 2, 1])),
                         reads=["flags", "vones"], writes=[f"vo{bi}"])
                    return
                P.op("dve", lambda e: e.tensor_copy(out=vaug[:, bi, :, 0:64], in_=vsrc), reads=[bkey[5]], writes=[f"va{bi}"])
                for n, bk in ((0, 2), (1, 3)):
                    P.op("act", lambda e, n=n, bk=bk: e.activation(out=uv[:, sl, n, :], in_=bank[bk][:, :], func=AF.Gelu_apprx_tanh),
                         reads=[bkey[bk]], writes=[f"uv{sl}{n}"])
                P.op("act", lambda e: e.activation(out=qT[:, :, b * 128:(b + 1) * 128],
                                                   in_=bank[4][:, :].rearrange("p (c t) -> p c t", c=4), func=AF.Copy),
                     reads=[bkey[4]], writes=[f"qT{b}"])

            def cst_ln(it):
                bi, halo, hs, xt, xk, sl = blk(it)
                if halo:
                    return
                b = bi - 1
                vk = f"ssv{b}"
                c6 = SS_V + 2 * b
                P.op("dve", lambda e: e.bn_stats(out=stat[:, 150:156], in_=uv[:, sl, 1, :]), reads=[f"uv{sl}1"], writes=["bn6"])
                P.op("dve", lambda e: e.bn_aggr(out=stat[:, c6:c6 + 2], in_=stat[:, 150:156]), reads=["bn6"], writes=[vk])
                P.op("dve", lambda e: e.tensor_scalar(out=stat[:, c6 + 1:c6 + 2], in0=stat[:, c6 + 1:c6 + 2], scalar1=EPS,
                                                      scalar2=None, op0=ALU.add), reads=[vk], writes=[vk])
                P.op("pool", lambda e: e.tensor_tensor(out=stat[:, c6 + 1:c6 + 2], in0=stat[:, c6 + 1:c6 + 2],
                                                       in1=cst_[:, 0:1], op=ALU.pow), reads=[vk, "cst"], writes=[vk])
                P.op("dve", lambda e: e.tensor_scalar(out=vc[:], in0=uv[:, sl, 1, :], scalar1=stat[:, c6:c6 + 1],
                                                      scalar2=stat[:, c6 + 1:c6 + 2], op0=ALU.subtract, op1=ALU.mult),
                     reads=[f"uv{sl}1", vk], writes=["vc"])
                P.op("dve", lambda e: e.tensor_tensor(out=vc[:], in0=vc[:], in1=lnv[:, 0, :], op=ALU.mult),
                     reads=["vc", "lnv"], writes=["vc"])
                P.op("dve", lambda e: e.tensor_tensor(out=vn[:], in0=vc[:], in1=lnv[:, 1, :], op=ALU.add),
                     reads=["vc", "lnv"], writes=["vn"])

            def cst_sp(it):
                bi, halo, hs, xt, xk, sl = blk(it)
                if halo:
                    return
                b = bi - 1
                for hh in range(4):
                    P.op("pe", lambda e, hh=hh: e.matmul(bank[6][:, hh * 128:(hh + 1) * 128], lhsT=wsT[:, hh * 128:(hh + 1) * 128],
                                                         rhs=vn[:, hh * 128:(hh + 1) * 128], start=True, stop=True),
                         reads=["vn", "wsT"], writes=[bkey[6]])
                for hh in range(4):
                    P.op("dve", lambda e, hh=hh: e.scalar_tensor_tensor(out=ya[:, hh * 128:(hh + 1) * 128],
                                                                        in0=bank[6][:, hh * 128:(hh + 1) * 128],
                                                                        scalar=bsT[:, hh:hh + 1],
                                                                        in1=uv[:, sl, 0, hh * 128:(hh + 1) * 128],
                                                                        op0=ALU.add, op1=ALU.mult),
                         reads=[bkey[6], "bsT", f"uv{sl}0"], writes=["ya"])
                ak = f"ssa{b}"
                P.op("act", lambda e: e.activation(out=junk[:, 0:512], in_=ya[:], func=AF.Square, accum_out=stat[:, SS_A + b:SS_A + b + 1]),
                     reads=["ya"], writes=["junk", ak])
                rstd_from_ss(SS_A + b, 512, ak)
                P.op("act", lambda e: e.activation(out=yna[:, b, :], in_=ya[:], func=AF.Copy, scale=stat[:, SS_A + b:SS_A + b + 1]),
                     reads=["ya", ak], writes=[f"yna{b}"])

            nit = len(order)
            for it in range(nit):
                a0(it)
            if nit > 0:
                a1(0)
                a1x(0)
                a2(0)
            if nit > 1:
                a1(1)
                a1x(1)
            for it in range(nit + 1):
                if it - 1 >= 0:
                    cst_ln(it - 1)
                if it + 2 < nit:
                    a1(it + 2)
                if it + 1 < nit:
                    a2(it + 1)
                if it < nit:
                    bst(it)
                if it + 2 < nit:
                    a1x(it + 2)
                if it - 1 >= 0:
                    cst_sp(it - 1)
                if it >= 1:
                    conv_step(2)

            if stage == 1 and nblk is not None:
                dump(expB[:, 0:512], 512, ["expB"], cast=True)
                P.barrier()
            if stage == 1 and nblk is None:
                dump(yna[:, 0, :], 512, ["yna0"], cast=True)
                dump(yna[:, 15, :], 512, ["yna15"], cast=True)
                dump(qT[:, :, 0:128], 512, ["qT0"], cast=True)
                dump(kT[:, 0:256], 256, ["kT0", "kT1"], cast=True)
                dump(vaug[:, 0:2], 264, ["va0", "va1", "vo0", "vones"], cast=True)
                dump(vaug[:, 17], 132, ["va17", "vo17"], cast=True)
                dump(stat[:, 0:18], 18, [f"ssx{i}" for i in range(18)], cast=False)
                P.barrier()
        if stage == 1:
            P.finish()
            P.emit()
            return nc
        P.barrier()

        with contextlib.ExitStack() as _es:
            w_out_bf = _es.enter_context(nc.sbuf_tensor("s_w_out_bf", [128, 8, D], BF16))
            wr = _es.enter_context(nc.sbuf_tensor("s_wr", [128, 8, 36], BF16))
            brt = _es.enter_context(nc.sbuf_tensor("s_brt", [128, 36], F32))
            E = _es.enter_context(nc.sbuf_tensor("s_E", [128, 2, 3, 512], BF16))
            den = _es.enter_context(nc.sbuf_tensor("s_den", [128, 8], F32))
            yb = _es.enter_context(nc.sbuf_tensor("s_yb", [128, 512], F32))
            ynb = _es.enter_context(nc.sbuf_tensor("s_ynb", [128, NB, 512], BF16))
            yT = _es.enter_context(nc.sbuf_tensor("s_yT", [128, 2, 8, 128], BF16))
            mTf = _es.enter_context(nc.sbuf_tensor("s_mTf", [128, 8, 128], BF16))
            for c in range(8):
                P.dma("pool", lambda e, c=c: e.dma_start(out=w_out_bf[:, c, :], in_=w_out_d[c * 128:(c + 1) * 128, :]),
                      writes=[f"w_out{c}"])
            P.dma("pool", lambda e: e.dma_start(out=wr[:], in_=wr_d.rearrange("(c p) f -> p c f", p=128)), writes=["wr"])
            P.dma("sp", lambda e: e.dma_start(out=brt[:], in_=br_d[0:1, :].broadcast_to([128, 36])), writes=["brt"])
            zrow = _es.enter_context(nc.sbuf_tensor("s_zrow", [128, D], BF16))
            P.op("dve", lambda e: e.memset(zrow[:], 0.0), writes=["zrow"])
            for r0 in range(0, NSLOT, 128 * 5):
                nr = min(128 * 5, NSLOT - r0)
                P.dma("sp", lambda e, r0=r0, nr=nr: e.dma_start(
                    out=mg_d[r0:r0 + nr, :].rearrange("(j p) d -> p j d", p=128),
                    in_=zrow[:].unsqueeze(1).broadcast_to([128, nr // 128, D])), reads=["zrow"], writes=["mg"])
            expB4 = expB[:].rearrange("p (kb hh q) -> p kb hh q", kb=3, hh=8)
            SB = (0, 1, 2)
            OB = (3, 4)
            TYB, OPB, RB = 5, (6, 7), 5

            def k1_s(b, kv):
                bi = b + 1
                pr = slice(kv * 64, (kv + 1) * 64)
                for kb in range(3):
                    kblk = bi - 1 + kb
                    P.op("pe", lambda e, kb=kb, kblk=kblk: e.matmul(bank[SB[kb]][:, :], lhsT=kT[pr, kblk * 128:(kblk + 1) * 128],
                                                                     rhs=qT[pr, :, b * 128:(b + 1) * 128], start=True, stop=True),
                         reads=[f"kT{kblk}", f"qT{b}"], writes=[bkey[SB[kb]]])

            def k1_e(b, kv):
                for kb in range(3):
                    P.op("act", lambda e, kb=kb: e.activation(out=E[:, kv, kb, :], in_=bank[SB[kb]][:, :], func=AF.Exp, scale=0.125),
                         reads=[bkey[SB[kb]]], writes=[f"E{kv}{kb}"])
                for kb in range(3):
                    P.op("dve", lambda e, kb=kb: e.tensor_tensor(
                        out=E[:, kv, kb, :].rearrange("p (g q) -> p g q", g=4), in0=E[:, kv, kb, :].rearrange("p (g q) -> p g q", g=4),
                        in1=expB4[:, kb, kv * 4:(kv + 1) * 4, :], op=ALU.mult),
                         reads=[f"E{kv}{kb}", "expB"], writes=[f"E{kv}{kb}"])

            def k1_pv(b, kv):
                bi = b + 1
                ob = OB[kv]
                for g in range(4):
                    for kb in range(3):
                        kblk = bi - 1 + kb
                        P.op("pe", lambda e, kb=kb, g=g, kblk=kblk: e.matmul(
                            bank[ob][:, g * 65:(g + 1) * 65], lhsT=E[:, kv, kb, g * 128:(g + 1) * 128],
                            rhs=vaug[:, kblk, kv, 0:65], start=(kb == 0), stop=(kb == 2)),
                             reads=[f"E{kv}{kb}", f"va{kblk}", f"vo{kblk}", "vones"], writes=[bkey[ob]])

            def k2a(b):
                for kv in range(2):
                    ob = OB[kv]
                    o3 = bank[ob][:, 0:260].rearrange("p (g d) -> p g d", g=4)
                    P.op("dve", lambda e, kv=kv, o3=o3: e.tensor_tensor(out=den[:, kv * 4:(kv + 1) * 4].unsqueeze(2), in0=o3[:, :, 64:65],
                                                                        in1=esink[:, kv * 4:(kv + 1) * 4].unsqueeze(2), op=ALU.add),
                         reads=[bkey[ob], "esink"], writes=[f"den{kv}"])
                    P.op("dve", lambda e, kv=kv: e.reciprocal(out=den[:, kv * 4:(kv + 1) * 4], in_=den[:, kv * 4:(kv + 1) * 4]),
                         reads=[f"den{kv}"], writes=[f"den{kv}"])
                    P.op("dve", lambda e, kv=kv, o3=o3: e.tensor_tensor(
                        out=yb[:, kv * 256:(kv + 1) * 256].rearrange("p (g d) -> p g d", g=4), in0=o3[:, :, 0:64],
                        in1=bc(den[:, kv * 4:(kv + 1) * 4], [128, 4, 64]), op=ALU.mult),
                         reads=[bkey[ob], f"den{kv}"], writes=[f"yb{kv}"])
                bk_ = f"ssb{b}"
                P.op("act", lambda e: e.activation(out=junk[:, 0:512], in_=yb[:], func=AF.Square, accum_out=stat[:, SS_B + b:SS_B + b + 1]),
                     reads=["yb0", "yb1"], writes=["junk", bk_])
                rstd_from_ss(SS_B + b, 512, bk_)

            def k2y(b):
                P.op("act", lambda e: e.activation(out=ynb[:, b, :], in_=yb[:], func=AF.Copy, scale=stat[:, SS_B + b:SS_B + b + 1]),
                     reads=["yb0", "yb1", f"ssb{b}"], writes=[f"ynb{b}"])

            def k2b_t(b):
                par = b % 2
                tyb = (5, 2)[par]
                opb = ((6, 7), (3, 4))[par]
                T0 = bank[tyb][:].bitcast(BF16)
                for c in range(8):
                    src = yna[:, b, c * 128:(c + 1) * 128] if c < 4 else ynb[:, b, (c - 4) * 128:(c - 3) * 128]
                    P.op("pe", lambda e, c=c, src=src: e.transpose(T0[:, c * 128:(c + 1) * 128], src, idb[:]),
                         reads=[f"yna{b}", f"ynb{b}", "idb"], writes=[bkey[tyb]])
                P.op("dve", lambda e: e.tensor_tensor(out=yT[:, par], in0=T0.rearrange("p (c t) -> p c t", c=8),
                                                      in1=bc(gout, [128, 8, 128]), op=ALU.mult),
                     reads=[bkey[tyb], "gvec"], writes=[f"yT{par}"])
                for n in range(2):
                    for c in range(8):
                        P.op("pe", lambda e, c=c, n=n: e.matmul(bank[opb[n]][:, :], lhsT=yT[:, par, c, :],
                                                                rhs=w_out_bf[:, c, n * 512:(n + 1) * 512], start=(c == 0), stop=(c == 7)),
                             reads=[f"yT{par}", f"w_out{c}"], writes=[bkey[opb[n]]])

            def k2b_h(b):
                opb = ((6, 7), (3, 4))[b % 2]
                for n in range(2):
                    P.op("dve", lambda e, n=n: e.tensor_tensor(out=h[:, b, n * 512:(n + 1) * 512], in0=bank[opb[n]][:, :],
                                                               in1=h[:, b, n * 512:(n + 1) * 512], op=ALU.add),
                         reads=[bkey[opb[n]], f"h{b}"], writes=[f"h{b}"])

            def k3a(b):
                mk = f"ssm{b}"
                P.op("act", lambda e: e.activation(out=junk[:], in_=h[:, b, :], func=AF.Square, accum_out=stat[:, SS_M + b:SS_M + b + 1]),
                     reads=[f"h{b}"], writes=["junk", mk])
                rstd_from_ss(SS_M + b, D, mk)

            def k3b(b):
                P.op("act", lambda e: e.activation(out=hnb[:, b, :], in_=h[:, b, :], func=AF.Copy, scale=stat[:, SS_M + b:SS_M + b + 1]),
                     reads=[f"h{b}", f"ssm{b}"], writes=[f"hnb{b}"])
                tb = 0
                TBm = bank[tb][:].bitcast(BF16)
                for c in range(8):
                    P.op("pe", lambda e, c=c: e.transpose(TBm[:, c * 128:(c + 1) * 128], hnb[:, b, c * 128:(c + 1) * 128], idb[:]),
                         reads=[f"hnb{b}", "idb"], writes=[bkey[tb]])
                P.op("dve", lambda e: e.tensor_tensor(out=mTf[:], in0=TBm.rearrange("p (c t) -> p c t", c=8),
                                                      in1=bc(gffn, [128, 8, 128]), op=ALU.mult),
                     reads=[bkey[tb], "gvec"], writes=["mTf"])
                for c in range(8):
                    P.op("pe", lambda e, c=c: e.matmul(bank[1][:, 0:36], lhsT=mTf[:, c, :], rhs=wr[:, c, :], start=(c == 0), stop=(c == 7)),
                         reads=["mTf", "wr"], writes=[bkey[1]])
                P.op("dve", lambda e: e.tensor_tensor(out=L[:, b, :], in0=bank[1][:, 0:36], in1=brt[:], op=ALU.add),
                     reads=[bkey[1], "brt"], writes=["L"])

            for i in range(-1, NB):
                a, m_ = i + 1, i
                va, vm = 0 <= a < NB, 0 <= m_ < NB
                if va:
                    k1_s(a, 0)
                    k1_e(a, 0)
                if vm:
                    k2a(m_)
                if va:
                    k1_s(a, 1)
                    k1_e(a, 1)
                    k1_pv(a, 0)
                    k1_pv(a, 1)
                if vm:
                    k2y(m_)
                conv_step(3)
            for j in range(NB + 1):
                o_, z = j, j - 1
                vo, vz = 0 <= o_ < NB, 0 <= z < NB
                if vo:
                    k2b_t(o_)
                if vz:
                    k3a(z)
                if vo:
                    k2b_h(o_)
                if vz:
                    k3b(z)
                conv_step(3)
            conv_step(len(conv_todo))
            if stage == 2:
                dump(h[:, 0, :], 1024, ["h0"])
                dump(h[:, 15, :], 1024, ["h15"])
                dump(L[:].rearrange("p b f -> p (b f)"), 576, ["L"])
                P.barrier()
    if stage == 2:
        P.finish()
        P.emit()
        return nc
    P.barrier()

    conv_todo.extend(conv_late)
    conv_step(len(conv_todo))
    e256 = cvec[:, 0:32]
    thr = cvec[:, 32:46]
    uvec = cvec[:, 46:77]
    pcol = cvec[:, 77:78]
    onesr = cvec[:, 78:110]
    with contextlib.ExitStack() as _es:
        r_a = _es.enter_context(nc.sbuf_tensor("s_r_a", [128, NB, 4], F32))
        r_b = _es.enter_context(nc.sbuf_tensor("s_r_b", [128, NB, 4], F32))
        r_s = _es.enter_context(nc.sbuf_tensor("s_r_s", [128, 8, NB], F32))
        lem = _es.enter_context(nc.sbuf_tensor("s_lem", [128, NB, NE], F32))
        Mb = _es.enter_context(nc.sbuf_tensor("s_Mb", [128, NB, NE], BF16))
        oh1 = _es.enter_context(nc.sbuf_tensor("s_oh1", [128, NB, NE], F32))
        oh2 = _es.enter_context(nc.sbuf_tensor("s_oh2", [128, NB, NE], F32))
        tot = _es.enter_context(nc.sbuf_tensor("s_tot", [128, NB, NE], F32))
        rk = _es.enter_context(nc.sbuf_tensor("s_rk", [128, NB, NE], F32))
        acc = _es.enter_context(nc.sbuf_tensor("s_acc", [128, 8, NE], F32))
        cmp = _es.enter_context(nc.sbuf_tensor("s_cmp", [128, NE * 31], F32))
        posf = _es.enter_context(nc.sbuf_tensor("s_posf", [128, 2, NB], F32))
        lg = L[:, :, 0:4]
        le = L[:, :, 4:36]
        gmax, sg, m1, m2, rr = (r_s[:, i, :] for i in range(5))
        w1 = w12[:, 0, :]
        w2 = w12[:, 1, :]
        P.op("dve", lambda e: e.tensor_reduce(out=gmax, in_=lg, axis=AX.X, op=ALU.max), reads=["L"], writes=["gmax"])
        P.op("dve", lambda e: e.tensor_tensor(out=r_a[:], in0=lg, in1=bc(gmax, [128, NB, 4]), op=ALU.is_equal),
             reads=["L", "gmax"], writes=["gm"])
        P.op("dve", lambda e: e.tensor_tensor(out=r_b[:], in0=lg, in1=bc(gmax, [128, NB, 4]), op=ALU.subtract),
             reads=["L", "gmax"], writes=["r_b"])
        P.op("act", lambda e: e.activation(out=r_b[:], in_=r_b[:], func=AF.Exp), reads=["r_b"], writes=["r_b"])
        P.op("dve", lambda e: e.tensor_reduce(out=sg, in_=r_b[:], axis=AX.X, op=ALU.add), reads=["r_b"], writes=["sg"])
        P.op("dve", lambda e: e.tensor_scalar(out=r_a[:], in0=r_a[:], scalar1=1.0, scalar2=BIG, op0=ALU.subtract, op1=ALU.mult),
             reads=["gm"], writes=["gm"])
        P.op("dve", lambda e: e.tensor_tensor(out=lem[:].rearrange("p b (g x) -> p b g x", g=4),
                                              in0=le.rearrange("p b (g x) -> p b g x", g=4),
                                              in1=bc(r_a[:], [128, NB, 4, 8]), op=ALU.add), reads=["L", "gm"], writes=["lem"])
        P.op("dve", lambda e: e.tensor_reduce(out=m1, in_=lem[:], axis=AX.X, op=ALU.max), reads=["lem"], writes=["m1"])
        P.op("dve", lambda e: e.tensor_tensor(out=oh1[:], in0=lem[:], in1=bc(m1, [128, NB, NE]), op=ALU.is_equal),
             reads=["lem", "m1"], writes=["oh1"])
        P.op("dve", lambda e: e.scalar_tensor_tensor(out=lem[:], in0=oh1[:], scalar=-BIG, in1=lem[:], op0=ALU.mult, op1=ALU.add),
             reads=["oh1", "lem"], writes=["lem"])
        P.op("dve", lambda e: e.tensor_reduce(out=m2, in_=lem[:], axis=AX.X, op=ALU.max), reads=["lem"], writes=["m2"])
        P.op("dve", lambda e: e.tensor_tensor(out=oh2[:], in0=lem[:], in1=bc(m2, [128, NB, NE]), op=ALU.is_equal),
             reads=["lem", "m2"], writes=["oh2"])
        P.op("dve", lambda e: e.tensor_tensor(out=rr, in0=m2, in1=m1, op=ALU.subtract), reads=["m1", "m2"], writes=["rr"])
        P.op("act", lambda e: e.activation(out=rr, in_=rr, func=AF.Exp), reads=["rr"], writes=["rr"])
        P.op("dve", lambda e: e.scalar_tensor_tensor(out=w1, in0=rr, scalar=1.0, in1=sg, op0=ALU.add, op1=ALU.mult),
             reads=["rr", "sg"], writes=["w1"])
        P.op("dve", lambda e: e.reciprocal(out=w1, in_=w1), reads=["w1"], writes=["w1"])
        P.op("dve", lambda e: e.tensor_tensor(out=w2, in0=rr, in1=w1, op=ALU.mult), reads=["rr", "w1"], writes=["w2"])
        P.op("dve", lambda e: e.tensor_tensor(out=Mb[:], in0=oh1[:], in1=oh2[:], op=ALU.add), reads=["oh1", "oh2"], writes=["Mb"])
        Mflat = Mb[:].rearrange("p b f -> p (b f)")
        P.op("pe", lambda e: e.matmul(bank[0][:, :], lhsT=trib[:, 0:128], rhs=Mflat, start=True, stop=True),
             reads=["Mb", "trib"], writes=[bkey[0]])
        P.op("pe", lambda e: e.matmul(bank[1][:, :], lhsT=trib[:, 128:256], rhs=Mflat, start=True, stop=True),
             reads=["Mb", "trib"], writes=[bkey[1]])
        totf = tot[:].rearrange("p b f -> p (b f)")
        tot_eb = totf.rearrange("p (f b) -> p f b", b=NB)
        P.op("dve", lambda e: e.tensor_copy(out=tot_eb, in_=bank[1][:, :].rearrange("p (b f) -> p f b", b=NB)),
             reads=[bkey[1]], writes=["tot"])
        Sf = lem[:].rearrange("p b f -> p (b f)")
        S_eb = Sf.rearrange("p (f b) -> p f b", b=NB)
        onesf = cmp[:, 0:NB * NE]
        P.op("dve", lambda e: e.memset(onesf, 1.0), writes=["cmp"])
        P.op("dve", lambda e: e.tensor_tensor_scan(out=Sf, data0=onesf, data1=totf, initial=0.0, op0=ALU.mult, op1=ALU.add),
             reads=["cmp", "tot", "lem"], writes=["lem"])
        cnt = acc[:, 0, :]
        ckey = "cnt"
        P.op("dve", lambda e: e.tensor_reduce(out=cnt, in_=tot_eb, axis=AX.X, op=ALU.add), reads=["tot"], writes=["cnt"])
        base = acc[:, 1, :]
        P.op("dve", lambda e: e.tensor_tensor(out=base, in0=S_eb[:, :, NB - 1], in1=cnt, op=ALU.subtract), reads=["lem", "cnt"], writes=["base"])
        P.op("dve", lambda e: e.tensor_tensor(out=Sf, in0=Sf, in1=totf, op=ALU.subtract), reads=["lem", "tot"], writes=["lem"])
        P.op("dve", lambda e: e.tensor_tensor(out=S_eb, in0=S_eb, in1=bc(base, [128, NE, NB]), op=ALU.subtract), reads=["lem", "base"], writes=["lem"])
        P.op("dve", lambda e: e.tensor_tensor(out=rk[:].rearrange("p b f -> p f b"), in0=bank[0][:, :].rearrange("p (b f) -> p f b", b=NB),
                                              in1=S_eb, op=ALU.add), reads=[bkey[0], "lem"], writes=[f"rk{b}" for b in range(NB)])
        ont, ote, ots, dlt = (acc[:, i, :] for i in range(2, 6))
        cmp3 = cmp[:, 0:NE * 14].rearrange("p (f k) -> p f k", k=14)
        P.op("dve", lambda e: e.tensor_tensor(out=cmp3, in0=bc(cnt, [128, NE, 14]), in1=thr.unsqueeze(1).broadcast_to([128, NE, 14]),
                                              op=ALU.is_gt), reads=[ckey, "cvec"], writes=["cmp"])
        P.op("dve", lambda e: e.tensor_reduce(out=ont, in_=cmp3, axis=AX.X, op=ALU.add), reads=["cmp"], writes=["ont"])
        P.op("dve", lambda e: e.tensor_tensor_scan(out=ote, data0=onesr, data1=ont, initial=0.0, op0=ALU.mult, op1=ALU.add),
             reads=["ont", "cvec"], writes=["ote"])
        P.op("dve", lambda e: e.tensor_tensor(out=ots, in0=ote, in1=ont, op=ALU.subtract), reads=["ote", "ont"], writes=["ots"])
        P.op("dve", lambda e: e.tensor_scalar(out=dlt, in0=ots, scalar1=128.0, scalar2=float(OVB - CAP), op0=ALU.mult, op1=ALU.add),
             reads=["ots"], writes=["dlt"])
        P.op("dve", lambda e: e.tensor_tensor(out=dlt, in0=dlt, in1=e256, op=ALU.subtract), reads=["dlt", "cvec"], writes=["dlt"])
        rkeys = [f"rk{b}" for b in range(NB)]
        P.op("dve", lambda e: e.tensor_scalar(out=lem[:], in0=rk[:], scalar1=float(CAP), scalar2=None, op0=ALU.is_ge),
             reads=rkeys, writes=["lem"])
        P.op("dve", lambda e: e.tensor_tensor(out=lem[:], in0=lem[:], in1=dlt.unsqueeze(1).broadcast_to([128, NB, NE]), op=ALU.mult),
             reads=["lem", "dlt"], writes=["lem"])
        P.op("dve", lambda e: e.tensor_tensor(out=rk[:], in0=rk[:], in1=e256.unsqueeze(1).broadcast_to([128, NB, NE]), op=ALU.add),
             reads=rkeys + ["cvec"], writes=["pos"])
        P.op("dve", lambda e: e.tensor_tensor(out=rk[:], in0=rk[:], in1=lem[:], op=ALU.add), reads=["pos", "lem"], writes=["pos"])
        for k, oh in ((0, oh1), (1, oh2)):
            P.op("dve", lambda e, oh=oh: e.tensor_tensor(out=lem[:], in0=rk[:], in1=oh[:], op=ALU.mult),
                 reads=["pos", f"oh{k + 1}", "lem"], writes=["lem"])
            P.op("dve", lambda e, k=k: e.tensor_reduce(out=posf[:, k, :], in_=lem[:], axis=AX.X, op=ALU.add),
                 reads=["lem"], writes=[f"posf{k}"])
        P.op("dve", lambda e: e.tensor_copy(out=idx[:], in_=posf[:]), reads=["posf0", "posf1"], writes=["idx"])
        eov = acc[:, 6, 0:NOV]
        cmpu = cmp[:, 0:NOV * NE].rearrange("p (u f) -> p u f", f=NE)
        P.op("dve", lambda e: e.tensor_tensor(out=cmpu, in0=ote.unsqueeze(1).broadcast_to([128, NOV, NE]), in1=bc(uvec, [128, NOV, NE]),
                                              op=ALU.is_le), reads=["ote", "cvec", "cmp"], writes=["cmp"])
        P.op("dve", lambda e: e.tensor_reduce(out=eov, in_=cmpu, axis=AX.X, op=ALU.add), reads=["cmp"], writes=["eov"])
        gf = acc[:, 7, 0:NOV]
        P.op("dve", lambda e: e.tensor_scalar(out=gf, in0=eov, scalar1=128.0, scalar2=None, op0=ALU.mult),
             reads=["eov"], writes=["gf"])
        P.op("dve", lambda e: e.tensor_scalar(out=gf, in0=gf, scalar1=pcol, scalar2=None, op0=ALU.add),
             reads=["gf", "cvec"], writes=["gf"])
        P.op("dve", lambda e: e.tensor_copy(out=gidx[:], in_=gf), reads=["gf"], writes=["gidx"])
        if stage == 3:
            dump(idx[:].rearrange("p k b -> p (k b)").bitcast(F32), 32, ["idx"])
            dump(w12[:].rearrange("p k b -> p (k b)"), 32, ["w1", "w2"])
            dump(acc[:].rearrange("p k f -> p (k f)"), 256, ["eov", "ote", "ots", "dlt", ckey, "ont"])
        P.barrier(dma=(stage == 3))
    if stage == 3:
        P.finish()
        P.emit()
        return nc

    NW = 4
    with contextlib.ExitStack() as _es:
        wall = _es.enter_context(nc.sbuf_tensor("s_wall", [128, NW, 3, 2048], BF16))
        xg = _es.enter_context(nc.sbuf_tensor("s_xg", [128, 3, 2, D], BF16))
        xgT = _es.enter_context(nc.sbuf_tensor("s_xgT", [128, 2, 8, CAP], BF16))
        sgt = _es.enter_context(nc.sbuf_tensor("s_sgt", [128, 2, CAP], BF16))
        hdT = _es.enter_context(nc.sbuf_tensor("s_hdT", [128, 2, 2, CAP], BF16))
        ogt = _es.enter_context(nc.sbuf_tensor("s_ogt", [128, 2, 2, D], BF16))
        n_ov = NOV if os.environ.get("K_NOV") is None else int(os.environ["K_NOV"])
        _regs = {}

        def breg(e, val):
            if val not in _regs:
                _regs[val] = e.to_reg(val)
            return _regs[val]

        tiles = [(ex * CAP, 2, ex, None) for ex in range(NE)] + [(OVB + u * 128, 1, None, u) for u in range(n_ov)]
        NT = len(tiles)
        mgkeys = [f"mgs{b}{k}" for b in range(NB) for k in range(2)]
        okeys = [f"og{i}" for i in range(NT)]

        def st_weights(i):
            row0, nj, ex, u = tiles[i]
            ws = i % NW
            dst = wall[:, ws].rearrange("p k f -> p (k f)")
            if ex is not None:
                P.dma("pool", lambda e: e.dma_start(out=dst, in_=wbf_d[ex * 128:(ex + 1) * 128, :]),
                      reads=[f"wbf{ex}_{k}" for k in range(3)], writes=[f"w{ws}"])
            else:
                P.dma("pool", lambda e: e.indirect_dma_start(
                    out=dst, out_offset=None, in_=wbf_d[:, :], in_offset=bass.IndirectOffsetOnAxis(ap=gidx[:, u:u + 1], axis=0),
                    bounds_check=breg(e, NE * 128 - 1), oob_is_err=False), reads=["gidx"], writes=[f"w{ws}"])

        def st_load(i):
            row0, nj, ex, u = tiles[i]
            s3 = i % 3
            P.dma("sp", lambda e: e.dma_start(out=xg[:, s3, 0:nj, :],
                                              in_=mg_d[row0:row0 + nj * 128, :].rearrange("(j p) d -> p j d", p=128)),
                  reads=mgkeys, writes=[f"xg{s3}"])

        def st_T(i):
            row0, nj, ex, u = tiles[i]
            s3, sl = i % 3, i % 2
            for j in range(nj):
                TB = bank[j][:].bitcast(BF16)
                for c in range(8):
                    P.op("pe", lambda e, j=j, c=c, TB=TB: e.transpose(TB[:, c * 128:(c + 1) * 128],
                                                                      xg[:, s3, j, c * 128:(c + 1) * 128], idb[:]),
                         reads=[f"xg{s3}", "idb"], writes=[bkey[j]])
                P.op("dve", lambda e, j=j, TB=TB: e.tensor_tensor(out=xgT[:, sl, :, j * 128:(j + 1) * 128],
                                                                  in0=TB.rearrange("p (c t) -> p c t", c=8),
                                                                  in1=bc(gffn, [128, 8, 128]), op=ALU.mult),
                     reads=[bkey[j], "gvec"], writes=[f"xgT{sl}"])

        def st_GU(i):
            row0, nj, ex, u = tiles[i]
            sl, ws, ns = i % 2, i % NW, nj * 128
            for fc in range(2):
                gb = bank[2 + fc]
                for half in range(2):
                    for c in range(8):
                        P.op("pe", lambda e, c=c, gb=gb, fc=fc, half=half: e.matmul(
                            gb[:, half * CAP:half * CAP + ns], lhsT=wall[:, ws, half, c * 256 + fc * 128:c * 256 + (fc + 1) * 128],
                            rhs=xgT[:, sl, c, 0:ns], start=(c == 0), stop=(c == 7)),
                             reads=[f"w{ws}", f"xgT{sl}"], writes=[bkey[2 + fc]])
                P.op("act", lambda e, fc=fc, gb=gb: e.activation(out=sgt[:, fc, 0:ns], in_=gb[:, 0:ns], func=AF.Silu),
                     reads=[bkey[2 + fc]], writes=[f"sgt{fc}"])
                P.op("dve", lambda e, fc=fc, gb=gb: e.tensor_tensor(out=hdT[:, sl, fc, 0:ns], in0=gb[:, CAP:CAP + ns],
                                                                    in1=sgt[:, fc, 0:ns], op=ALU.mult),
                     reads=[bkey[2 + fc], f"sgt{fc}"], writes=[f"hdT{sl}{fc}"])

        def st_D(i):
            row0, nj, ex, u = tiles[i]
            sl, ws, ns = i % 2, i % NW, nj * 128
            for j in range(nj):
                for n in range(2):
                    ob = 4 + j * 2 + n
                    for fc in range(2):
                        P.op("pe", lambda e, fc=fc, ob=ob, j=j, n=n: e.matmul(
                            bank[ob][:, :], lhsT=hdT[:, sl, fc, j * 128:(j + 1) * 128],
                            rhs=wall[:, ws, 2, fc * D + n * 512:fc * D + (n + 1) * 512], start=(fc == 0), stop=(fc == 1)),
                             reads=[f"hdT{sl}{fc}", f"w{ws}"], writes=[bkey[ob]])
                    if n == 0:
                        P.op("act", lambda e, ob=ob, j=j, n=n: e.activation(out=ogt[:, sl, j, n * 512:(n + 1) * 512], in_=bank[ob][:, :],
                                                                           func=AF.Copy), reads=[bkey[ob]], writes=[f"ogt{sl}{j}{n}"])
                    else:
                        P.op("dve", lambda e, ob=ob, j=j, n=n: e.tensor_copy(out=ogt[:, sl, j, n * 512:(n + 1) * 512], in_=bank[ob][:, :]),
                             reads=[bkey[ob]], writes=[f"ogt{sl}{j}{n}"])
            P.dma("sp", lambda e: e.dma_start(out=og_d[row0:row0 + ns, :].rearrange("(j p) d -> p j d", p=128), in_=ogt[:, sl, 0:nj, :]),
                  reads=[f"ogt{sl}{j}{n}" for j in range(nj) for n in range(2)], writes=[okeys[i]])

        for i in range(min(3, NT)):
            st_weights(i)
        for b in range(NB):
            for k in range(2):
                P.dma("pool", lambda e, b=b, k=k: e.indirect_dma_start(
                    out=mg_d[:, :], out_offset=bass.IndirectOffsetOnAxis(ap=idx[:, k, b:b + 1], axis=0),
                    in_=hnb[:, b, :], in_offset=None), reads=["idx"], writes=[f"mgs{b}{k}"])
        st_load(0)
        st_load(1)
        st_T(0)
        for i in range(NT + 1):
            if i + 2 < NT:
                st_load(i + 2)
            if i + 1 < NT:
                st_T(i + 1)
            if i - 1 >= 0:
                st_D(i - 1)
            if i + 3 < NT:
                st_weights(i + 3)
            if i < NT:
                st_GU(i)
        P.barrier()

    with contextlib.ExitStack() as _es:
        wpg = _es.enter_context(nc.sbuf_tensor("s_wpg", [128, 8, D], BF16))
        wpp = _es.enter_context(nc.sbuf_tensor("s_wpp", [128, 2, D], BF16))
        rows = _es.enter_context(nc.sbuf_tensor("s_rows", [128, 3, D], F32))
        pb = _es.enter_context(nc.sbuf_tensor("s_pb", [128, 2, 256], BF16))
        pT = _es.enter_context(nc.sbuf_tensor("s_pT", [128, 2, 128], BF16))
        pp = _es.enter_context(nc.sbuf_tensor("s_pp", [128, 2, D], F32))
        hb = _es.enter_context(nc.sbuf_tensor("s_hb", [128, D], BF16))
        hT = _es.enter_context(nc.sbuf_tensor("s_hT", [128, 2, 8, 128], BF16))
        gt = _es.enter_context(nc.sbuf_tensor("s_gt", [128, D], F32))
        ot = _es.enter_context(nc.sbuf_tensor("s_ot", [128, 2, D], F32))
        ogc = _es.enter_context(nc.sbuf_tensor("s_ogc", [128, 2, 2, D], BF16))
        P.dma("pool", lambda e: e.dma_start(out=wpp[:], in_=wpp_d.rearrange("(c p) f -> p c f", p=128)), writes=["wpp"])
        P.dma("pool", lambda e: e.dma_start(out=wpg[:], in_=wpg_d.rearrange("(c p) f -> p c f", p=128)), writes=["wpg"])
        for i in range(3):
            P.dma("sp", lambda e, i=i: e.dma_start(out=rows[:, i, :], in_=rows_d[i:i + 1, :].broadcast_to([128, D])),
                  writes=["rows"])

        def s0a(b):
            sl = b % 2
            P.dma("pool", lambda e: e.dma_start(out=pb[:, sl, :], in_=pin[b * 128:(b + 1) * 128, :]), writes=[f"pb{sl}"])
            for k in range(2):
                P.dma("pool", lambda e, k=k: e.indirect_dma_start(
                    out=ogc[:, sl, k, :], out_offset=None, in_=og_d[:, :],
                    in_offset=bass.IndirectOffsetOnAxis(ap=idx[:, k, b:b + 1], axis=0)), writes=[f"ogc{sl}{k}"])

        def s0b(b):
            sl = b % 2
            for k in range(2):
                P.op("dve", lambda e, k=k: e.scalar_tensor_tensor(
                    out=h[:, b, :], in0=ogc[:, sl, k, :], scalar=w12[:, k, b:b + 1], in1=h[:, b, :], op0=ALU.mult, op1=ALU.add),
                     reads=[f"ogc{sl}{k}", f"h{b}"], writes=[f"h{b}"])

        def s1(b):
            sl = b % 2
            T0 = bank[0][:].bitcast(BF16)
            for k in range(2):
                P.op("pe", lambda e, k=k: e.transpose(T0[:, k * 128:(k + 1) * 128], pb[:, sl, k * 128:(k + 1) * 128], idb[:]),
                     reads=[f"pb{sl}", "idb"], writes=[bkey[0]])
            P.op("act", lambda e: e.activation(out=pT[:], in_=T0[:, 0:256].rearrange("p (k t) -> p k t", k=2), func=AF.Copy),
                 reads=[bkey[0]], writes=["pT"])
            P.op("act", lambda e: e.activation(out=hb[:], in_=h[:, b, :], func=AF.Copy), reads=[f"h{b}"], writes=["hb"])
            for n in range(2):
                for k in range(2):
                    P.op("pe", lambda e, k=k, n=n: e.matmul(bank[1 + n][:, :], lhsT=pT[:, k, :], rhs=wpp[:, k, n * 512:(n + 1) * 512],
                                                            start=(k == 0), stop=(k == 1)),
                         reads=["pT", "wpp"], writes=[bkey[1 + n]])
            T3 = bank[3][:].bitcast(BF16)
            for c in range(8):
                P.op("pe", lambda e, c=c: e.transpose(T3[:, c * 128:(c + 1) * 128], hb[:, c * 128:(c + 1) * 128], idb[:]),
                     reads=["hb", "idb"], writes=[bkey[3]])
            for n in range(2):
                P.op("act", lambda e, n=n: e.activation(out=pp[:, sl, n * 512:(n + 1) * 512], in_=bank[1 + n][:, :], func=AF.Copy),
                     reads=[bkey[1 + n]], writes=[f"pp{sl}"])
            P.op("act", lambda e: e.activation(out=hT[:, sl], in_=T3.rearrange("p (c t) -> p c t", c=8), func=AF.Copy),
                 reads=[bkey[3]], writes=[f"hT{sl}"])

        def s1b(b):
            sl = b % 2
            pk = f"ssp{b}"
            P.op("act", lambda e: e.activation(out=junk[:], in_=pp[:, sl, :], func=AF.Square, accum_out=stat[:, SS_P + b:SS_P + b + 1]),
                 reads=[f"pp{sl}"], writes=["junk", pk])
            rstd_from_ss(SS_P + b, D, pk)

        def s2a(b):
            sl = b % 2
            pk = f"ssp{b}"
            for n in range(2):
                for c in range(8):
                    P.op("pe", lambda e, c=c, n=n: e.matmul(bank[4 + n][:, :], lhsT=hT[:, sl, c, :], rhs=wpg[:, c, n * 512:(n + 1) * 512],
                                                            start=(c == 0), stop=(c == 7)),
                         reads=[f"hT{sl}", "wpg"], writes=[bkey[4 + n]])
            for n in range(2):
                P.op("dve", lambda e, n=n: e.tensor_tensor(out=gt[:, n * 512:(n + 1) * 512], in0=bank[4 + n][:, :],
                                                           in1=rows[:, 0, n * 512:(n + 1) * 512], op=ALU.add),
                     reads=[bkey[4 + n], "rows"], writes=["gt"])
            P.op("act", lambda e: e.activation(out=gt[:], in_=gt[:], func=AF.Sigmoid), reads=["gt"], writes=["gt"])
            P.op("dve", lambda e: e.scalar_tensor_tensor(out=pp[:, sl, :], in0=pp[:, sl, :], scalar=stat[:, SS_P + b:SS_P + b + 1],
                                                         in1=rows[:, 1, :], op0=ALU.mult, op1=ALU.mult),
                 reads=[f"pp{sl}", pk, "rows"], writes=[f"pp{sl}"])
            P.op("dve", lambda e: e.tensor_tensor(out=pp[:, sl, :], in0=pp[:, sl, :], in1=gt[:], op=ALU.mult),
                 reads=[f"pp{sl}", "gt"], writes=[f"pp{sl}"])
            P.op("pool", lambda e: e.tensor_tensor(out=h[:, b, :], in0=h[:, b, :], in1=pp[:, sl, :], op=ALU.add),
                 reads=[f"pp{sl}", f"h{b}"], writes=[f"h{b}"])

        def s2b(b):
            fk = f"ssf{b}"
            P.op("act", lambda e: e.activation(out=junk[:], in_=h[:, b, :], func=AF.Square, accum_out=stat[:, SS_F + b:SS_F + b + 1]),
                 reads=[f"h{b}"], writes=["junk", fk])
            rstd_from_ss(SS_F + b, D, fk)

        def s2c(b):
            sl = b % 2
            fk = f"ssf{b}"
            P.op("dve", lambda e: e.scalar_tensor_tensor(out=ot[:, sl, :], in0=h[:, b, :], scalar=stat[:, SS_F + b:SS_F + b + 1],
                                                         in1=rows[:, 2, :], op0=ALU.mult, op1=ALU.mult),
                 reads=[f"h{b}", fk, "rows"], writes=[f"ot{sl}"])
            P.dma("sp", lambda e: e.dma_start(out=out_d[b * 128:(b + 1) * 128, :], in_=ot[:, sl, :]),
                  reads=[f"ot{sl}"], is_out=True)

        s0a(0)
        s0a(1)
        s0b(0)
        s0b(1)
        s1(0)
        s0a(2)
        s1b(0)
        for i in range(NB + 2):
            if i + 2 < NB:
                s0b(i + 2)
            if i + 1 < NB:
                s1(i + 1)
            if i + 3 < NB:
                s0a(i + 3)
            if i < NB:
                s2a(i)
            if 0 <= i - 2 < NB:
                s2c(i - 2)
            if 0 <= i - 1 < NB:
                s2b(i - 1)
            if i + 1 < NB:
                s1b(i + 1)
    P.finish()
    P.emit()
    return nc


def _t5_bucket(rel):
    n = 16
    max_exact = 8
    ret = np.where(rel > 0, n, 0)
    a = np.abs(rel)
    af = np.maximum(a, 1).astype(np.float32)
    large = max_exact + (np.log(af / np.float32(max_exact)) / np.float32(np.log(128 / 8)) * np.float32(n - max_exact)).astype(np.int32)
    large = np.minimum(large, n - 1)
    return ret + np.where(a < max_exact, a, large)


def _static_tables():
    j = np.arange(128)[:, None, None]
    kb = np.arange(3)[None, :, None]
    q = np.arange(128)[None, None, :]
    rel = (kb - 1) * 128 + j - q
    bucket = _t5_bucket(rel)
    valid = (np.abs(rel) <= 128).astype(np.float32)
    return bucket, valid


def _w_in_layout(w):
    w = w.copy()
    w[:, 1024:1536] = w[:, 1024:1536].reshape(D, 2, 4, 64).transpose(0, 2, 1, 3).reshape(D, 512)
    return np.ascontiguousarray(w)


def _expert_layout(wg, wu, wd):
    out = np.empty((NE, 128, 3, 2048), np.float32)
    out[:, :, 0, :] = wg.reshape(NE, 8, 128, 256).transpose(0, 2, 1, 3).reshape(NE, 128, 2048)
    out[:, :, 1, :] = wu.reshape(NE, 8, 128, 256).transpose(0, 2, 1, 3).reshape(NE, 128, 2048)
    out[:, :, 2, :] = wd.reshape(NE, 2, 128, D).transpose(0, 2, 1, 3).reshape(NE, 128, 2048)
    return out.reshape(NE * 128, 3, 2048)


def _tri_const():
    t = np.zeros((128, 256), np.float32)
    t[:, 0:128] = (np.arange(128)[:, None] < np.arange(128)[None, :]).astype(np.float32)
    t[:, 128:256] = 1.0
    return t


def _cvec_const():
    c = np.zeros((128, 128), np.float32)
    c[:, 0:32] = np.arange(32)[None, :] * CAP
    c[:, 32:46] = CAP + 128 * np.arange(14)[None, :]
    c[:, 46:77] = np.arange(31)[None, :]
    c[:, 77] = np.arange(128)
    c[:, 78:110] = 1.0
    return c


def make_in_maps(inp):
    f = lambda a: np.ascontiguousarray(np.asarray(a, dtype=np.float32))
    x = f(inp["x"])
    p = f(inp["p"])[0]
    bucket, valid = _static_tables()
    rel_bias = f(inp["rel_bias"])
    bg = rel_bias[bucket]
    biasg = np.ascontiguousarray(bg.transpose(0, 1, 3, 2)).reshape(128, 3072)
    maskc = np.ascontiguousarray(np.broadcast_to(valid[:, :, None, :], (128, 3, 8, 128))).reshape(128, 3072)
    colmajor = lambda v: np.ascontiguousarray(f(v).reshape(8, 128).T)
    gvec = np.concatenate([colmajor(inp["g_mix"][0]), colmajor(inp["g_out_grp"][0]), colmajor(inp["g_ffn"][0])], axis=1)
    shared = {
        "biasg": biasg, "maskc": maskc, "ident": np.eye(128, dtype=np.float32),
        "w_in": _w_in_layout(f(inp["w_in"][0])), "gvec": np.ascontiguousarray(gvec),
        "lnv": np.ascontiguousarray(np.stack([f(inp["ln_v_g"][0]), f(inp["ln_v_b"][0])])),
        "wsT": np.ascontiguousarray(f(inp["w_spatial"][0]).transpose(2, 0, 1)).reshape(128, 512),
        "bsT": np.ascontiguousarray(f(inp["b_spatial"][0]).T),
        "sink": f(inp["sink"]), "w_out": f(inp["w_out"][0]),
        "wr": np.ascontiguousarray(np.concatenate([f(inp["w_router_group"][0]), f(inp["w_router_expert"][0])], axis=1)),
        "br": np.ascontiguousarray(np.concatenate([f(inp["b_router_group"][0]), f(inp["b_router_expert"][0])])[None, :]),
        "w_exp": _expert_layout(f(inp["w_gate_e"][0]), f(inp["w_up_e"][0]), f(inp["w_down_e"][0])),
        "w_ple_proj": f(inp["w_ple_proj"][0]), "w_ple_gate": f(inp["w_ple_gate"][0]),
        "rows": np.ascontiguousarray(np.stack([f(inp["b_ple_gate"][0]), f(inp["g_ple"][0]), f(inp["g_final"])])),
        "tri": _tri_const(), "cvec": _cvec_const(),
    }
    maps = []
    cps = NCORES // BATCH
    for core in range(NCORES):
        bidx, ci = divmod(core, cps)
        t0 = ci * TPC
        xh = np.zeros((NBH * 128, D), np.float32)
        lo, hi = t0 - 128, t0 + TPC + 128
        slo, shi = max(lo, 0), min(hi, SEQ)
        xh[slo - lo:shi - lo] = x[bidx, slo:shi]
        flags = np.zeros((128, 2), np.float32)
        flags[:, 0] = 1.0 if lo >= 0 else 0.0
        flags[:, 1] = 1.0 if hi <= SEQ else 0.0
        m = dict(shared)
        m["xh"] = xh
        m["p"] = np.ascontiguousarray(p[bidx, t0:t0 + TPC])
        m["flags"] = flags
        maps.append(m)
    return maps


_NC_CACHE = {}


def kernel(**inputs):
    if "nc" not in _NC_CACHE:
        _NC_CACHE["nc"] = build_program()
    nc = _NC_CACHE["nc"]
    maps = make_in_maps(inputs)
    res = run_bass_kernel_spmd(nc, maps, core_ids=list(range(NCORES)))
    cps = NCORES // BATCH
    out = np.empty((BATCH, SEQ, D), np.float32)
    for core in range(NCORES):
        bidx, ci = divmod(core, cps)
        out[bidx, ci * TPC:(ci + 1) * TPC] = res.results[core]["out"]
    return out
```

```python
import contextlib
import os
import numpy as np
import concourse.bass as bass
import concourse.mybir as mybir
from concourse.bass_utils import run_bass_kernel_spmd

F32 = mybir.dt.float32
BF16 = mybir.dt.bfloat16
AF = mybir.ActivationFunctionType
ALU = mybir.AluOpType
AX = mybir.AxisListType

NCORES = 8
D = 1024
SEQ = 8192
BATCH = 2
TPC = 2048
NB = 16
NBH = 18
D_IN = 1792
NE = 32
EPS = 1e-6
BIG = 1.0e30
CAP = 256
NOV = 31
OVB = NE * CAP
NSLOT = OVB + NOV * 128
I32 = mybir.dt.int32

ENGS = ("pe", "act", "dve", "pool", "sp")
EPOCH = 8192
NDMASEM = 24
NDMASEM_Q = {"pool": 72}


class Prog:
    def __init__(self, nc):
        self.nc = nc
        self.streams = {e: [] for e in ENGS}
        self.cnt = {e: 0 for e in ENGS}
        self.last_w = {}
        self.readers = {}
        self.waited = {}
        self.dma_known = {e: set() for e in ENGS}
        self.sems = {}
        self.dsems = {}
        self.dma_rr = {"sp": 0, "pool": 0, "act": 0}
        self.ndma = 0
        self.out_dmas = []
        self.bank_acc = {}

    def _sem(self, eng, epoch):
        k = (eng, epoch)
        if k not in self.sems:
            self.sems[k] = self.nc.alloc_semaphore(f"s_{eng}_{epoch}")
        return self.sems[k]

    def _semval(self, eng, seq):
        return (self._sem(eng, (seq - 1) // EPOCH), (seq - 1) % EPOCH + 1)

    def _dep_waits(self, eng, reads, writes, strict=False):
        deps = []
        for k in reads:
            r = self.last_w.get(k)
            if r is not None:
                deps.append((r, True))
        for k in writes:
            r = self.last_w.get(k)
            if r is not None:
                deps.append((r, False))
            for r in self.readers.get(k, {}).values():
                deps.append((r, False))
        for k in list(reads) + list(writes):
            if k.startswith("bank"):
                r = self.bank_acc.get(k)
                if r is not None and r[1] != eng:
                    deps.append((r, False))
        waits = []
        for r, raw in deps:
            if r[0] == "dma":
                _, did, sem, val = r
                if did in self.dma_known[eng]:
                    continue
                self.dma_known[eng].add(did)
                waits.append((sem, val))
            else:
                _, peng, seq = r
                if peng == eng and not strict and eng == "pe":
                    continue
                if self.waited.get((eng, peng), 0) >= seq:
                    continue
                self.waited[(eng, peng)] = seq
                waits.append(self._semval(peng, seq))
        return waits

    def _commit(self, ref, rkey, reads, writes):
        for k in writes:
            self.last_w[k] = ref
            self.readers[k] = {}
        for k in reads:
            self.readers.setdefault(k, {})[rkey] = ref

    op_limit = None

    def op(self, eng, fn, reads=(), writes=()):
        if self.op_limit is not None:
            if self.op_limit <= 0:
                return None
            self.op_limit -= 1
        waits = self._dep_waits(eng, reads, writes)
        self.cnt[eng] += 1
        seq = self.cnt[eng]
        ref = ("eng", eng, seq)
        self._commit(ref, eng, reads, writes)
        for k in list(reads) + list(writes):
            if k.startswith("bank"):
                self.bank_acc[k] = ref
        self.streams[eng].append((waits, fn, self._semval(eng, seq)[0], 1))
        return ref

    def dma(self, queue, fn, reads=(), writes=(), is_out=False):
        waits = self._dep_waits(queue, reads, writes, strict=True)
        nsem = NDMASEM_Q.get(queue, NDMASEM)
        idx = self.dma_rr[queue]
        self.dma_rr[queue] = (idx + 1) % nsem
        k = (queue, idx)
        if k not in self.dsems:
            self.dsems[k] = [self.nc.alloc_semaphore(f"d_{queue}_{idx}"), 0, None]
        ent = self.dsems[k]
        if ent[2] is not None and ent[2] not in self.dma_known[queue]:
            waits.append((ent[0], ent[1] * 16))
            self.dma_known[queue].add(ent[2])
        ent[1] += 1
        self.ndma += 1
        did = self.ndma
        ent[2] = did
        ref = ("dma", did, ent[0], ent[1] * 16)
        self._commit(ref, ("d", did), reads, writes)
        self.streams[queue].append((waits, fn, ent[0], 16))
        if is_out:
            self.out_dmas.append(ref)
        return ref

    def barrier(self, dma=True):
        for eng in ENGS:
            waits = []
            for peng in ENGS:
                if (peng != eng or eng in ("act", "dve", "pool")) and self.cnt[peng] > 0 and self.waited.get((eng, peng), 0) < self.cnt[peng]:
                    self.waited[(eng, peng)] = self.cnt[peng]
                    waits.append(self._semval(peng, self.cnt[peng]))
            for (q, idx), ent in (self.dsems.items() if dma else ()):
                if ent[2] is not None and ent[2] not in self.dma_known[eng]:
                    self.dma_known[eng].add(ent[2])
                    waits.append((ent[0], ent[1] * 16))
            self.streams[eng].append((waits, None, None, 0))
        self.last_w = {} if dma else {k: r for k, r in self.last_w.items() if r[0] == "dma"}
        self.readers = {}
        self.bank_acc = {}

    def finish(self):
        waits = []
        for r in self.out_dmas:
            if r[1] not in self.dma_known["sp"]:
                self.dma_known["sp"].add(r[1])
                waits.append((r[2], r[3]))
        self.streams["sp"].append((waits, None, None, 0))

    def emit(self):
        nc = self.nc
        streams = self.streams

        def run(name, eng):
            for waits, fn, sem, inc in streams[name]:
                for s, v in waits:
                    eng.wait_ge(s, v)
                if fn is None:
                    continue
                fn(eng).then_inc(sem, inc)

        with nc.Block() as block:
            @block.sync
            def _(e):
                run("sp", e)

            @block.tensor
            def _(e):
                run("pe", e)

            @block.scalar
            def _(e):
                run("act", e)

            @block.vector
            def _(e):
                run("dve", e)

            @block.gpsimd
            def _(e):
                run("pool", e)


def bc(ap, shape):
    return ap.unsqueeze(len(ap.shape)).broadcast_to(list(shape))


def build_program(stage=99, ndbg=0, nblk=None):
    nc = bass.Bass("TRN2", target_bir_lowering=False)

    def din(name, shape):
        return nc.dram_tensor(name, list(shape), F32, kind="ExternalInput").ap()

    xh = din("xh", [NBH * 128, D])
    pin = din("p", [TPC, 256])
    flags_d = din("flags", [128, 2])
    biasg_d = din("biasg", [128, 3072])
    maskc_d = din("maskc", [128, 3072])
    ident_d = din("ident", [128, 128])
    w_in_d = din("w_in", [D, D_IN])
    gvec_d = din("gvec", [128, 24])
    lnv_d = din("lnv", [2, 512])
    wsT_d = din("wsT", [128, 512])
    bsT_d = din("bsT", [128, 4])
    sink_d = din("sink", [1, 8])
    w_out_d = din("w_out", [D, D])
    wr_d = din("wr", [D, 36])
    br_d = din("br", [1, 36])
    wall_d = din("w_exp", [NE * 128, 3, 2048])
    wpp_d = din("w_ple_proj", [256, D])
    wpg_d = din("w_ple_gate", [D, D])
    rows_d = din("rows", [3, D])
    tri_d = din("tri", [128, 256])
    cvec_d = din("cvec", [128, 128])
    mg_d = nc.dram_tensor("mg", [NSLOT, D], BF16, kind="Internal").ap()
    og_d = nc.dram_tensor("og", [NSLOT, D], BF16, kind="Internal").ap()
    wbf_d = nc.dram_tensor("wbf", [NE * 128, 6144], BF16, kind="Internal").ap()
    out_d = nc.dram_tensor("out", [TPC, D], F32, kind="ExternalOutput").ap()
    dbg_d = None
    if ndbg:
        dbg_d = nc.dram_tensor("dbg", [128, ndbg], F32, kind="ExternalOutput").ap()

    P = Prog(nc)
    def sb(name, shape, dt):
        return nc.alloc_sbuf_tensor("s_" + name, shape, dt)
    bank = [nc.alloc_psum_tensor(f"bank{i}", [128, 512], F32) for i in range(8)]
    bkey = [f"bank{i}" for i in range(8)]

    h = sb("h", [128, NB, D], F32)
    hnb = sb("hnb", [128, NB, D], BF16)
    w12 = sb("w12", [128, 2, NB], F32)
    idx = sb("idx", [128, 2, NB], I32)
    trib = sb("trib", [128, 256], BF16)
    cvec = sb("cvec", [128, 128], F32)
    gidx = sb("gidx", [128, NOV], I32)
    idb = sb("idb", [128, 128], BF16)
    idf = sb("idf", [128, 128], F32)
    gvec = sb("gvec", [128, 24], F32)
    cst_ = sb("cst", [128, 4], F32)
    stat = sb("stat", [128, 160], F32)
    junk = sb("junk", [128, D], BF16)
    L = sb("L", [128, NB, 36], F32)
    flags = sb("flags", [128, 2], F32)

    dbg_col = [0]

    def dump(ap_sb, ncols, keys, cast=False):
        c0 = dbg_col[0]
        dbg_col[0] += ncols
        assert dbg_col[0] <= ndbg, dbg_col[0]
        q = "pool" if cast else "sp"
        P.dma(q, lambda e: e.dma_start(out=dbg_d[:, c0:c0 + ncols], in_=ap_sb), reads=keys, is_out=True)
        return c0

    N_LATE = 8
    conv_todo = [(ex, k) for ex in range(NE - N_LATE) for k in range(3)]
    conv_late = [(ex, k) for ex in range(NE - N_LATE, NE) for k in range(3)]

    def conv_step(n):
        for _ in range(n):
            if not conv_todo:
                return
            ex, k = conv_todo.pop(0)
            P.dma("pool", lambda e, ex=ex, k=k: e.dma_start(out=wbf_d[ex * 128:(ex + 1) * 128, k * 2048:(k + 1) * 2048],
                                                            in_=wall_d[ex * 128:(ex + 1) * 128, k, :]), writes=[f"wbf{ex}_{k}"])

    def rstd_from_ss(col, n, key):
        P.op("dve", lambda e: e.tensor_scalar(out=stat[:, col:col + 1], in0=stat[:, col:col + 1], scalar1=1.0 / n,
                                              scalar2=EPS, op0=ALU.mult, op1=ALU.add), reads=[key], writes=[key])
        P.op("pool", lambda e: e.tensor_tensor(out=stat[:, col:col + 1], in0=stat[:, col:col + 1], in1=cst_[:, 0:1],
                                               op=ALU.pow), reads=[key, "cst"], writes=[key])

    P.dma("sp", lambda e: e.dma_start(out=idf[:], in_=ident_d[:, :]), writes=["idf"])
    P.dma("pool", lambda e: e.dma_start(out=idb[:], in_=ident_d[:, :]), writes=["idb"])
    P.dma("sp", lambda e: e.dma_start(out=gvec[:], in_=gvec_d[:, :]), writes=["gvec"])
    P.dma("sp", lambda e: e.dma_start(out=flags[:], in_=flags_d[:, :]), writes=["flags"])
    P.op("dve", lambda e: e.memset(cst_[:, 0:1], -0.5), writes=["cst"])
    P.dma("pool", lambda e: e.dma_start(out=trib[:], in_=tri_d[:, :]), writes=["trib"])
    P.dma("sp", lambda e: e.dma_start(out=cvec[:], in_=cvec_d[:, :]), writes=["cvec"])
    gmix = gvec[:, 0:8]
    gout = gvec[:, 8:16]
    gffn = gvec[:, 16:24]

    SS_X, SS_A, SS_B, SS_M, SS_V, SS_P, SS_F = 0, 18, 34, 50, 66, 98, 114

    with contextlib.ExitStack() as _es:
        yna = _es.enter_context(nc.sbuf_tensor("s_yna", [128, NB, 512], BF16))
        qT = _es.enter_context(nc.sbuf_tensor("s_qT", [128, 4, TPC], BF16))
        kT = _es.enter_context(nc.sbuf_tensor("s_kT", [128, NBH * 128], BF16))
        vaug = _es.enter_context(nc.sbuf_tensor("s_vaug", [128, NBH, 2, 66], BF16))
        expB = _es.enter_context(nc.sbuf_tensor("s_expB", [128, 3072], BF16))
        esink = _es.enter_context(nc.sbuf_tensor("s_esink", [128, 8], F32))

        with contextlib.ExitStack() as _es:
            w_in_bf = _es.enter_context(nc.sbuf_tensor("s_w_in_bf", [128, 8, D_IN], BF16))
            xn = _es.enter_context(nc.sbuf_tensor("s_xn", [128, 2, D], BF16))
            aT = _es.enter_context(nc.sbuf_tensor("s_aT", [128, 2, 8, 128], BF16))
            uv = _es.enter_context(nc.sbuf_tensor("s_uv", [128, 2, 2, 512], F32))
            vc = _es.enter_context(nc.sbuf_tensor("s_vc", [128, 512], F32))
            vn = _es.enter_context(nc.sbuf_tensor("s_vn", [128, 512], BF16))
            ya = _es.enter_context(nc.sbuf_tensor("s_ya", [128, 512], F32))
            lnv = _es.enter_context(nc.sbuf_tensor("s_lnv", [128, 2, 512], F32))
            wsT = _es.enter_context(nc.sbuf_tensor("s_wsT", [128, 512], BF16))
            bsT = _es.enter_context(nc.sbuf_tensor("s_bsT", [128, 4], F32))
            mTf32 = hnb[:].rearrange("p c t -> p (c t)").bitcast(F32)
            btmp = mTf32[:, 0:3072]
            mtmp = mTf32[:, 3072:6144]
            xhalo = mTf32[:, 6144:8192].rearrange("p (s d) -> p s d", s=2)
            for c in range(8):
                P.dma("pool", lambda e, c=c: e.dma_start(out=w_in_bf[:, c, :], in_=w_in_d[c * 128:(c + 1) * 128, :]),
                      writes=[f"w_in{c}"])
            P.dma("pool", lambda e: e.dma_start(out=wsT[:], in_=wsT_d[:, :]), writes=["wsT"])
            P.dma("sp", lambda e: e.dma_start(out=bsT[:], in_=bsT_d[:, :]), writes=["bsT"])
            for i in range(2):
                P.dma("sp", lambda e, i=i: e.dma_start(out=lnv[:, i, :], in_=lnv_d[i:i + 1, :].broadcast_to([128, 512])),
                      writes=["lnv"])
            P.dma("sp", lambda e: e.dma_start(out=esink[:], in_=sink_d[0:1, :].broadcast_to([128, 8])), writes=["esink"])
            P.op("act", lambda e: e.activation(out=esink[:], in_=esink[:], func=AF.Exp), reads=["esink"], writes=["esink"])
            P.dma("sp", lambda e: e.dma_start(out=btmp, in_=biasg_d[:, :]), writes=["btmp"])
            P.dma("sp", lambda e: e.dma_start(out=mtmp, in_=maskc_d[:, :]), writes=["mtmp"])
            P.op("act", lambda e: e.activation(out=btmp, in_=btmp, func=AF.Exp), reads=["btmp"], writes=["btmp"])
            P.op("dve", lambda e: e.tensor_tensor(out=expB[:], in0=btmp, in1=mtmp, op=ALU.mult),
                 reads=["btmp", "mtmp"], writes=["expB"])
            P.op("dve", lambda e: e.memset(vaug[:, :, :, 64:65], 1.0), writes=["vones"])

            order = [0, NBH - 1] + list(range(1, NBH - 1))
            if nblk is not None:
                order = order[:nblk]
            wk = [f"w_in{c}" for c in range(8)]

            def blk(it):
                bi = order[it]
                halo = bi in (0, NBH - 1)
                hs = 0 if bi == 0 else 1
                xt = xhalo[:, hs, :] if halo else h[:, bi - 1, :]
                xk = f"xhalo{hs}" if halo else f"h{bi - 1}"
                return bi, halo, hs, xt, xk, it % 2

            def a0(it):
                bi, halo, hs, xt, xk, sl = blk(it)
                P.dma("sp", lambda e: e.dma_start(out=xt, in_=xh[bi * 128:(bi + 1) * 128, :]), writes=[xk])

            def a1(it):
                bi, halo, hs, xt, xk, sl = blk(it)
                sk = f"ssx{bi}"
                P.op("act", lambda e: e.activation(out=junk[:], in_=xt, func=AF.Square, accum_out=stat[:, SS_X + bi:SS_X + bi + 1]),
                     reads=[xk], writes=["junk", sk])
                rstd_from_ss(SS_X + bi, D, sk)

            def a1x(it):
                bi, halo, hs, xt, xk, sl = blk(it)
                P.op("act", lambda e: e.activation(out=xn[:, sl, :], in_=xt, func=AF.Copy, scale=stat[:, SS_X + bi:SS_X + bi + 1]),
                     reads=[xk, f"ssx{bi}"], writes=[f"xn{sl}"])

            def a2(it):
                bi, halo, hs, xt, xk, sl = blk(it)
                TBb = bank[sl][:].bitcast(BF16)
                for c in range(8):
                    P.op("pe", lambda e, c=c: e.transpose(TBb[:, c * 128:(c + 1) * 128], xn[:, sl, c * 128:(c + 1) * 128], idb[:]),
                         reads=[f"xn{sl}", "idb"], writes=[bkey[sl]])
                P.op("dve", lambda e: e.tensor_tensor(out=aT[:, sl], in0=TBb.rearrange("p (c t) -> p c t", c=8),
                                                      in1=bc(gmix, [128, 8, 128]), op=ALU.mult),
                     reads=[bkey[sl], "gvec"], writes=[f"aT{sl}"])

            def bst(it):
                bi, halo, hs, xt, xk, sl = blk(it)
                b = bi - 1
                for c in range(8):
                    P.op("pe", lambda e, c=c: e.matmul(bank[5][:, 0:128], lhsT=w_in_bf[:, c, 1536:1664], rhs=aT[:, sl, c, :],
                                                       start=(c == 0), stop=(c == 7)),
                         reads=[f"aT{sl}", wk[c]], writes=[bkey[5]])
                for c in range(8):
                    P.op("pe", lambda e, c=c: e.matmul(bank[5][:, 128:256], lhsT=aT[:, sl, c, :], rhs=w_in_bf[:, c, 1664:1792],
                                                       start=(c == 0), stop=(c == 7)),
                         reads=[f"aT{sl}", wk[c]], writes=[bkey[5]])
                if not halo:
                    for n, bk in ((0, 2), (1, 3)):
                        for c in range(8):
                            P.op("pe", lambda e, c=c, n=n, bk=bk: e.matmul(bank[bk][:, :], lhsT=aT[:, sl, c, :],
                                                                           rhs=w_in_bf[:, c, n * 512:(n + 1) * 512],
                                                                           start=(c == 0), stop=(c == 7)),
                                 reads=[f"aT{sl}", wk[c]], writes=[bkey[bk]])
                    for qc in range(4):
                        for c in range(8):
                            P.op("pe", lambda e, c=c, qc=qc: e.matmul(bank[4][:, qc * 128:(qc + 1) * 128],
                                                                      lhsT=w_in_bf[:, c, 1024 + qc * 128:1024 + (qc + 1) * 128],
                                                                      rhs=aT[:, sl, c, :], start=(c == 0), stop=(c == 7)),
                                 reads=[f"aT{sl}", wk[c]], writes=[bkey[4]])
                P.op("act", lambda e: e.activation(out=kT[:, bi * 128:(bi + 1) * 128], in_=bank[5][:, 0:128], func=AF.Copy),
                     reads=[bkey[5]], writes=[f"kT{bi}"])
                vsrc = bank[5][:, 128:256].rearrange("p (k d) -> p k d", k=2)
                if halo:
                    P.op("dve", lambda e: e.tensor_scalar(out=vaug[:, bi, :, 0:64], in0=vsrc, scalar1=flags[:, hs:hs + 1], scalar2=None,
                                                          op0=ALU.mult), reads=[bkey[5], "flags"], writes=[f"va{bi}"])
                    P.op("dve", lambda e: e.tensor_copy(out=vaug[:, bi, :, 64:65],
                                                        in_=flags[:, hs:hs + 1].unsqueeze(1).broadcast_to([128, 2, 1])),
                         reads=["flags", "vones"], writes=[f"vo{bi}"])
                    return
                P.op("dve", lambda e: e.tensor_copy(out=vaug[:, bi, :, 0:64], in_=vsrc), reads=[bkey[5]], writes=[f"va{bi}"])
                for n, bk in ((0, 2), (1, 3)):
                    P.op("act", lambda e, n=n, bk=bk: e.activation(out=uv[:, sl, n, :], in_=bank[bk][:, :], func=AF.Gelu_apprx_tanh),
                         reads=[bkey[bk]], writes=[f"uv{sl}{n}"])
                P.op("act", lambda e: e.activation(out=qT[:, :, b * 128:(b + 1) * 128],
                                                   in_=bank[4][:, :].rearrange("p (c t) -> p c t", c=4), func=AF.Copy),
                     reads=[bkey[4]], writes=[f"qT{b}"])

            def cst_ln(it):
                bi, halo, hs, xt, xk, sl = blk(it)
                if halo:
                    return
                b = bi - 1
                vk = f"ssv{b}"
                c6 = SS_V + 2 * b
                P.op("dve", lambda e: e.bn_stats(out=stat[:, 150:156], in_=uv[:, sl, 1, :]), reads=[f"uv{sl}1"], writes=["bn6"])
                P.op("dve", lambda e: e.bn_aggr(out=stat[:, c6:c6 + 2], in_=stat[:, 150:156]), reads=["bn6"], writes=[vk])
                P.op("dve", lambda e: e.tensor_scalar(out=stat[:, c6 + 1:c6 + 2], in0=stat[:, c6 + 1:c6 + 2], scalar1=EPS,
                                                      scalar2=None, op0=ALU.add), reads=[vk], writes=[vk])
                P.op("pool", lambda e: e.tensor_tensor(out=stat[:, c6 + 1:c6 + 2], in0=stat[:, c6 + 1:c6 + 2],
                                                       in1=cst_[:, 0:1], op=ALU.pow), reads=[vk, "cst"], writes=[vk])
                P.op("dve", lambda e: e.tensor_scalar(out=vc[:], in0=uv[:, sl, 1, :], scalar1=stat[:, c6:c6 + 1],
                                                      scalar2=stat[:, c6 + 1:c6 + 2], op0=ALU.subtract, op1=ALU.mult),
                     reads=[f"uv{sl}1", vk], writes=["vc"])
                P.op("dve", lambda e: e.tensor_tensor(out=vc[:], in0=vc[:], in1=lnv[:, 0, :], op=ALU.mult),
                     reads=["vc", "lnv"], writes=["vc"])
                P.op("dve", lambda e: e.tensor_tensor(out=vn[:], in0=vc[:], in1=lnv[:, 1, :], op=ALU.add),
                     reads=["vc", "lnv"], writes=["vn"])

            def cst_sp(it):
                bi, halo, hs, xt, xk, sl = blk(it)
                if halo:
                    return
                b = bi - 1
                for hh in range(4):
                    P.op("pe", lambda e, hh=hh: e.matmul(bank[6][:, hh * 128:(hh + 1) * 128], lhsT=wsT[:, hh * 128:(hh + 1) * 128],
                                                         rhs=vn[:, hh * 128:(hh + 1) * 128], start=True, stop=True),
                         reads=["vn", "wsT"], writes=[bkey[6]])
                for hh in range(4):
                    P.op("dve", lambda e, hh=hh: e.scalar_tensor_tensor(out=ya[:, hh * 128:(hh + 1) * 128],
                                                                        in0=bank[6][:, hh * 128:(hh + 1) * 128],
                                                                        scalar=bsT[:, hh:hh + 1],
                                                                        in1=uv[:, sl, 0, hh * 128:(hh + 1) * 128],
                                                                        op0=ALU.add, op1=ALU.mult),
                         reads=[bkey[6], "bsT", f"uv{sl}0"], writes=["ya"])
                ak = f"ssa{b}"
                P.op("act", lambda e: e.activation(out=junk[:, 0:512], in_=ya[:], func=AF.Square, accum_out=stat[:, SS_A + b:SS_A + b + 1]),
                     reads=["ya"], writes=["junk", ak])
                rstd_from_ss(SS_A + b, 512, ak)
                P.op("act", lambda e: e.activation(out=yna[:, b, :], in_=ya[:], func=AF.Copy, scale=stat[:, SS_A + b:SS_A + b + 1]),
                     reads=["ya", ak], writes=[f"yna{b}"])

            nit = len(order)
            for it in range(nit):
                a0(it)
            if nit > 0:
                a1(0)
                a1x(0)
                a2(0)
            if nit > 1:
                a1(1)
                a1x(1)
            for it in range(nit + 1):
                if it - 1 >= 0:
                    cst_ln(it - 1)
                if it + 2 < nit:
                    a1(it + 2)
                if it + 1 < nit:
                    a2(it + 1)
                if it < nit:
                    bst(it)
                if it + 2 < nit:
                    a1x(it + 2)
                if it - 1 >= 0:
                    cst_sp(it - 1)
                if it >= 1:
                    conv_step(2)

            if stage == 1 and nblk is not None:
                dump(expB[:, 0:512], 512, ["expB"], cast=True)
                P.barrier()
            if stage == 1 and nblk is None:
                dump(yna[:, 0, :], 512, ["yna0"], cast=True)
                dump(yna[:, 15, :], 512, ["yna15"], cast=True)
                dump(qT[:, :, 0:128], 512, ["qT0"], cast=True)
                dump(kT[:, 0:256], 256, ["kT0", "kT1"], cast=True)
                dump(vaug[:, 0:2], 264, ["va0", "va1", "vo0", "vones"], cast=True)
                dump(vaug[:, 17], 132, ["va17", "vo17"], cast=True)
                dump(stat[:, 0:18], 18, [f"ssx{i}" for i in range(18)], cast=False)
                P.barrier()
        if stage == 1:
            P.finish()
            P.emit()
            return nc
        P.barrier()

        with contextlib.ExitStack() as _es:
            w_out_bf = _es.enter_context(nc.sbuf_tensor("s_w_out_bf", [128, 8, D], BF16))
            wr = _es.enter_context(nc.sbuf_tensor("s_wr", [128, 8, 36], BF16))
            brt = _es.enter_context(nc.sbuf_tensor("s_brt", [128, 36], F32))
            E = _es.enter_context(nc.sbuf_tensor("s_E", [128, 2, 3, 512], BF16))
            den = _es.enter_context(nc.sbuf_tensor("s_den", [128, 8], F32))
            yb = _es.enter_context(nc.sbuf_tensor("s_yb", [128, 512], F32))
            ynb = _es.enter_context(nc.sbuf_tensor("s_ynb", [128, NB, 512], BF16))
            yT = _es.enter_context(nc.sbuf_tensor("s_yT", [128, 2, 8, 128], BF16))
            mTf = _es.enter_context(nc.sbuf_tensor("s_mTf", [128, 8, 128], BF16))
            for c in range(8):
                P.dma("pool", lambda e, c=c: e.dma_start(out=w_out_bf[:, c, :], in_=w_out_d[c * 128:(c + 1) * 128, :]),
                      writes=[f"w_out{c}"])
            P.dma("pool", lambda e: e.dma_start(out=wr[:], in_=wr_d.rearrange("(c p) f -> p c f", p=128)), writes=["wr"])
            P.dma("sp", lambda e: e.dma_start(out=brt[:], in_=br_d[0:1, :].broadcast_to([128, 36])), writes=["brt"])
            zrow = _es.enter_context(nc.sbuf_tensor("s_zrow", [128, D], BF16))
            P.op("dve", lambda e: e.memset(zrow[:], 0.0), writes=["zrow"])
            for r0 in range(0, NSLOT, 128 * 5):
                nr = min(128 * 5, NSLOT - r0)
                P.dma("sp", lambda e, r0=r0, nr=nr: e.dma_start(
                    out=mg_d[r0:r0 + nr, :].rearrange("(j p) d -> p j d", p=128),
                    in_=zrow[:].unsqueeze(1).broadcast_to([128, nr // 128, D])), reads=["zrow"], writes=["mg"])
            expB4 = expB[:].rearrange("p (kb hh q) -> p kb hh q", kb=3, hh=8)
            SB = (0, 1, 2)
            OB = (3, 4)
            TYB, OPB, RB = 5, (6, 7), 5

            def k1_s(b, kv):
                bi = b + 1
                pr = slice(kv * 64, (kv + 1) * 64)
                for kb in range(3):
                    kblk = bi - 1 + kb
                    P.op("pe", lambda e, kb=kb, kblk=kblk: e.matmul(bank[SB[kb]][:, :], lhsT=kT[pr, kblk * 128:(kblk + 1) * 128],
                                                                     rhs=qT[pr, :, b * 128:(b + 1) * 128], start=True, stop=True),
                         reads=[f"kT{kblk}", f"qT{b}"], writes=[bkey[SB[kb]]])

            def k1_e(b, kv):
                for kb in range(3):
                    P.op("act", lambda e, kb=kb: e.activation(out=E[:, kv, kb, :], in_=bank[SB[kb]][:, :], func=AF.Exp, scale=0.125),
                         reads=[bkey[SB[kb]]], writes=[f"E{kv}{kb}"])
                for kb in range(3):
                    P.op("dve", lambda e, kb=kb: e.tensor_tensor(
                        out=E[:, kv, kb, :].rearrange("p (g q) -> p g q", g=4), in0=E[:, kv, kb, :].rearrange("p (g q) -> p g q", g=4),
                        in1=expB4[:, kb, kv * 4:(kv + 1) * 4, :], op=ALU.mult),
                         reads=[f"E{kv}{kb}", "expB"], writes=[f"E{kv}{kb}"])

            def k1_pv(b, kv):
                bi = b + 1
                ob = OB[kv]
                for g in range(4):
                    for kb in range(3):
                        kblk = bi - 1 + kb
                        P.op("pe", lambda e, kb=kb, g=g, kblk=kblk: e.matmul(
                            bank[ob][:, g * 65:(g + 1) * 65], lhsT=E[:, kv, kb, g * 128:(g + 1) * 128],
                            rhs=vaug[:, kblk, kv, 0:65], start=(kb == 0), stop=(kb == 2)),
                             reads=[f"E{kv}{kb}", f"va{kblk}", f"vo{kblk}", "vones"], writes=[bkey[ob]])

            def k2a(b):
                for kv in range(2):
                    ob = OB[kv]
                    o3 = bank[ob][:, 0:260].rearrange("p (g d) -> p g d", g=4)
                    P.op("dve", lambda e, kv=kv, o3=o3: e.tensor_tensor(out=den[:, kv * 4:(kv + 1) * 4].unsqueeze(2), in0=o3[:, :, 64:65],
                                                                        in1=esink[:, kv * 4:(kv + 1) * 4].unsqueeze(2), op=ALU.add),
                         reads=[bkey[ob], "esink"], writes=[f"den{kv}"])
                    P.op("dve", lambda e, kv=kv: e.reciprocal(out=den[:, kv * 4:(kv + 1) * 4], in_=den[:, kv * 4:(kv + 1) * 4]),
                         reads=[f"den{kv}"], writes=[f"den{kv}"])
                    P.op("dve", lambda e, kv=kv, o3=o3: e.tensor_tensor(
                        out=yb[:, kv * 256:(kv + 1) * 256].rearrange("p (g d) -> p g d", g=4), in0=o3[:, :, 0:64],
                        in1=bc(den[:, kv * 4:(kv + 1) * 4], [128, 4, 64]), op=ALU.mult),
                         reads=[bkey[ob], f"den{kv}"], writes=[f"yb{kv}"])
                bk_ = f"ssb{b}"
                P.op("act", lambda e: e.activation(out=junk[:, 0:512], in_=yb[:], func=AF.Square, accum_out=stat[:, SS_B + b:SS_B + b + 1]),
                     reads=["yb0", "yb1"], writes=["junk", bk_])
                rstd_from_ss(SS_B + b, 512, bk_)

            def k2y(b):
                P.op("act", lambda e: e.activation(out=ynb[:, b, :], in_=yb[:], func=AF.Copy, scale=stat[:, SS_B + b:SS_B + b + 1]),
                     reads=["yb0", "yb1", f"ssb{b}"], writes=[f"ynb{b}"])

            def k2b_t(b):
                par = b % 2
                tyb = (5, 2)[par]
                opb = ((6, 7), (3, 4))[par]
                T0 = bank[tyb][:].bitcast(BF16)
                for c in range(8):
                    src = yna[:, b, c * 128:(c + 1) * 128] if c < 4 else ynb[:, b, (c - 4) * 128:(c - 3) * 128]
                    P.op("pe", lambda e, c=c, src=src: e.transpose(T0[:, c * 128:(c + 1) * 128], src, idb[:]),
                         reads=[f"yna{b}", f"ynb{b}", "idb"], writes=[bkey[tyb]])
                P.op("dve", lambda e: e.tensor_tensor(out=yT[:, par], in0=T0.rearrange("p (c t) -> p c t", c=8),
                                                      in1=bc(gout, [128, 8, 128]), op=ALU.mult),
                     reads=[bkey[tyb], "gvec"], writes=[f"yT{par}"])
                for n in range(2):
                    for c in range(8):
                        P.op("pe", lambda e, c=c, n=n: e.matmul(bank[opb[n]][:, :], lhsT=yT[:, par, c, :],
                                                                rhs=w_out_bf[:, c, n * 512:(n + 1) * 512], start=(c == 0), stop=(c == 7)),
                             reads=[f"yT{par}", f"w_out{c}"], writes=[bkey[opb[n]]])

            def k2b_h(b):
                opb = ((6, 7), (3, 4))[b % 2]
                for n in range(2):
                    P.op("dve", lambda e, n=n: e.tensor_tensor(out=h[:, b, n * 512:(n + 1) * 512], in0=bank[opb[n]][:, :],
                                                               in1=h[:, b, n * 512:(n + 1) * 512], op=ALU.add),
                         reads=[bkey[opb[n]], f"h{b}"], writes=[f"h{b}"])

            def k3a(b):
                mk = f"ssm{b}"
                P.op("act", lambda e: e.activation(out=junk[:], in_=h[:, b, :], func=AF.Square, accum_out=stat[:, SS_M + b:SS_M + b + 1]),
                     reads=[f"h{b}"], writes=["junk", mk])
                rstd_from_ss(SS_M + b, D, mk)

            def k3b(b):
                P.op("act", lambda e: e.activation(out=hnb[:, b, :], in_=h[:, b, :], func=AF.Copy, scale=stat[:, SS_M + b:SS_M + b + 1]),
                     reads=[f"h{b}", f"ssm{b}"], writes=[f"hnb{b}"])
                tb = 0
                TBm = bank[tb][:].bitcast(BF16)
                for c in range(8):
                    P.op("pe", lambda e, c=c: e.transpose(TBm[:, c * 128:(c + 1) * 128], hnb[:, b, c * 128:(c + 1) * 128], idb[:]),
                         reads=[f"hnb{b}", "idb"], writes=[bkey[tb]])
                P.op("dve", lambda e: e.tensor_tensor(out=mTf[:], in0=TBm.rearrange("p (c t) -> p c t", c=8),
                                                      in1=bc(gffn, [128, 8, 128]), op=ALU.mult),
                     reads=[bkey[tb], "gvec"], writes=["mTf"])
                for c in range(8):
                    P.op("pe", lambda e, c=c: e.matmul(bank[1][:, 0:36], lhsT=mTf[:, c, :], rhs=wr[:, c, :], start=(c == 0), stop=(c == 7)),
                         reads=["mTf", "wr"], writes=[bkey[1]])
                P.op("dve", lambda e: e.tensor_tensor(out=L[:, b, :], in0=bank[1][:, 0:36], in1=brt[:], op=ALU.add),
                     reads=[bkey[1], "brt"], writes=["L"])

            for i in range(-1, NB):
                a, m_ = i + 1, i
                va, vm = 0 <= a < NB, 0 <= m_ < NB
                if va:
                    k1_s(a, 0)
                    k1_e(a, 0)
                if vm:
                    k2a(m_)
                if va:
                    k1_s(a, 1)
                    k1_e(a, 1)
                    k1_pv(a, 0)
                    k1_pv(a, 1)
                if vm:
                    k2y(m_)
                conv_step(3)
            for j in range(NB + 1):
                o_, z = j, j - 1
                vo, vz = 0 <= o_ < NB, 0 <= z < NB
                if vo:
                    k2b_t(o_)
                if vz:
                    k3a(z)
                if vo:
                    k2b_h(o_)
                if vz:
                    k3b(z)
                conv_step(3)
            conv_step(len(conv_todo))
            if stage == 2:
                dump(h[:, 0, :], 1024, ["h0"])
                dump(h[:, 15, :], 1024, ["h15"])
                dump(L[:].rearrange("p b f -> p (b f)"), 576, ["L"])
                P.barrier()
    if stage == 2:
        P.finish()
        P.emit()
        return nc
    P.barrier()

    conv_todo.extend(conv_late)
    conv_step(len(conv_todo))
    e256 = cvec[:, 0:32]
    thr = cvec[:, 32:46]
    uvec = cvec[:, 46:77]
    pcol = cvec[:, 77:78]
    onesr = cvec[:, 78:110]
    with contextlib.ExitStack() as _es:
        r_a = _es.enter_context(nc.sbuf_tensor("s_r_a", [128, NB, 4], F32))
        r_b = _es.enter_context(nc.sbuf_tensor("s_r_b", [128, NB, 4], F32))
        r_s = _es.enter_context(nc.sbuf_tensor("s_r_s", [128, 8, NB], F32))
        lem = _es.enter_context(nc.sbuf_tensor("s_lem", [128, NB, NE], F32))
        Mb = _es.enter_context(nc.sbuf_tensor("s_Mb", [128, NB, NE], BF16))
        oh1 = _es.enter_context(nc.sbuf_tensor("s_oh1", [128, NB, NE], F32))
        oh2 = _es.enter_context(nc.sbuf_tensor("s_oh2", [128, NB, NE], F32))
        tot = _es.enter_context(nc.sbuf_tensor("s_tot", [128, NB, NE], F32))
        rk = _es.enter_context(nc.sbuf_tensor("s_rk", [128, NB, NE], F32))
        acc = _es.enter_context(nc.sbuf_tensor("s_acc", [128, 8, NE], F32))
        cmp = _es.enter_context(nc.sbuf_tensor("s_cmp", [128, NE * 31], F32))
        posf = _es.enter_context(nc.sbuf_tensor("s_posf", [128, 2, NB], F32))
        lg = L[:, :, 0:4]
        le = L[:, :, 4:36]
        gmax, sg, m1, m2, rr = (r_s[:, i, :] for i in range(5))
        w1 = w12[:, 0, :]
        w2 = w12[:, 1, :]
        P.op("dve", lambda e: e.tensor_reduce(out=gmax, in_=lg, axis=AX.X, op=ALU.max), reads=["L"], writes=["gmax"])
        P.op("dve", lambda e: e.tensor_tensor(out=r_a[:], in0=lg, in1=bc(gmax, [128, NB, 4]), op=ALU.is_equal),
             reads=["L", "gmax"], writes=["gm"])
        P.op("dve", lambda e: e.tensor_tensor(out=r_b[:], in0=lg, in1=bc(gmax, [128, NB, 4]), op=ALU.subtract),
             reads=["L", "gmax"], writes=["r_b"])
        P.op("act", lambda e: e.activation(out=r_b[:], in_=r_b[:], func=AF.Exp), reads=["r_b"], writes=["r_b"])
        P.op("dve", lambda e: e.tensor_reduce(out=sg, in_=r_b[:], axis=AX.X, op=ALU.add), reads=["r_b"], writes=["sg"])
        P.op("dve", lambda e: e.tensor_scalar(out=r_a[:], in0=r_a[:], scalar1=1.0, scalar2=BIG, op0=ALU.subtract, op1=ALU.mult),
             reads=["gm"], writes=["gm"])
        P.op("dve", lambda e: e.tensor_tensor(out=lem[:].rearrange("p b (g x) -> p b g x", g=4),
                                              in0=le.rearrange("p b (g x) -> p b g x", g=4),
                                              in1=bc(r_a[:], [128, NB, 4, 8]), op=ALU.add), reads=["L", "gm"], writes=["lem"])
        P.op("dve", lambda e: e.tensor_reduce(out=m1, in_=lem[:], axis=AX.X, op=ALU.max), reads=["lem"], writes=["m1"])
        P.op("dve", lambda e: e.tensor_tensor(out=oh1[:], in0=lem[:], in1=bc(m1, [128, NB, NE]), op=ALU.is_equal),
             reads=["lem", "m1"], writes=["oh1"])
        P.op("dve", lambda e: e.scalar_tensor_tensor(out=lem[:], in0=oh1[:], scalar=-BIG, in1=lem[:], op0=ALU.mult, op1=ALU.add),
             reads=["oh1", "lem"], writes=["lem"])
        P.op("dve", lambda e: e.tensor_reduce(out=m2, in_=lem[:], axis=AX.X, op=ALU.max), reads=["lem"], writes=["m2"])
        P.op("dve", lambda e: e.tensor_tensor(out=oh2[:], in0=lem[:], in1=bc(m2, [128, NB, NE]), op=ALU.is_equal),
             reads=["lem", "m2"], writes=["oh2"])
        P.op("dve", lambda e: e.tensor_tensor(out=rr, in0=m2, in1=m1, op=ALU.subtract), reads=["m1", "m2"], writes=["rr"])
        P.op("act", lambda e: e.activation(out=rr, in_=rr, func=AF.Exp), reads=["rr"], writes=["rr"])
        P.op("dve", lambda e: e.scalar_tensor_tensor(out=w1, in0=rr, scalar=1.0, in1=sg, op0=ALU.add, op1=ALU.mult),
             reads=["rr", "sg"], writes=["w1"])
        P.op("dve", lambda e: e.reciprocal(out=w1, in_=w1), reads=["w1"], writes=["w1"])
        P.op("dve", lambda e: e.tensor_tensor(out=w2, in0=rr, in1=w1, op=ALU.mult), reads=["rr", "w1"], writes=["w2"])
        P.op("dve", lambda e: e.tensor_tensor(out=Mb[:], in0=oh1[:], in1=oh2[:], op=ALU.add), reads=["oh1", "oh2"], writes=["Mb"])
        Mflat = Mb[:].rearrange("p b f -> p (b f)")
        P.op("pe", lambda e: e.matmul(bank[0][:, :], lhsT=trib[:, 0:128], rhs=Mflat, start=True, stop=True),
             reads=["Mb", "trib"], writes=[bkey[0]])
        P.op("pe", lambda e: e.matmul(bank[1][:, :], lhsT=trib[:, 128:256], rhs=Mflat, start=True, stop=True),
             reads=["Mb", "trib"], writes=[bkey[1]])
        totf = tot[:].rearrange("p b f -> p (b f)")
        tot_eb = totf.rearrange("p (f b) -> p f b", b=NB)
        P.op("dve", lambda e: e.tensor_copy(out=tot_eb, in_=bank[1][:, :].rearrange("p (b f) -> p f b", b=NB)),
             reads=[bkey[1]], writes=["tot"])
        Sf = lem[:].rearrange("p b f -> p (b f)")
        S_eb = Sf.rearrange("p (f b) -> p f b", b=NB)
        onesf = cmp[:, 0:NB * NE]
        P.op("dve", lambda e: e.memset(onesf, 1.0), writes=["cmp"])
        P.op("dve", lambda e: e.tensor_tensor_scan(out=Sf, data0=onesf, data1=totf, initial=0.0, op0=ALU.mult, op1=ALU.add),
             reads=["cmp", "tot", "lem"], writes=["lem"])
        cnt = acc[:, 0, :]
        ckey = "cnt"
        P.op("dve", lambda e: e.tensor_reduce(out=cnt, in_=tot_eb, axis=AX.X, op=ALU.add), reads=["tot"], writes=["cnt"])
        base = acc[:, 1, :]
        P.op("dve", lambda e: e.tensor_tensor(out=base, in0=S_eb[:, :, NB - 1], in1=cnt, op=ALU.subtract), reads=["lem", "cnt"], writes=["base"])
        P.op("dve", lambda e: e.tensor_tensor(out=Sf, in0=Sf, in1=totf, op=ALU.subtract), reads=["lem", "tot"], writes=["lem"])
        P.op("dve", lambda e: e.tensor_tensor(out=S_eb, in0=S_eb, in1=bc(base, [128, NE, NB]), op=ALU.subtract), reads=["lem", "base"], writes=["lem"])
        P.op("dve", lambda e: e.tensor_tensor(out=rk[:].rearrange("p b f -> p f b"), in0=bank[0][:, :].rearrange("p (b f) -> p f b", b=NB),
                                              in1=S_eb, op=ALU.add), reads=[bkey[0], "lem"], writes=[f"rk{b}" for b in range(NB)])
        ont, ote, ots, dlt = (acc[:, i, :] for i in range(2, 6))
        cmp3 = cmp[:, 0:NE * 14].rearrange("p (f k) -> p f k", k=14)
        P.op("dve", lambda e: e.tensor_tensor(out=cmp3, in0=bc(cnt, [128, NE, 14]), in1=thr.unsqueeze(1).broadcast_to([128, NE, 14]),
                                              op=ALU.is_gt), reads=[ckey, "cvec"], writes=["cmp"])
        P.op("dve", lambda e: e.tensor_reduce(out=ont, in_=cmp3, axis=AX.X, op=ALU.add), reads=["cmp"], writes=["ont"])
        P.op("dve", lambda e: e.tensor_tensor_scan(out=ote, data0=onesr, data1=ont, initial=0.0, op0=ALU.mult, op1=ALU.add),
             reads=["ont", "cvec"], writes=["ote"])
        P.op("dve", lambda e: e.tensor_tensor(out=ots, in0=ote, in1=ont, op=ALU.subtract), reads=["ote", "ont"], writes=["ots"])
        P.op("dve", lambda e: e.tensor_scalar(out=dlt, in0=ots, scalar1=128.0, scalar2=float(OVB - CAP), op0=ALU.mult, op1=ALU.add),
             reads=["ots"], writes=["dlt"])
        P.op("dve", lambda e: e.tensor_tensor(out=dlt, in0=dlt, in1=e256, op=ALU.subtract), reads=["dlt", "cvec"], writes=["dlt"])
        rkeys = [f"rk{b}" for b in range(NB)]
        P.op("dve", lambda e: e.tensor_scalar(out=lem[:], in0=rk[:], scalar1=float(CAP), scalar2=None, op0=ALU.is_ge),
             reads=rkeys, writes=["lem"])
        P.op("dve", lambda e: e.tensor_tensor(out=lem[:], in0=lem[:], in1=dlt.unsqueeze(1).broadcast_to([128, NB, NE]), op=ALU.mult),
             reads=["lem", "dlt"], writes=["lem"])
        P.op("dve", lambda e: e.tensor_tensor(out=rk[:], in0=rk[:], in1=e256.unsqueeze(1).broadcast_to([128, NB, NE]), op=ALU.add),
             reads=rkeys + ["cvec"], writes=["pos"])
        P.op("dve", lambda e: e.tensor_tensor(out=rk[:], in0=rk[:], in1=lem[:], op=ALU.add), reads=["pos", "lem"], writes=["pos"])
        for k, oh in ((0, oh1), (1, oh2)):
            P.op("dve", lambda e, oh=oh: e.tensor_tensor(out=lem[:], in0=rk[:], in1=oh[:], op=ALU.mult),
                 reads=["pos", f"oh{k + 1}", "lem"], writes=["lem"])
            P.op("dve", lambda e, k=k: e.tensor_reduce(out=posf[:, k, :], in_=lem[:], axis=AX.X, op=ALU.add),
                 reads=["lem"], writes=[f"posf{k}"])
        P.op("dve", lambda e: e.tensor_copy(out=idx[:], in_=posf[:]), reads=["posf0", "posf1"], writes=["idx"])
        eov = acc[:, 6, 0:NOV]
        cmpu = cmp[:, 0:NOV * NE].rearrange("p (u f) -> p u f", f=NE)
        P.op("dve", lambda e: e.tensor_tensor(out=cmpu, in0=ote.unsqueeze(1).broadcast_to([128, NOV, NE]), in1=bc(uvec, [128, NOV, NE]),
                                              op=ALU.is_le), reads=["ote", "cvec", "cmp"], writes=["cmp"])
        P.op("dve", lambda e: e.tensor_reduce(out=eov, in_=cmpu, axis=AX.X, op=ALU.add), reads=["cmp"], writes=["eov"])
        gf = acc[:, 7, 0:NOV]
        P.op("dve", lambda e: e.tensor_scalar(out=gf, in0=eov, scalar1=128.0, scalar2=None, op0=ALU.mult),
             reads=["eov"], writes=["gf"])
        P.op("dve", lambda e: e.tensor_scalar(out=gf, in0=gf, scalar1=pcol, scalar2=None, op0=ALU.add),
             reads=["gf", "cvec"], writes=["gf"])
        P.op("dve", lambda e: e.tensor_copy(out=gidx[:], in_=gf), reads=["gf"], writes=["gidx"])
        if stage == 3:
            dump(idx[:].rearrange("p k b -> p (k b)").bitcast(F32), 32, ["idx"])
            dump(w12[:].rearrange("p k b -> p (k b)"), 32, ["w1", "w2"])
            dump(acc[:].rearrange("p k f -> p (k f)"), 256, ["eov", "ote", "ots", "dlt", ckey, "ont"])
        P.barrier(dma=(stage == 3))
    if stage == 3:
        P.finish()
        P.emit()
        return nc

    NW = 4
    with contextlib.ExitStack() as _es:
        wall = _es.enter_context(nc.sbuf_tensor("s_wall", [128, NW, 3, 2048], BF16))
        xg = _es.enter_context(nc.sbuf_tensor("s_xg", [128, 3, 2, D], BF16))
        xgT = _es.enter_context(nc.sbuf_tensor("s_xgT", [128, 2, 8, CAP], BF16))
        sgt = _es.enter_context(nc.sbuf_tensor("s_sgt", [128, 2, CAP], BF16))
        hdT = _es.enter_context(nc.sbuf_tensor("s_hdT", [128, 2, 2, CAP], BF16))
        ogt = _es.enter_context(nc.sbuf_tensor("s_ogt", [128, 2, 2, D], BF16))
        n_ov = NOV if os.environ.get("K_NOV") is None else int(os.environ["K_NOV"])
        _regs = {}

        def breg(e, val):
            if val not in _regs:
                _regs[val] = e.to_reg(val)
            return _regs[val]

        tiles = [(ex * CAP, 2, ex, None) for ex in range(NE)] + [(OVB + u * 128, 1, None, u) for u in range(n_ov)]
        NT = len(tiles)
        mgkeys = [f"mgs{b}{k}" for b in range(NB) for k in range(2)]
        okeys = [f"og{i}" for i in range(NT)]

        def st_weights(i):
            row0, nj, ex, u = tiles[i]
            ws = i % NW
            dst = wall[:, ws].rearrange("p k f -> p (k f)")
            if ex is not None:
                P.dma("pool", lambda e: e.dma_start(out=dst, in_=wbf_d[ex * 128:(ex + 1) * 128, :]),
                      reads=[f"wbf{ex}_{k}" for k in range(3)], writes=[f"w{ws}"])
            else:
                P.dma("pool", lambda e: e.indirect_dma_start(
                    out=dst, out_offset=None, in_=wbf_d[:, :], in_offset=bass.IndirectOffsetOnAxis(ap=gidx[:, u:u + 1], axis=0),
                    bounds_check=breg(e, NE * 128 - 1), oob_is_err=False), reads=["gidx"], writes=[f"w{ws}"])

        def st_load(i):
            row0, nj, ex, u = tiles[i]
            s3 = i % 3
            P.dma("sp", lambda e: e.dma_start(out=xg[:, s3, 0:nj, :],
                                              in_=mg_d[row0:row0 + nj * 128, :].rearrange("(j p) d -> p j d", p=128)),
                  reads=mgkeys, writes=[f"xg{s3}"])

        def st_T(i):
            row0, nj, ex, u = tiles[i]
            s3, sl = i % 3, i % 2
            for j in range(nj):
                TB = bank[j][:].bitcast(BF16)
                for c in range(8):
                    P.op("pe", lambda e, j=j, c=c, TB=TB: e.transpose(TB[:, c * 128:(c + 1) * 128],
                                                                      xg[:, s3, j, c * 128:(c + 1) * 128], idb[:]),
                         reads=[f"xg{s3}", "idb"], writes=[bkey[j]])
                P.op("dve", lambda e, j=j, TB=TB: e.tensor_tensor(out=xgT[:, sl, :, j * 128:(j + 1) * 128],
                                                                  in0=TB.rearrange("p (c t) -> p c t", c=8),
                                                                  in1=bc(gffn, [128, 8, 128]), op=ALU.mult),
                     reads=[bkey[j], "gvec"], writes=[f"xgT{sl}"])

        def st_GU(i):
            row0, nj, ex, u = tiles[i]
            sl, ws, ns = i % 2, i % NW, nj * 128
            for fc in range(2):
                gb = bank[2 + fc]
                for half in range(2):
                    for c in range(8):
                        P.op("pe", lambda e, c=c, gb=gb, fc=fc, half=half: e.matmul(
                            gb[:, half * CAP:half * CAP + ns], lhsT=wall[:, ws, half, c * 256 + fc * 128:c * 256 + (fc + 1) * 128],
                            rhs=xgT[:, sl, c, 0:ns], start=(c == 0), stop=(c == 7)),
                             reads=[f"w{ws}", f"xgT{sl}"], writes=[bkey[2 + fc]])
                P.op("act", lambda e, fc=fc, gb=gb: e.activation(out=sgt[:, fc, 0:ns], in_=gb[:, 0:ns], func=AF.Silu),
                     reads=[bkey[2 + fc]], writes=[f"sgt{fc}"])
                P.op("dve", lambda e, fc=fc, gb=gb: e.tensor_tensor(out=hdT[:, sl, fc, 0:ns], in0=gb[:, CAP:CAP + ns],
                                                                    in1=sgt[:, fc, 0:ns], op=ALU.mult),
                     reads=[bkey[2 + fc], f"sgt{fc}"], writes=[f"hdT{sl}{fc}"])

        def st_D(i):
            row0, nj, ex, u = tiles[i]
            sl, ws, ns = i % 2, i % NW, nj * 128
            for j in range(nj):
                for n in range(2):
                    ob = 4 + j * 2 + n
                    for fc in range(2):
                        P.op("pe", lambda e, fc=fc, ob=ob, j=j, n=n: e.matmul(
                            bank[ob][:, :], lhsT=hdT[:, sl, fc, j * 128:(j + 1) * 128],
                            rhs=wall[:, ws, 2, fc * D + n * 512:fc * D + (n + 1) * 512], start=(fc == 0), stop=(fc == 1)),
                             reads=[f"hdT{sl}{fc}", f"w{ws}"], writes=[bkey[ob]])
                    if n == 0:
                        P.op("act", lambda e, ob=ob, j=j, n=n: e.activation(out=ogt[:, sl, j, n * 512:(n + 1) * 512], in_=bank[ob][:, :],
                                                                           func=AF.Copy), reads=[bkey[ob]], writes=[f"ogt{sl}{j}{n}"])
                    else:
                        P.op("dve", lambda e, ob=ob, j=j, n=n: e.tensor_copy(out=ogt[:, sl, j, n * 512:(n + 1) * 512], in_=bank[ob][:, :]),
                             reads=[bkey[ob]], writes=[f"ogt{sl}{j}{n}"])
            P.dma("sp", lambda e: e.dma_start(out=og_d[row0:row0 + ns, :].rearrange("(j p) d -> p j d", p=128), in_=ogt[:, sl, 0:nj, :]),
                  reads=[f"ogt{sl}{j}{n}" for j in range(nj) for n in range(2)], writes=[okeys[i]])

        for i in range(min(3, NT)):
            st_weights(i)
        for b in range(NB):
            for k in range(2):
                P.dma("pool", lambda e, b=b, k=k: e.indirect_dma_start(
                    out=mg_d[:, :], out_offset=bass.IndirectOffsetOnAxis(ap=idx[:, k, b:b + 1], axis=0),
                    in_=hnb[:, b, :], in_offset=None), reads=["idx"], writes=[f"mgs{b}{k}"])
        st_load(0)
        st_load(1)
        st_T(0)
        for i in range(NT + 1):
            if i + 2 < NT:
                st_load(i + 2)
            if i + 1 < NT:
                st_T(i + 1)
            if i - 1 >= 0:
                st_D(i - 1)
            if i + 3 < NT:
                st_weights(i + 3)
            if i < NT:
                st_GU(i)
        P.barrier()

    with contextlib.ExitStack() as _es:
        wpg = _es.enter_context(nc.sbuf_tensor("s_wpg", [128, 8, D], BF16))
        wpp = _es.enter_context(nc.sbuf_tensor("s_wpp", [128, 2, D], BF16))
        rows = _es.enter_context(nc.sbuf_tensor("s_rows", [128, 3, D], F32))
        pb = _es.enter_context(nc.sbuf_tensor("s_pb", [128, 2, 256], BF16))
        pT = _es.enter_context(nc.sbuf_tensor("s_pT", [128, 2, 128], BF16))
        pp = _es.enter_context(nc.sbuf_tensor("s_pp", [128, 2, D], F32))
        hb = _es.enter_context(nc.sbuf_tensor("s_hb", [128, D], BF16))
        hT = _es.enter_context(nc.sbuf_tensor("s_hT", [128, 2, 8, 128], BF16))
        gt = _es.enter_context(nc.sbuf_tensor("s_gt", [128, D], F32))
        ot = _es.enter_context(nc.sbuf_tensor("s_ot", [128, 2, D], F32))
        ogc = _es.enter_context(nc.sbuf_tensor("s_ogc", [128, 2, 2, D], BF16))
        brow = _es.enter_context(nc.sbuf_tensor("s_brow", [1, D], BF16))
        one1 = _es.enter_context(nc.sbuf_tensor("s_one1", [1, 128], BF16))
        P.dma("pool", lambda e: e.dma_start(out=brow[:], in_=rows_d[0:1, :]), writes=["brow"])
        P.op("dve", lambda e: e.memset(one1[:], 1.0), writes=["one1"])
        P.dma("pool", lambda e: e.dma_start(out=wpp[:], in_=wpp_d.rearrange("(c p) f -> p c f", p=128)), writes=["wpp"])
        P.dma("pool", lambda e: e.dma_start(out=wpg[:], in_=wpg_d.rearrange("(c p) f -> p c f", p=128)), writes=["wpg"])
        for i in range(3):
            P.dma("sp", lambda e, i=i: e.dma_start(out=rows[:, i, :], in_=rows_d[i:i + 1, :].broadcast_to([128, D])),
                  writes=["rows"])

        def s0a(b):
            sl = b % 2
            P.dma("pool", lambda e: e.dma_start(out=pb[:, sl, :], in_=pin[b * 128:(b + 1) * 128, :]), writes=[f"pb{sl}"])
            for k in range(2):
                P.dma("pool", lambda e, k=k: e.indirect_dma_start(
                    out=ogc[:, sl, k, :], out_offset=None, in_=og_d[:, :],
                    in_offset=bass.IndirectOffsetOnAxis(ap=idx[:, k, b:b + 1], axis=0)), writes=[f"ogc{sl}{k}"])

        def s0b(b):
            sl = b % 2
            for k in range(2):
                P.op("dve", lambda e, k=k: e.scalar_tensor_tensor(
                    out=h[:, b, :], in0=ogc[:, sl, k, :], scalar=w12[:, k, b:b + 1], in1=h[:, b, :], op0=ALU.mult, op1=ALU.add),
                     reads=[f"ogc{sl}{k}", f"h{b}"], writes=[f"h{b}"])

        def s1(b):
            sl = b % 2
            T0 = bank[0][:].bitcast(BF16)
            for k in range(2):
                P.op("pe", lambda e, k=k: e.transpose(T0[:, k * 128:(k + 1) * 128], pb[:, sl, k * 128:(k + 1) * 128], idb[:]),
                     reads=[f"pb{sl}", "idb"], writes=[bkey[0]])
            P.op("act", lambda e: e.activation(out=pT[:], in_=T0[:, 0:256].rearrange("p (k t) -> p k t", k=2), func=AF.Copy),
                 reads=[bkey[0]], writes=["pT"])
            P.op("act", lambda e: e.activation(out=hb[:], in_=h[:, b, :], func=AF.Copy), reads=[f"h{b}"], writes=["hb"])
            for n in range(2):
                for k in range(2):
                    P.op("pe", lambda e, k=k, n=n: e.matmul(bank[1 + n][:, :], lhsT=pT[:, k, :], rhs=wpp[:, k, n * 512:(n + 1) * 512],
                                                            start=(k == 0), stop=(k == 1)),
                         reads=["pT", "wpp"], writes=[bkey[1 + n]])
            T3 = bank[3][:].bitcast(BF16)
            for c in range(8):
                P.op("pe", lambda e, c=c: e.transpose(T3[:, c * 128:(c + 1) * 128], hb[:, c * 128:(c + 1) * 128], idb[:]),
                     reads=["hb", "idb"], writes=[bkey[3]])
            for n in range(2):
                P.op("act", lambda e, n=n: e.activation(out=pp[:, sl, n * 512:(n + 1) * 512], in_=bank[1 + n][:, :], func=AF.Copy),
                     reads=[bkey[1 + n]], writes=[f"pp{sl}"])
            P.op("act", lambda e: e.activation(out=hT[:, sl], in_=T3.rearrange("p (c t) -> p c t", c=8), func=AF.Copy),
                 reads=[bkey[3]], writes=[f"hT{sl}"])

        def s1b(b):
            sl = b % 2
            pk = f"ssp{b}"
            P.op("act", lambda e: e.activation(out=junk[:], in_=pp[:, sl, :], func=AF.Square, accum_out=stat[:, SS_P + b:SS_P + b + 1]),
                 reads=[f"pp{sl}"], writes=["junk", pk])
            rstd_from_ss(SS_P + b, D, pk)

        def s2a(b):
            sl = b % 2
            pk = f"ssp{b}"
            for n in range(2):
                for c in range(8):
                    P.op("pe", lambda e, c=c, n=n: e.matmul(bank[4 + n][:, :], lhsT=hT[:, sl, c, :], rhs=wpg[:, c, n * 512:(n + 1) * 512],
                                                            start=(c == 0), stop=False),
                         reads=[f"hT{sl}", "wpg"], writes=[bkey[4 + n]])
                P.op("pe", lambda e, n=n: e.matmul(bank[4 + n][:, :], lhsT=one1[0:1, :], rhs=brow[0:1, n * 512:(n + 1) * 512],
                                                   start=False, stop=True),
                     reads=["one1", "brow"], writes=[bkey[4 + n]])
            for n in range(2):
                P.op("act", lambda e, n=n: e.activation(out=gt[:, n * 512:(n + 1) * 512], in_=bank[4 + n][:, :], func=AF.Sigmoid),
                     reads=[bkey[4 + n]], writes=["gt"])
            P.op("dve", lambda e: e.scalar_tensor_tensor(out=pp[:, sl, :], in0=pp[:, sl, :], scalar=stat[:, SS_P + b:SS_P + b + 1],
                                                         in1=rows[:, 1, :], op0=ALU.mult, op1=ALU.mult),
                 reads=[f"pp{sl}", pk, "rows"], writes=[f"pp{sl}"])
            P.op("dve", lambda e: e.tensor_tensor(out=pp[:, sl, :], in0=pp[:, sl, :], in1=gt[:], op=ALU.mult),
                 reads=[f"pp{sl}", "gt"], writes=[f"pp{sl}"])
            P.op("pool", lambda e: e.tensor_tensor(out=h[:, b, :], in0=h[:, b, :], in1=pp[:, sl, :], op=ALU.add),
                 reads=[f"pp{sl}", f"h{b}"], writes=[f"h{b}"])

        def s2b(b):
            fk = f"ssf{b}"
            P.op("act", lambda e: e.activation(out=junk[:], in_=h[:, b, :], func=AF.Square, accum_out=stat[:, SS_F + b:SS_F + b + 1]),
                 reads=[f"h{b}"], writes=["junk", fk])
            rstd_from_ss(SS_F + b, D, fk)

        def s2c(b):
            sl = b % 2
            fk = f"ssf{b}"
            P.op("dve", lambda e: e.scalar_tensor_tensor(out=ot[:, sl, :], in0=h[:, b, :], scalar=stat[:, SS_F + b:SS_F + b + 1],
                                                         in1=rows[:, 2, :], op0=ALU.mult, op1=ALU.mult),
                 reads=[f"h{b}", fk, "rows"], writes=[f"ot{sl}"])
            P.dma("sp", lambda e: e.dma_start(out=out_d[b * 128:(b + 1) * 128, :], in_=ot[:, sl, :]),
                  reads=[f"ot{sl}"], is_out=True)

        s0a(0)
        s0a(1)
        s0b(0)
        s0b(1)
        s1(0)
        s0a(2)
        s1b(0)
        for i in range(NB + 2):
            if i + 2 < NB:
                s0b(i + 2)
            if i + 1 < NB:
                s1(i + 1)
            if i + 3 < NB:
                s0a(i + 3)
            if i < NB:
                s2a(i)
            if 0 <= i - 2 < NB:
                s2c(i - 2)
            if 0 <= i - 1 < NB:
                s2b(i - 1)
            if i + 1 < NB:
                s1b(i + 1)
    P.finish()
    P.emit()
    return nc


def _t5_bucket(rel):
    n = 16
    max_exact = 8
    ret = np.where(rel > 0, n, 0)
    a = np.abs(rel)
    af = np.maximum(a, 1).astype(np.float32)
    large = max_exact + (np.log(af / np.float32(max_exact)) / np.float32(np.log(128 / 8)) * np.float32(n - max_exact)).astype(np.int32)
    large = np.minimum(large, n - 1)
    return ret + np.where(a < max_exact, a, large)


def _static_tables():
    j = np.arange(128)[:, None, None]
    kb = np.arange(3)[None, :, None]
    q = np.arange(128)[None, None, :]
    rel = (kb - 1) * 128 + j - q
    bucket = _t5_bucket(rel)
    valid = (np.abs(rel) <= 128).astype(np.float32)
    return bucket, valid


def _w_in_layout(w):
    w = w.copy()
    w[:, 1024:1536] = w[:, 1024:1536].reshape(D, 2, 4, 64).transpose(0, 2, 1, 3).reshape(D, 512)
    return np.ascontiguousarray(w)


def _expert_layout(wg, wu, wd):
    out = np.empty((NE, 128, 3, 2048), np.float32)
    out[:, :, 0, :] = wg.reshape(NE, 8, 128, 256).transpose(0, 2, 1, 3).reshape(NE, 128, 2048)
    out[:, :, 1, :] = wu.reshape(NE, 8, 128, 256).transpose(0, 2, 1, 3).reshape(NE, 128, 2048)
    out[:, :, 2, :] = wd.reshape(NE, 2, 128, D).transpose(0, 2, 1, 3).reshape(NE, 128, 2048)
    return out.reshape(NE * 128, 3, 2048)


def _tri_const():
    t = np.zeros((128, 256), np.float32)
    t[:, 0:128] = (np.arange(128)[:, None] < np.arange(128)[None, :]).astype(np.float32)
    t[:, 128:256] = 1.0
    return t


def _cvec_const():
    c = np.zeros((128, 128), np.float32)
    c[:, 0:32] = np.arange(32)[None, :] * CAP
    c[:, 32:46] = CAP + 128 * np.arange(14)[None, :]
    c[:, 46:77] = np.arange(31)[None, :]
    c[:, 77] = np.arange(128)
    c[:, 78:110] = 1.0
    return c


def make_in_maps(inp):
    f = lambda a: np.ascontiguousarray(np.asarray(a, dtype=np.float32))
    x = f(inp["x"])
    p = f(inp["p"])[0]
    bucket, valid = _static_tables()
    rel_bias = f(inp["rel_bias"])
    bg = rel_bias[bucket]
    biasg = np.ascontiguousarray(bg.transpose(0, 1, 3, 2)).reshape(128, 3072)
    maskc = np.ascontiguousarray(np.broadcast_to(valid[:, :, None, :], (128, 3, 8, 128))).reshape(128, 3072)
    colmajor = lambda v: np.ascontiguousarray(f(v).reshape(8, 128).T)
    gvec = np.concatenate([colmajor(inp["g_mix"][0]), colmajor(inp["g_out_grp"][0]), colmajor(inp["g_ffn"][0])], axis=1)
    shared = {
        "biasg": biasg, "maskc": maskc, "ident": np.eye(128, dtype=np.float32),
        "w_in": _w_in_layout(f(inp["w_in"][0])), "gvec": np.ascontiguousarray(gvec),
        "lnv": np.ascontiguousarray(np.stack([f(inp["ln_v_g"][0]), f(inp["ln_v_b"][0])])),
        "wsT": np.ascontiguousarray(f(inp["w_spatial"][0]).transpose(2, 0, 1)).reshape(128, 512),
        "bsT": np.ascontiguousarray(f(inp["b_spatial"][0]).T),
        "sink": f(inp["sink"]), "w_out": f(inp["w_out"][0]),
        "wr": np.ascontiguousarray(np.concatenate([f(inp["w_router_group"][0]), f(inp["w_router_expert"][0])], axis=1)),
        "br": np.ascontiguousarray(np.concatenate([f(inp["b_router_group"][0]), f(inp["b_router_expert"][0])])[None, :]),
        "w_exp": _expert_layout(f(inp["w_gate_e"][0]), f(inp["w_up_e"][0]), f(inp["w_down_e"][0])),
        "w_ple_proj": f(inp["w_ple_proj"][0]), "w_ple_gate": f(inp["w_ple_gate"][0]),
        "rows": np.ascontiguousarray(np.stack([f(inp["b_ple_gate"][0]), f(inp["g_ple"][0]), f(inp["g_final"])])),
        "tri": _tri_const(), "cvec": _cvec_const(),
    }
    maps = []
    cps = NCORES // BATCH
    for core in range(NCORES):
        bidx, ci = divmod(core, cps)
        t0 = ci * TPC
        xh = np.zeros((NBH * 128, D), np.float32)
        lo, hi = t0 - 128, t0 + TPC + 128
        slo, shi = max(lo, 0), min(hi, SEQ)
        xh[slo - lo:shi - lo] = x[bidx, slo:shi]
        flags = np.zeros((128, 2), np.float32)
        flags[:, 0] = 1.0 if lo >= 0 else 0.0
        flags[:, 1] = 1.0 if hi <= SEQ else 0.0
        m = dict(shared)
        m["xh"] = xh
        m["p"] = np.ascontiguousarray(p[bidx, t0:t0 + TPC])
        m["flags"] = flags
        maps.append(m)
    return maps


_NC_CACHE = {}


def kernel(**inputs):
    if "nc" not in _NC_CACHE:
        _NC_CACHE["nc"] = build_program()
    nc = _NC_CACHE["nc"]
    maps = make_in_maps(inputs)
    res = run_bass_kernel_spmd(nc, maps, core_ids=list(range(NCORES)))
    cps = NCORES // BATCH
    out = np.empty((BATCH, SEQ, D), np.float32)
    for core in range(NCORES):
        bidx, ci = divmod(core, cps)
        out[bidx, ci * TPC:(ci + 1) * TPC] = res.results[core]["out"]
    return out
```
